# Optimizing a Trainium2 kernel written in Bass

```python
import math
import jax, jax.numpy as jnp
from jax import lax
import numpy as np

D_MODEL = 1024
BATCH = 4
SEQ = 8192
DEPTH = 1
DEC_BATCH = 16
DEC_SEQ = 2048
PAST_LEN = 128

HEAD_DIM = 64
A_Q_HEADS = 8
A_KV_HEADS = 2
A_HALF_WINDOW = 128
B_GROUPS = ((128, 1), (512, 4), (2048, 16))
B_HEADS_PER_GROUP = 4
N_BUCKETS = 32
MAX_DISTANCE = 1024
N_EXPERTS = 64
TOP_K = 6
N_EXPERT_GROUPS = 8
TOPK_GROUPS = 4
D_EXPERT = 256
D_SHARED = 256
ROUTED_SCALE = 2.5
MOE_BLOCK = 128
RMS_EPS = 1e-6
NEG_INF = -1e30

A_Q_W = A_Q_HEADS * HEAD_DIM
A_KV_W = A_KV_HEADS * HEAD_DIM
B_HEADS = len(B_GROUPS) * B_HEADS_PER_GROUP
B_W = B_HEADS * HEAD_DIM
B_OUT_W = B_HEADS_PER_GROUP * HEAD_DIM
N_BIAS_HEADS = A_Q_HEADS + B_HEADS
IN_WIDTHS = (A_Q_W, A_KV_W, A_KV_W, B_W, B_W, B_W, D_MODEL, D_MODEL)
D_IN = A_Q_W + 2 * A_KV_W + 3 * B_W + 2 * D_MODEL

kernel_name = "hybrid_gated_window_dilated_moe_encoder"


def _rmsnorm(x, g):
    xf = x.astype(jnp.float32)
    y = xf * lax.rsqrt(jnp.mean(xf * xf, axis=-1, keepdims=True) + RMS_EPS)
    return (y * g.astype(jnp.float32)).astype(x.dtype)


def _rel_bucket(rel):
    half = N_BUCKETS // 2
    max_exact = half // 2
    n = np.abs(rel)
    large = max_exact + (np.log(np.maximum(n, 1) / max_exact) / math.log(MAX_DISTANCE / max_exact)
                         * (half - max_exact)).astype(np.int32)
    large = np.minimum(large, half - 1)
    return ((rel > 0).astype(np.int32) * half + np.where(n < max_exact, n, large)).astype(np.int32)


def _banded_attention(q, k, v, half, dist_scale, bias_tab, sink=None):
    b, L, kv, g, dh = q.shape
    nb = -(-L // half)
    pad = nb * half - L
    qb = jnp.pad(q, ((0, 0), (0, pad), (0, 0), (0, 0), (0, 0))).reshape(b, nb, half, kv, g, dh)
    kp = jnp.pad(k, ((0, 0), (half, half + pad), (0, 0), (0, 0))).reshape(b, nb + 2, half, kv, dh)
    vp = jnp.pad(v, ((0, 0), (half, half + pad), (0, 0), (0, 0))).reshape(b, nb + 2, half, kv, dh)
    kw = jnp.concatenate([kp[:, :-2], kp[:, 1:-1], kp[:, 2:]], axis=2)
    vw = jnp.concatenate([vp[:, :-2], vp[:, 1:-1], vp[:, 2:]], axis=2)
    s = jnp.einsum('bnqkgd,bnskd->bnkgqs', qb, kw, preferred_element_type=jnp.float32) * (dh ** -0.5)
    rel = np.arange(3 * half)[None, :] - half - np.arange(half)[:, None]
    bucket = _rel_bucket(rel * dist_scale)
    bias = jnp.transpose(bias_tab[bucket].astype(jnp.float32), (2, 0, 1)).reshape(kv, g, half, 3 * half)
    key_pos = np.arange(nb)[:, None] * half + np.arange(3 * half)[None, :] - half
    mask = (np.abs(rel) <= half)[None] & ((key_pos >= 0) & (key_pos < L))[:, None, :]
    logits = jnp.where(mask[None, :, None, None], s + bias, NEG_INF)
    m = jnp.max(logits, axis=-1)
    if sink is not None:
        sink_b = sink.astype(jnp.float32).reshape(kv, g)[..., None]
        m = jnp.maximum(m, sink_b)
    p = jnp.exp(logits - m[..., None])
    den = jnp.sum(p, axis=-1)
    if sink is not None:
        den = den + jnp.exp(sink_b - m)
    o = jnp.einsum('bnkgqs,bnskd->bnqkgd', p.astype(v.dtype), vw, preferred_element_type=jnp.float32)
    den_t = jnp.transpose(den, (0, 1, 4, 2, 3))
    o = (o / den_t[..., None]).astype(q.dtype).reshape(b, nb * half, kv, g, dh)[:, :L]
    lse = (jnp.transpose(m, (0, 1, 4, 2, 3)) + jnp.log(den_t)).reshape(b, nb * half, kv, g)[:, :L]
    return o, lse


def _dilated_branch(q, k, v, rel_bias):
    bsz, S, _ = q.shape
    outs, lses = [], []
    for gi, (w, d) in enumerate(B_GROUPS):
        sl = slice(gi * B_OUT_W, (gi + 1) * B_OUT_W)

        def to_res(t):
            t = t[..., sl].reshape(bsz, S // d, d, B_HEADS_PER_GROUP, HEAD_DIM)
            return jnp.swapaxes(t, 1, 2).reshape(bsz * d, S // d, B_HEADS_PER_GROUP, HEAD_DIM)

        qg, kg, vg = to_res(q), to_res(k), to_res(v)
        h0 = A_Q_HEADS + gi * B_HEADS_PER_GROUP
        o, lse = _banded_attention(qg[:, :, :, None, :], kg, vg, w // (2 * d), d,
                                   rel_bias[:, h0:h0 + B_HEADS_PER_GROUP])
        o = jnp.swapaxes(o.reshape(bsz, d, S // d, B_HEADS_PER_GROUP, HEAD_DIM), 1, 2)
        lse = jnp.swapaxes(lse.reshape(bsz, d, S // d, B_HEADS_PER_GROUP), 1, 2)
        outs.append(o.reshape(bsz, S, B_HEADS_PER_GROUP, HEAD_DIM))
        lses.append(lse.reshape(bsz, S, B_HEADS_PER_GROUP))
    wts = jax.nn.softmax(jnp.stack(lses), axis=0)
    ob = jnp.einsum('gbsh,gbshd->bshd', wts.astype(q.dtype), jnp.stack(outs))
    return ob.reshape(bsz, S, B_OUT_W)


def _token_mixers(h, rel_bias, w_in, sink, w_pa, w_pb, w_o):
    bsz, S, _ = h.shape
    proj = h @ w_in
    offs = np.cumsum(np.array(IN_WIDTHS))[:-1]
    qa, ka, va, qb, kb, vb, ga, gb = jnp.split(proj, offs, axis=-1)
    G = A_Q_HEADS // A_KV_HEADS
    ya, _ = _banded_attention(qa.reshape(bsz, S, A_KV_HEADS, G, HEAD_DIM),
                              ka.reshape(bsz, S, A_KV_HEADS, HEAD_DIM),
                              va.reshape(bsz, S, A_KV_HEADS, HEAD_DIM),
                              A_HALF_WINDOW, 1, rel_bias[:, :A_Q_HEADS], sink)
    ya = ya.reshape(bsz, S, A_Q_W)
    yb = _dilated_branch(qb, kb, vb, rel_bias)
    merged = jax.nn.sigmoid(ga) * (ya @ w_pa) + jax.nn.sigmoid(gb) * (yb @ w_pb)
    return merged @ w_o


def _moe(h, w_router, router_bias, w_gate, w_up, w_down, ws_gate, ws_up, ws_down):
    shape = h.shape
    x = h.reshape(-1, D_MODEL)
    n = x.shape[0]
    scores = jax.nn.sigmoid((x @ w_router).astype(jnp.float32))
    sel = scores + router_bias.astype(jnp.float32)
    grp_score = jnp.sum(lax.top_k(sel.reshape(n, N_EXPERT_GROUPS, -1), 2)[0], axis=-1)
    _, gidx = lax.top_k(grp_score, TOPK_GROUPS)
    gmask = jnp.sum(jax.nn.one_hot(gidx, N_EXPERT_GROUPS, dtype=jnp.float32), axis=1) > 0
    emask = jnp.repeat(gmask, N_EXPERTS // N_EXPERT_GROUPS, axis=1)
    _, eidx = lax.top_k(jnp.where(emask, sel, NEG_INF), TOP_K)
    wts = jnp.take_along_axis(scores, eidx, axis=1)
    wts = wts / jnp.sum(wts, axis=-1, keepdims=True) * ROUTED_SCALE
    nk = n * TOP_K
    M = MOE_BLOCK
    flat_e = eidx.reshape(-1).astype(jnp.int32)
    flat_t = jnp.repeat(jnp.arange(n, dtype=jnp.int32), TOP_K)
    flat_w = wts.reshape(-1)
    order = jnp.argsort(flat_e)
    se = flat_e[order]
    counts = jnp.bincount(flat_e, length=N_EXPERTS).astype(jnp.int32)
    padded = (counts + M - 1) // M * M
    start = jnp.cumsum(counts) - counts
    pstart = jnp.cumsum(padded) - padded
    dest = pstart[se] + jnp.arange(nk, dtype=jnp.int32) - start[se]
    nblk = -(-nk // M) + N_EXPERTS
    R = nblk * M
    row_tok = jnp.full((R,), n, jnp.int32).at[dest].set(flat_t[order])
    row_w = jnp.zeros((R,), jnp.float32).at[dest].set(flat_w[order])
    blk_end = jnp.cumsum(padded) // M
    blk_e = jnp.minimum(jnp.searchsorted(blk_end, jnp.arange(nblk), side='right'), N_EXPERTS - 1)
    x_pad = jnp.concatenate([x, jnp.zeros((1, D_MODEL), x.dtype)], axis=0)

    def expert_block(args):
        rows, e = args
        xb = x_pad[rows]
        hb = jax.nn.silu(xb @ w_gate[e]) * (xb @ w_up[e])
        return hb @ w_down[e]

    out = lax.map(expert_block, (row_tok.reshape(nblk, M), blk_e))
    out = out.reshape(R, D_MODEL) * row_w[:, None].astype(out.dtype)
    routed = jax.ops.segment_sum(out, row_tok, num_segments=n + 1)[:n]
    shared = (jax.nn.silu(x @ ws_gate) * (x @ ws_up)) @ ws_down
    return (routed + shared).reshape(shape)


def _layer(x, c, rel_bias, w_ada, b_ada, norm1, w_in, sink, w_pa, w_pb, w_o, norm2,
           w_router, router_bias, w_gate, w_up, w_down, ws_gate, ws_up, ws_down):
    mod = (jax.nn.silu(c) @ w_ada + b_ada)[:, None, :]
    sh1, sc1, g1, sh2, sc2, g2 = jnp.split(mod, 6, axis=-1)
    h = _rmsnorm(x, norm1) * (1 + sc1) + sh1
    x = x + g1 * _token_mixers(h, rel_bias, w_in, sink, w_pa, w_pb, w_o)
    h = _rmsnorm(x, norm2) * (1 + sc2) + sh2
    x = x + g2 * _moe(h, w_router, router_bias, w_gate, w_up, w_down, ws_gate, ws_up, ws_down)
    return x


def setup_inputs(seed: int = 0) -> dict:
    key = jax.random.key(seed)
    ks = jax.random.split(key, 24)
    f32 = jnp.float32
    nrm = lambda k, shape, s: jax.random.normal(k, shape, f32) * s
    L = DEPTH
    return {
        "x_prompt": nrm(ks[0], (BATCH, SEQ, D_MODEL), 1.0),
        "x_sample": nrm(ks[1], (DEC_BATCH, DEC_SEQ, D_MODEL), 1.0),
        "c_prompt": nrm(ks[2], (BATCH, D_MODEL), 1.0),
        "c_sample": nrm(ks[3], (DEC_BATCH, D_MODEL), 1.0),
        "rel_bias": nrm(ks[4], (N_BUCKETS, N_BIAS_HEADS), 0.5),
        "w_ada": nrm(ks[5], (L, D_MODEL, 6 * D_MODEL), 0.5 * D_MODEL ** -0.5),
        "b_ada": nrm(ks[6], (L, 6 * D_MODEL), 0.01),
        "norm1": 1.0 + nrm(ks[7], (L, D_MODEL), 0.02),
        "w_in": nrm(ks[8], (L, D_MODEL, D_IN), D_MODEL ** -0.5),
        "sink": nrm(ks[9], (L, A_Q_HEADS), 0.5),
        "w_pa": nrm(ks[10], (L, A_Q_W, D_MODEL), A_Q_W ** -0.5),
        "w_pb": nrm(ks[11], (L, B_OUT_W, D_MODEL), B_OUT_W ** -0.5),
        "w_o": nrm(ks[12], (L, D_MODEL, D_MODEL), D_MODEL ** -0.5),
        "norm2": 1.0 + nrm(ks[13], (L, D_MODEL), 0.02),
        "w_router": nrm(ks[14], (L, D_MODEL, N_EXPERTS), D_MODEL ** -0.5),
        "router_bias": nrm(ks[15], (L, N_EXPERTS), 0.01),
        "w_gate": nrm(ks[16], (L, N_EXPERTS, D_MODEL, D_EXPERT), D_MODEL ** -0.5),
        "w_up": nrm(ks[17], (L, N_EXPERTS, D_MODEL, D_EXPERT), D_MODEL ** -0.5),
        "w_down": nrm(ks[18], (L, N_EXPERTS, D_EXPERT, D_MODEL), D_EXPERT ** -0.5),
        "ws_gate": nrm(ks[19], (L, D_MODEL, D_SHARED), D_MODEL ** -0.5),
        "ws_up": nrm(ks[20], (L, D_MODEL, D_SHARED), D_MODEL ** -0.5),
        "ws_down": nrm(ks[21], (L, D_SHARED, D_MODEL), D_SHARED ** -0.5),
        "final_norm": 1.0 + nrm(ks[22], (D_MODEL,), 0.02),
    }


def reference(x_prompt, x_sample, c_prompt, c_sample, rel_bias, w_ada, b_ada, norm1, w_in, sink,
              w_pa, w_pb, w_o, norm2, w_router, router_bias, w_gate, w_up, w_down,
              ws_gate, ws_up, ws_down, final_norm):
    def trunk(x, c):
        for l in range(DEPTH):
            x = _layer(x, c, rel_bias, w_ada[l], b_ada[l], norm1[l], w_in[l], sink[l], w_pa[l],
                       w_pb[l], w_o[l], norm2[l], w_router[l], router_bias[l], w_gate[l], w_up[l],
                       w_down[l], ws_gate[l], ws_up[l], ws_down[l])
        return _rmsnorm(x, final_norm)

    y_prompt = trunk(x_prompt, c_prompt)
    y_sample = trunk(x_sample, c_sample)
    return (y_prompt, y_sample)
```

```python
import contextlib
import math
import numpy as np
import concourse.bass as bass
import concourse.mybir as mybir
from concourse.bass_utils import run_bass_kernel_spmd

F32 = mybir.dt.float32
BF16 = mybir.dt.bfloat16
AF = mybir.ActivationFunctionType
ALU = mybir.AluOpType
AX = mybir.AxisListType

NT = 8192
D = 1024
CH = 1024
NCH = NT // CH
EPS = 1e-6
NE = 64
MB = 512
NBR = NT * 6 // MB + NE
NBS = NT // MB
NBLK = NBR + NBS
RROWS = NBLK * MB
NCST = 64 + 64 + 1 + NBLK + 16
GROUPS = ((128, 1), (512, 4), (2048, 16))


def _flat(toks):
    for t in toks:
        if t is None:
            continue
        if isinstance(t, (list, tuple)) and not (len(t) == 2 and isinstance(t[1], int)):
            yield from _flat(t)
        else:
            yield t


class Eng:
    def __init__(self, nc, eng, name, es):
        self.eng = eng
        self.name = name
        self.sem = es.enter_context(nc.semaphore("s_" + name))
        self.cnt = 0
        self.seen = {}

    def wait(self, *toks):
        for p, v in _flat(toks):
            if self.seen.get(id(p), 0) >= v:
                continue
            self.seen[id(p)] = v
            self.eng.wait_ge(p.sem, v)

    def sig(self, instr):
        self.cnt += 1
        instr.then_inc(self.sem, 1)
        return (self, self.cnt)


class DSem:
    def __init__(self, nc, name, es):
        self.sem = es.enter_context(nc.semaphore("d_" + name))
        self.cnt = 0

    def sig(self, instr):
        self.cnt += 16
        instr.then_inc(self.sem, 16)
        return (self, self.cnt)


def build(debug=None):
    nc = bass.Bass("TRN2", target_bir_lowering=False)
    es = contextlib.ExitStack()

    def din(name, shape, dt=F32):
        return nc.dram_tensor(name, list(shape), dt, kind="ExternalInput").ap()

    x = din("x", [NT, D])
    cT = din("cT", [128, 8, 4])
    conn = din("conn", [128, 1])
    rel_bias = din("rel_bias", [32, 20])
    sink = din("sink", [1, 8])
    w_ada = din("w_ada", [D, 6 * D])
    b_adaT = din("b_adaT", [128, 48])
    norm1T = din("norm1T", [128, 8])
    norm2T = din("norm2T", [128, 8])
    final_norm = din("final_norm", [1, D])
    w_in = din("w_in", [D, 5120])
    w_pa = din("w_pa", [512, D])
    w_pb = din("w_pb", [256, D])
    w_o = din("w_o", [D, D])
    w_router = din("w_router", [D, NE])
    router_bias = din("router_bias", [1, NE])
    wg2 = din("wg2", [65 * 128, 2048])
    wu2 = din("wu2", [65 * 128, 2048])
    wd2 = din("wd2", [65 * 128, 2048])
    cst_d = din("cst", [128, NCST])
    umat_d = din("umat", [128, 128])
    ident_d = din("ident", [128, 128])
    jmat_d = din("jmat", [128, 128])
    oh_d = din("oh", [32, 4, 512])
    y = nc.dram_tensor("y", [NT, D], F32, kind="ExternalOutput").ap()

    skind = "ExternalOutput" if debug else "Internal"
    PT = nc.dram_tensor("PT", [2176, NT], BF16, kind=skind).ap()
    GT = nc.dram_tensor("GT", [2048, NT], BF16, kind=skind).ap()
    VV = nc.dram_tensor("VV", [NT, 896], BF16, kind=skind).ap()
    X1 = nc.dram_tensor("X1", [NT, D], F32, kind=skind).ap()
    TBL = nc.dram_tensor("TBL", [20, 512], F32, kind="Internal").ap()
    H2 = nc.dram_tensor("H2", [NT + 1, D], BF16, kind="Internal").ap()
    ROWTW = nc.dram_tensor("ROWTW", [RROWS, 2], F32, kind="Internal").ap()
    OUTR = nc.dram_tensor("OUTR", [RROWS, D], BF16, kind="Internal").ap()
    WB = [nc.dram_tensor("WB%d" % i, [65 * 128, 2048], BF16, kind="Internal").ap() for i in range(3)]

    PE = Eng(nc, nc.tensor, "pe", es)
    ACT = Eng(nc, nc.scalar, "act", es)
    DVE = Eng(nc, nc.vector, "dve", es)
    POOL = Eng(nc, nc.gpsimd, "pool", es)
    SP = Eng(nc, nc.sync, "sp", es)
    ENGS = [PE, ACT, DVE, POOL, SP]

    def sb(name, shape, dt, st=None):
        return (st or es).enter_context(nc.sbuf_tensor("sb_" + name, list(shape), dt))

    pst = contextlib.ExitStack()

    def ps(name, st=None):
        return (st or pst).enter_context(nc.psum_tensor(name, [128, 512], F32))

    all_dsems = []

    def dsem(name):
        d = DSem(nc, name, es)
        all_dsems.append(d)
        return d

    def barrier():
        DVE.sig(nc.vector.memset(bar_t[:, 0:1], 0.0))
        ACT.wait(t_ones)
        ACT.sig(nc.scalar.activation(out=bar_t[:, 1:2], in_=ones_f[:, 0:1], func=AF.Copy))
        POOL.sig(nc.gpsimd.memset(bar_t[:, 2:3], 0.0))
        toks = [(e, e.cnt) for e in ENGS if e.cnt > 0] + [(d, d.cnt) for d in all_dsems if d.cnt > 0]
        for e in ENGS:
            e.wait(*[t for t in toks if t[0] is not e])

    bar_t = sb("bar_t", [128, 4], F32)
    ident = sb("ident", [128, 128], F32)
    jmat = sb("jmat", [128, 128], F32)
    ones_f = sb("ones_f", [128, 128], F32)
    ones_b = sb("ones_b", [128, 64], BF16)
    ones_bd = sb("ones_bd", [128, 128], BF16)
    connc = sb("connc", [128, 1], F32)
    modT = sb("modT", [128, 48, 4], F32)
    A1 = sb("A1", [128, 4, 8], F32)
    A2 = sb("A2", [128, 4, 8], F32)
    n1T = sb("n1T", [128, 8], F32)
    n2T = sb("n2T", [128, 8], F32)
    badaT = sb("badaT", [128, 48], F32)
    scT = sb("scT", [128, 8, 4], F32)
    es_bc = sb("es_bc", [128, 8], F32)
    EBA = sb("EBA", [128, 8, 2, 384], BF16)
    EBB = sb("EBB", [128, 6, 2, 192], BF16)
    psA = [ps("psA%d" % i) for i in range(8)]

    d_c = dsem("const")
    toks = []
    for dst, src in ((ident, ident_d), (jmat, jmat_d), (connc, conn), (n1T, norm1T), (n2T, norm2T),
                     (badaT, b_adaT), (scT, cT)):
        toks.append(d_c.sig(nc.sync.dma_start(out=dst[:], in_=src)))
    toks.append(d_c.sig(nc.sync.dma_start(out=es_bc[:], in_=sink[0:1, :].partition_broadcast(128))))
    t_const = toks[-1]
    t_ones = DVE.sig(nc.vector.memset(ones_f[:], 1.0))
    t_onesb = DVE.sig(nc.vector.memset(ones_b[:], 1.0))
    nc.vector.memset(ones_bd[:], 0.0)
    nc.vector.memset(ones_bd[0:64, 0:64], 1.0)
    t_onesb = DVE.sig(nc.vector.memset(ones_bd[64:128, 64:128], 1.0))
    ACT.wait(t_const)
    t_sc = ACT.sig(nc.scalar.activation(out=scT[:], in_=scT[:], func=AF.Silu))
    t_es = ACT.sig(nc.scalar.activation(out=es_bc[:], in_=es_bc[:], func=AF.Exp))

    st0 = contextlib.ExitStack()
    wi = sb("wi", [128, 8, 5120], BF16, st0)
    d_wi = dsem("wi")
    w_in_v = w_in.rearrange("(kt p) n -> p kt n", p=128)
    t_wi = None
    for blk in range(10):
        t_wi = d_wi.sig(nc.gpsimd.dma_start(out=wi[:, :, blk * 512:(blk + 1) * 512], in_=w_in_v[:, :, blk * 512:(blk + 1) * 512]))

    with contextlib.ExitStack() as st:
        wst = [sb("wada%d" % i, [128, 8, 512], F32, st) for i in range(2)]
        d_w = [dsem("wada%d" % i) for i in range(2)]
        w_ada_v = w_ada.rearrange("(kt p) n -> p kt n", p=128)
        rel_t = [None, None]
        PE.wait(t_sc)
        ev_t = [None] * 8
        for blk in range(12):
            s = blk % 2
            SP.wait(rel_t[s])
            tl = d_w[s].sig(nc.sync.dma_start(out=wst[s][:], in_=w_ada_v[:, :, blk * 512:(blk + 1) * 512]))
            PE.wait(tl, ev_t[blk % 8])
            pt = psA[blk % 8]
            for ft in range(4):
                for kt in range(8):
                    mm = nc.tensor.matmul(pt[:, ft * 4:ft * 4 + 4], lhsT=wst[s][:, kt, ft * 128:(ft + 1) * 128],
                                          rhs=scT[:, kt, :], start=(kt == 0), stop=(kt == 7))
            tm = PE.sig(mm)
            rel_t[s] = tm
            DVE.wait(tm, t_const)
            for ft in range(4):
                i_ = nc.vector.tensor_scalar(out=modT[:, blk * 4 + ft, :], in0=pt[:, ft * 4:ft * 4 + 4],
                                             scalar1=badaT[:, blk * 4 + ft:blk * 4 + ft + 1], scalar2=None,
                                             op0=ALU.add)
            ev_t[blk % 8] = DVE.sig(i_)
        t_mod = ev_t[11 % 8]
        DVE.wait(t_mod)
        for s in range(4):
            for (Ax, nT, part) in ((A1, n1T, 1), (A2, n2T, 4)):
                DVE.wait(DVE.sig(nc.vector.tensor_scalar(out=Ax[:, s, :], in0=modT[:, part * 8:(part + 1) * 8, s], scalar1=1.0,
                                        scalar2=None, op0=ALU.add)))
                t_A = DVE.sig(nc.vector.tensor_tensor(out=Ax[:, s, :], in0=Ax[:, s, :], in1=nT[:, :], op=ALU.mult))
                DVE.wait(t_A)
        barrier()

    def row_bcast(dst, part, seg, diag, pss):
        last = None
        for kt in range(8):
            DVE.wait(last)
            td = DVE.sig(nc.vector.tensor_scalar(out=diag[:], in0=ident[:], scalar1=modT[:, part * 8 + kt, seg:seg + 1],
                                                 scalar2=None, op0=ALU.mult))
            PE.wait(td, t_ones)
            last = PE.sig(nc.tensor.matmul(pss[kt // 4][:, (kt % 4) * 128:(kt % 4 + 1) * 128], lhsT=ones_f[:], rhs=diag[:],
                                           start=True, stop=True))
        DVE.wait(last)
        nc.vector.tensor_copy(out=dst[:, 0:512], in_=pss[0][:, :])
        return DVE.sig(nc.vector.tensor_copy(out=dst[:, 512:1024], in_=pss[1][:, :]))

    with contextlib.ExitStack() as st:
        rb = sb("rb", [32, 20], F32, st)
        oh = sb("oh", [32, 4, 512], F32, st)
        tb = sb("tb", [32, 512], F32, st)
        tkr = sb("tkr", [128, 3, 128], F32, st)
        d_t = dsem("tbl")
        t1 = d_t.sig(nc.sync.dma_start(out=rb[:], in_=rel_bias))
        t2 = d_t.sig(nc.sync.dma_start(out=oh[:], in_=oh_d))
        ACT.wait(t2)
        t_rb = ACT.sig(nc.scalar.activation(out=rb[:], in_=rb[:], func=AF.Exp))
        PE.wait(t_rb)
        hs = [(0, 8), (8, 4), (12, 4), (16, 4)]
        for kind, (h0, nh) in enumerate(hs):
            tm = PE.sig(nc.tensor.matmul(psA[kind][0:nh, :], lhsT=rb[:, h0:h0 + nh], rhs=oh[:, kind, :], start=True, stop=True))
            DVE.wait(tm)
            tcp = DVE.sig(nc.vector.tensor_copy(out=tb[0:nh, :], in_=psA[kind][0:nh, :]))
            SP.wait(tcp)
            tst = d_t.sig(nc.sync.dma_start(out=TBL[h0:h0 + nh, :], in_=tb[0:nh, :]))
            DVE.wait(tst)
        SP.wait(tst)
        tprev = None
        for h in range(20):
            Hb = 128 if h < 8 else 64
            Rmax = 2 * Hb - 1
            SP.wait(tprev)
            for di in range(3):
                src = bass.AP(tensor=TBL.tensor, offset=h * 512 + Rmax - Hb * (di - 1) - Hb + 1, ap=[[1, Hb], [1, Hb]])
                tl = d_t.sig(nc.sync.dma_start(out=tkr[0:Hb, di, 0:Hb], in_=src))
            PE.wait(tl)
            pt = psA[h % 8]
            if h < 8:
                tm = PE.sig(nc.tensor.matmul(pt[:, 0:384], lhsT=jmat[:, :], rhs=tkr[:, :, :].rearrange("p a b -> p (a b)"),
                                             start=True, stop=True))
                dst0 = EBA[:, h, 0, :]
                dst1 = EBA[:, h, 1, :]
                src_ps = pt[:, 0:384]
                cc = connc[:, 0:1]
            else:
                hh = h - 8
                pp = (hh % 2) * 64
                tm = PE.sig(nc.tensor.matmul(pt[pp:pp + 64, 0:192].rearrange("p (a b) -> p a b", a=3),
                                             lhsT=jmat[0:64, 64:128] if pp == 0 else jmat[0:64, 64:128],
                                             rhs=tkr[0:64, :, 0:64], start=True, stop=True))
                dst0 = EBB[pp:pp + 64, hh // 2, 0, :]
                dst1 = EBB[pp:pp + 64, hh // 2, 1, :]
                src_ps = pt[pp:pp + 64, 0:192]
                cc = connc[pp:pp + 64, 0:1]
            tprev = tm
            DVE.wait(tm)
            nc.vector.tensor_copy(out=dst0, in_=src_ps)
            t_eb = DVE.sig(nc.vector.tensor_scalar(out=dst1, in0=src_ps, scalar1=cc, scalar2=None, op0=ALU.mult))
        barrier()

    pt_cols = [i * 128 for i in range(4)] + [512] + [768 + i * 128 for i in range(6)] + [1536 + i * 128 for i in range(6)]
    x_v = x.rearrange("(t p) n -> t p n", p=128)
    X1_v = X1.rearrange("(t p) n -> t p n", p=128)
    y_v = y.rearrange("(t p) n -> t p n", p=128)

    def norm_transpose(st_name, src_v, tile_idx, xin, xs, ssq, rstd, junk, tok_xin_free, d_x, psT, psT_free, hT, hcol, Ax, seg,
                       Bpart, hT_free):
        raise NotImplementedError

    with contextlib.ExitStack() as st:
        xin = [sb("xin%d" % i, [128, D], F32, st) for i in range(2)]
        xs = [sb("xs%d" % i, [128, D], F32, st) for i in range(2)]
        junk = sb("junk", [128, D], F32, st)
        ssq = sb("ssq", [128, 16], F32, st)
        hT = sb("hT", [128, 8, CH], BF16, st)
        stQ = [sb("stQ%d" % i, [128, 17, 512], BF16, st) for i in range(1)]
        stG = [sb("stG%d" % i, [128, 16, 512], BF16, st) for i in range(1)]
        stV = sb("stV", [128, 8, 896], BF16, st)
        d_x = [dsem("x%d" % i) for i in range(2)]
        d_st = dsem("st")
        cvb = [sb("cvb%d" % i, [128, 3, 2048], BF16, st) for i in range(2)]
        d_cl = [dsem("cvl%d" % i) for i in range(2)]
        d_cs = [dsem("cvs%d" % i) for i in range(2)]
        cv_state = {"ld": [None, None], "st": [None, None], "n": 0}
        wsrc = (wg2, wu2, wd2)

        def cv_load(e):
            sl = e % 2
            POOL.wait(cv_state["st"][sl])
            for m_ in range(3):
                t_ = d_cl[sl].sig(nc.gpsimd.dma_start(out=cvb[sl][:, m_, :], in_=wsrc[m_][e * 128:(e + 1) * 128, :]))
            cv_state["ld"][sl] = t_

        def cv_store(e):
            sl = e % 2
            POOL.wait(cv_state["ld"][sl])
            for m_ in range(3):
                t_ = d_cs[sl].sig(nc.gpsimd.dma_start(out=WB[m_][e * 128:(e + 1) * 128, :], in_=cvb[sl][:, m_, :]))
            cv_state["st"][sl] = t_

        def convert_some(k):
            for _ in range(k):
                e = cv_state["n"]
                if e >= 65:
                    return
                if e == 0:
                    cv_load(0)
                if e + 1 < 65:
                    cv_load(e + 1)
                cv_store(e)
                cv_state["n"] += 1

        xin_free = [None, None]
        xs_free = [None, None]
        psT_free = [None] * 4
        pp_free = [None] * 2
        pv_free = None
        hT_free = None
        stq_free = None
        stg_free = None
        stv_free = None
        cnt_tile = 0
        ev_flip = 0
        for c in range(NCH):
            seg = c // 2
            t_h = None
            convert_some(9)
            for tt in range(8):
                gt = c * 8 + tt
                b = gt % 2
                SP.wait(xin_free[b])
                tl = d_x[b].sig(nc.sync.dma_start(out=xin[b][:], in_=x_v[gt]))
                ACT.wait(tl)
                col = gt % 16
                ta = ACT.sig(nc.scalar.activation(out=junk[:], in_=xin[b][:], func=AF.Square, accum_out=ssq[:, col:col + 1]))
                ACT.wait(ta)
                ta = ACT.sig(nc.scalar.activation(out=ssq[:, col:col + 1], in_=ssq[:, col:col + 1], func=AF.Sqrt, scale=1.0 / D, bias=EPS))
                DVE.wait(ta, xs_free[b])
                td = DVE.sig(nc.vector.reciprocal(out=ssq[:, col:col + 1], in_=ssq[:, col:col + 1]))
                DVE.wait(td)
                td = DVE.sig(nc.vector.tensor_scalar(out=xs[b][:], in0=xin[b][:], scalar1=ssq[:, col:col + 1], scalar2=None, op0=ALU.mult))
                xin_free[b] = td
                PE.wait(td, psT_free[2 * b], psT_free[2 * b + 1])
                for kt in range(8):
                    tp = nc.tensor.transpose(psA[2 * b + kt // 4][:, (kt % 4) * 128:(kt % 4 + 1) * 128], xs[b][:, kt * 128:(kt + 1) * 128], ident[:])
                tp = PE.sig(tp)
                xs_free[b] = tp
                ACT.wait(tp, hT_free if tt == 0 else None)
                for kt in range(8):
                    ta = nc.scalar.activation(out=hT[:, kt, tt * 128:(tt + 1) * 128], in_=psA[2 * b + kt // 4][:, (kt % 4) * 128:(kt % 4 + 1) * 128],
                                              func=AF.Identity, scale=A1[:, seg, kt:kt + 1], bias=modT[:, kt, seg:seg + 1])
                ta = ACT.sig(ta)
                psT_free[2 * b] = ta
                psT_free[2 * b + 1] = ta
                t_h = ta
            PE.wait(t_h, t_wi)
            last_pe_read_h = None
            for half in range(2):
                tsl = slice(half * 512, (half + 1) * 512)
                gsl = slice(c * CH + half * 512, c * CH + (half + 1) * 512)
                evs = []
                for ti in range(33):
                    col0 = pt_cols[ti] if ti < 17 else 3072 + (ti - 17) * 128
                    pb_ = ti % 2
                    PE.wait(pp_free[pb_])
                    for kt in range(8):
                        mm = nc.tensor.matmul(psA[4 + pb_][:, :], lhsT=wi[:, kt, col0:col0 + 128], rhs=hT[:, kt, tsl], start=(kt == 0), stop=(kt == 7))
                    tm = PE.sig(mm)
                    last_pe_read_h = tm
                    if ti < 17:
                        if ti == 0:
                            DVE.wait(stq_free)
                        DVE.wait(tm)
                        te = DVE.sig(nc.vector.tensor_copy(out=stQ[0][:, ti, :], in_=psA[4 + pb_][:, :]))
                    else:
                        if ti == 17:
                            ACT.wait(stg_free)
                        ACT.wait(tm)
                        te = ACT.sig(nc.scalar.activation(out=stG[0][:, ti - 17, :], in_=psA[4 + pb_][:, :], func=AF.Sigmoid))
                    pp_free[pb_] = te
                    evs.append(te)
                SP.wait(evs[16])
                stq_free = d_st.sig(nc.sync.dma_start(out=PT.rearrange("(t p) n -> p t n", p=128)[:, :, gsl], in_=stQ[0][:, :, :]))
                SP.wait(evs[32])
                stg_free = d_st.sig(nc.sync.dma_start(out=GT.rearrange("(t p) n -> p t n", p=128)[:, :, gsl], in_=stG[0][:, :, :]))
            DVE.wait(stv_free)
            for i in range(8):
                PE.wait(pv_free)
                jobs = [(640, 128, psA[6][:, 0:128], lambda hk: hk[:, i * 128:(i + 1) * 128]),
                        (2304, 256, psA[6][:, 128:384], lambda hk: hk[:, i * 128:(i + 1) * 128]),
                        (2560, 256, psA[7][:, 0:256],
                         lambda hk: hk.rearrange("p (u r) -> p r u", r=4)[:, i // 2, (i % 2) * 128:(i % 2 + 1) * 128]),
                        (2816, 256, psA[7][0:64, 256:512], lambda hk: hk.rearrange("p (u r) -> p r u", r=16)[:, 2 * i, :]),
                        (2816, 256, psA[7][64:128, 256:512], lambda hk: hk.rearrange("p (u r) -> p r u", r=16)[:, 2 * i + 1, :])]
                for (c0, wd, o_, lf) in jobs:
                    for kt in range(8):
                        mm = nc.tensor.matmul(o_, lhsT=lf(hT[:, kt, :]), rhs=wi[:, kt, c0:c0 + wd], start=(kt == 0), stop=(kt == 7))
                tm = PE.sig(mm)
                last_pe_read_h = tm
                DVE.wait(tm)
                nc.vector.tensor_copy(out=stV[:, i, 0:384], in_=psA[6][:, 0:384])
                te = DVE.sig(nc.vector.tensor_copy(out=stV[:, i, 384:896], in_=psA[7][:, :]))
                pv_free = te
            SP.wait(te)
            stv_free = d_st.sig(nc.sync.dma_start(out=VV[c * CH:(c + 1) * CH, :].rearrange("(i p) f -> p i f", p=128), in_=stV[:, :, :]))
            hT_free = last_pe_read_h
        barrier()
    st0.close()

    if debug == "proj":
        es.close()
        return nc

    ps_free = [None] * 8

    def bc_last(a, n):
        return bass.AP(tensor=a.tensor, offset=a.offset, ap=[list(a.ap[0]), list(a.ap[1]), [0, n]])

    with contextlib.ExitStack() as st:
        wpa = sb("wpa", [128, 4, D], BF16, st)
        wpb = sb("wpb", [128, 2, D], BF16, st)
        wo = sb("wo", [128, 8, D], BF16, st)
        d_w2 = dsem("w2")
        nc.gpsimd.dma_start(out=wpa[:], in_=w_pa.rearrange("(kt p) n -> p kt n", p=128)).then_inc(d_w2.sem, 16)
        nc.gpsimd.dma_start(out=wpb[:], in_=w_pb.rearrange("(kt p) n -> p kt n", p=128)).then_inc(d_w2.sem, 16)
        d_w2.cnt = 32
        t_w2 = d_w2.sig(nc.gpsimd.dma_start(out=wo[:], in_=w_o.rearrange("(kt p) n -> p kt n", p=128)))
        QA = sb("QA", [64, 8, CH], BF16, st)
        QB = sb("QB", [128, 6, CH], BF16, st)
        KAw = sb("KAw", [64, 2, 1280], BF16, st)
        KBw = [sb("KBw%d" % g, [128, 2, CH + 128 * d], BF16, st) for g, (w_, d) in enumerate(GROUPS)]
        VAw = sb("VAw", [128, 10, 128], BF16, st)
        VBw = [sb("VBw%d" % g, [128, n_, 256], BF16, st) for g, n_ in enumerate((18, 24, 48))]
        yaT = sb("yaT", [128, 4, CH], BF16, st)
        ybT = sb("ybT", [128, 2, CH], BF16, st)
        accN = sb("accN", [128, CH], F32, st)
        accD = sb("accD", [128, CH], F32, st)
        P0 = [sb("P0_%d" % i, [128, 384], BF16, st) for i in range(2)]
        Pt = [sb("Pt_%d" % i, [128, 384], BF16, st) for i in range(4)]
        rd = sb("rd", [128, 128], F32, st)
        Gt = sb("Gt", [128, 16, 256], BF16, st)
        mg = sb("mg", [128, 8, 256], BF16, st)
        t12 = sb("t12", [128, 2, 512], F32, st)
        G1bc = sb("G1bc", [128, D], F32, st)
        diag = sb("diag", [128, 128], F32, st)
        xio = sb("xio", [128, D], F32, st)
        d_ld = dsem("attld")
        d_g = dsem("gld")
        d_xi = dsem("xi")
        d_xo = dsem("xo")
        PTq = PT[0:512, :].rearrange("(h p) n -> p h n", p=64)
        PTka = PT[512:640, :].rearrange("(h p) n -> p h n", p=64)
        PTqb = PT[640:1408, :].rearrange("(t p) n -> p t n", p=128)
        GTv = GT.rearrange("(t p) n -> p t n", p=128)
        att_free = None
        y_free = None
        g_free = None
        mg_free = None
        xio_free = None
        t_g1 = None
        P0_free = [None, None]
        Pt_free = [None] * 4
        rd_free = None
        t12_free = [None, None]
        for c in range(NCH):
            seg = c // 2
            if c % 2 == 0:
                PE.wait(ps_free[6], ps_free[7])
                DVE.wait(xio_free)
                t_g1 = row_bcast(G1bc, 2, seg, diag, [psA[6], psA[7]])
                ps_free[6] = t_g1
                ps_free[7] = t_g1
            SP.wait(att_free)
            lt = []
            c0 = c * CH
            lt.append(d_ld.sig(nc.sync.dma_start(out=QA[:, :, :], in_=PTq[:, :, c0:c0 + CH])))
            lt.append(d_ld.sig(nc.sync.dma_start(out=QB[:, :, :], in_=PTqb[:, :, c0:c0 + CH])))
            lo, hi = max(0, c0 - 128), min(NT, c0 + CH + 128)
            lt.append(d_ld.sig(nc.sync.dma_start(out=KAw[:, :, lo - (c0 - 128):hi - (c0 - 128)], in_=PTka[:, :, lo:hi])))
            for g, (w_, d) in enumerate(GROUPS):
                H = 64 * d
                lo, hi = max(0, c0 - H), min(NT, c0 + CH + H)
                src = PT[1408 + 256 * g:1408 + 256 * (g + 1), :].rearrange("(t p) n -> p t n", p=128)
                lt.append(d_ld.sig(nc.sync.dma_start(out=KBw[g][:, :, lo - (c0 - H):hi - (c0 - H)], in_=src[:, :, lo:hi])))
            lo, hi = max(0, c0 - 128), min(NT, c0 + CH + 128)
            b0, b1 = (lo - (c0 - 128)) // 128, (hi - (c0 - 128)) // 128
            lt.append(d_ld.sig(nc.sync.dma_start(out=VAw[:, b0:b1, :], in_=VV[lo:hi, 0:128].rearrange("(b p) f -> p b f", p=128))))
            for hp in (0, 64):
                lo, hi = max(0, c0 - 64), min(NT, c0 + CH + 64)
                b0, b1 = (lo - (c0 - 64)) // 64, (hi - (c0 - 64)) // 64
                lt.append(d_ld.sig(nc.sync.dma_start(out=VBw[0][hp:hp + 64, b0:b1, :], in_=VV[lo:hi, 128:384].rearrange("(b p) f -> p b f", p=64))))
                lt.append(d_ld.sig(nc.sync.dma_start(out=VBw[1][hp:hp + 64, 0:16, :], in_=VV[c0:c0 + CH, 384:640].rearrange("(b p) f -> p b f", p=64))))
                if c > 0:
                    lt.append(d_ld.sig(nc.sync.dma_start(out=VBw[1][hp:hp + 64, 16:20, :],
                                                         in_=VV[c0 - CH:c0, 384:640].rearrange("(r b p) f -> p r b f", r=4, b=4, p=64)[:, :, 3, :])))
                if c < NCH - 1:
                    lt.append(d_ld.sig(nc.sync.dma_start(out=VBw[1][hp:hp + 64, 20:24, :],
                                                         in_=VV[c0 + CH:c0 + 2 * CH, 384:640].rearrange("(r b p) f -> p r b f", r=4, b=4, p=64)[:, :, 0, :])))
                for dc in (-1, 0, 1):
                    if 0 <= c + dc < NCH:
                        lt.append(d_ld.sig(nc.sync.dma_start(out=VBw[2][hp:hp + 64, 16 * (dc + 1):16 * (dc + 2), :],
                                                             in_=VV[c0 + dc * CH:c0 + (dc + 1) * CH, 640:896].rearrange("(r p) f -> p r f", p=64))))
            t_ld = lt[-1]
            PE.wait(t_ld, t_w2)

            items = []
            for h in range(8):
                kv = h // 4
                for i in range(8):
                    gb = 8 * c + i
                    dl = [dd for dd in (-1, 0, 1) if 0 <= gb + dd < 64]
                    items.append(dict(kind="A", h=h, kv=kv, i=i, dl=dl, cross=[(gb + dd) // 16 != gb // 16 for dd in dl], Hb=128))
            for p in range(2):
                for g, (w_, d) in enumerate(GROUPS):
                    nb = 16 // d
                    for r in range(d):
                        for b in range(nb):
                            ub = nb * c + b
                            dl = [dd for dd in (-1, 0, 1) if 0 <= ub + dd < 8 * nb]
                            items.append(dict(kind="B", p=p, g=g, d=d, nb=nb, r=r, b=b, dl=dl,
                                              cross=[(ub + dd) // (2 * nb) != ub // (2 * nb) for dd in dl], Hb=64,
                                              first=(r == 0 and b == 0), last_of_pair=(g == 2 and r == d - 1 and b == nb - 1)))
            N = len(items)
            acc_tok = None

            def vidx(it, dd):
                g, nb, b, r = it["g"], it["nb"], it["b"], it["r"]
                ub = nb * c + b + dd
                dc = ub // nb - c
                bp = ub % nb
                if g == 0:
                    return ub - 16 * c + 1
                if g == 1:
                    return 4 * r + bp if dc == 0 else (16 + r if dc < 0 else 20 + r)
                return 16 * (dc + 1) + r

            def stage1(n):
                it = items[n]
                Hb = it["Hb"]
                S = psA[n % 3]
                PE.wait(ps_free[n % 3])
                for dd in it["dl"]:
                    cs = slice((dd + 1) * Hb, (dd + 2) * Hb)
                    if it["kind"] == "A":
                        i = it["i"]
                        mm = nc.tensor.matmul(S[:, cs], lhsT=KAw[0:64, it["kv"], (i + 1 + dd) * 128:(i + 2 + dd) * 128],
                                              rhs=QA[0:64, it["h"], i * 128:(i + 1) * 128], start=True, stop=True)
                    else:
                        d, g, b, r, p = it["d"], it["g"], it["b"], it["r"], it["p"]
                        qoff = d * 64 * b + r
                        koff = d * 64 * (b + dd) + r + 64 * d
                        for hp in (0, 64):
                            mm = nc.tensor.matmul(S[hp:hp + 64, cs], lhsT=KBw[g][hp:hp + 64, p, koff:koff + 63 * d + 1:d],
                                                  rhs=QB[hp:hp + 64, 2 * g + p, qoff:qoff + 63 * d + 1:d], start=True, stop=True)
                tm = PE.sig(mm)
                lo, hi = (it["dl"][0] + 1) * Hb, (it["dl"][-1] + 2) * Hb
                ACT.wait(tm, P0_free[n % 2])
                ta = ACT.sig(nc.scalar.activation(out=P0[n % 2][:, lo:hi], in_=S[:, lo:hi], func=AF.Exp, scale=0.125))
                ps_free[n % 3] = ta
                POOL.wait(ta, Pt_free[n % 4])
                if it["kind"] == "A":
                    eb = lambda v, a, b_: EBA[:, it["h"], v, a:b_]
                else:
                    eb = lambda v, a, b_: EBB[:, 2 * it["g"] + it["p"], v, a:b_]
                if not any(it["cross"]):
                    td = nc.gpsimd.tensor_tensor(out=Pt[n % 4][:, lo:hi], in0=P0[n % 2][:, lo:hi], in1=eb(0, lo, hi), op=ALU.mult)
                else:
                    for dd, cr in zip(it["dl"], it["cross"]):
                        a, b_ = (dd + 1) * Hb, (dd + 2) * Hb
                        td = nc.gpsimd.tensor_tensor(out=Pt[n % 4][:, a:b_], in0=P0[n % 2][:, a:b_], in1=eb(1 if cr else 0, a, b_), op=ALU.mult)
                td = POOL.sig(td)
                P0_free[n % 2] = td
                it["tP"] = td

            def stage2(n):
                nonlocal att_free, rd_free, acc_tok, y_free
                it = items[n]
                Hb = it["Hb"]
                O = psA[3 + n % 2]
                PE.wait(it["tP"], ps_free[3 + n % 2])
                nd = len(it["dl"])
                if it["kind"] == "A":
                    pb_ = (it["h"] % 2) * 64
                    i = it["i"]
                    for k_, dd in enumerate(it["dl"]):
                        nc.tensor.matmul(O[pb_:pb_ + 64, 0:128], lhsT=VAw[:, i + 1 + dd, it["kv"] * 64:(it["kv"] + 1) * 64],
                                         rhs=Pt[n % 4][:, (dd + 1) * 128:(dd + 2) * 128], start=(k_ == 0), stop=(k_ == nd - 1))
                    for k_, dd in enumerate(it["dl"]):
                        mm = nc.tensor.matmul(O[pb_:pb_ + 64, 128:256], lhsT=ones_b[:, 0:64],
                                              rhs=Pt[n % 4][:, (dd + 1) * 128:(dd + 2) * 128], start=(k_ == 0), stop=(k_ == nd - 1))
                    tm = PE.sig(mm)
                    att_free = tm
                    Pt_free[n % 4] = tm
                    DVE.wait(tm, t_es, rd_free, y_free)
                    h = it["h"]
                    DVE.wait(DVE.sig(nc.vector.tensor_scalar(out=rd[pb_:pb_ + 64, :], in0=O[pb_:pb_ + 64, 128:256],
                                                             scalar1=es_bc[pb_:pb_ + 64, h:h + 1], scalar2=None, op0=ALU.add)))
                    DVE.wait(DVE.sig(nc.vector.reciprocal(out=rd[pb_:pb_ + 64, :], in_=rd[pb_:pb_ + 64, :])))
                    td = DVE.sig(nc.vector.tensor_tensor(out=yaT[pb_:pb_ + 64, h // 2, i * 128:(i + 1) * 128], in0=O[pb_:pb_ + 64, 0:128],
                                                         in1=rd[pb_:pb_ + 64, :], op=ALU.mult))
                    rd_free = td
                    ps_free[3 + n % 2] = td
                else:
                    d, g, b, r, p = it["d"], it["g"], it["b"], it["r"], it["p"]
                    for hp in (0, 64):
                        for k_, dd in enumerate(it["dl"]):
                            nc.tensor.matmul(O[hp:hp + 64, 0:64], lhsT=VBw[g][hp:hp + 64, vidx(it, dd), (2 * p + hp // 64) * 64:(2 * p + hp // 64 + 1) * 64],
                                             rhs=Pt[n % 4][hp:hp + 64, (dd + 1) * 64:(dd + 2) * 64], start=(k_ == 0), stop=(k_ == nd - 1))
                    for k_, dd in enumerate(it["dl"]):
                        mm = nc.tensor.matmul(O[:, 64:128], lhsT=ones_bd[:, :],
                                              rhs=Pt[n % 4][:, (dd + 1) * 64:(dd + 2) * 64], start=(k_ == 0), stop=(k_ == nd - 1))
                    tm = PE.sig(mm)
                    att_free = tm
                    Pt_free[n % 4] = tm
                    qoff = d * 64 * b + r
                    sl = slice(qoff, qoff + 63 * d + 1, d)
                    DVE.wait(tm)
                    if it["first"]:
                        DVE.wait(acc_tok, y_free)
                    if g == 0:
                        nc.vector.tensor_copy(out=accN[:, sl], in_=O[:, 0:64])
                        td = DVE.sig(nc.vector.tensor_copy(out=accD[:, sl], in_=O[:, 64:128]))
                    else:
                        nc.vector.tensor_tensor(out=accN[:, sl], in0=accN[:, sl], in1=O[:, 0:64], op=ALU.add)
                        td = DVE.sig(nc.vector.tensor_tensor(out=accD[:, sl], in0=accD[:, sl], in1=O[:, 64:128], op=ALU.add))
                    acc_tok = td
                    ps_free[3 + n % 2] = td
                    if it["last_of_pair"]:
                        DVE.wait(td)
                        DVE.wait(DVE.sig(nc.vector.reciprocal(out=accD[:, :], in_=accD[:, :])))
                        acc_tok = DVE.sig(nc.vector.tensor_tensor(out=ybT[:, p, :], in0=accN[:, :], in1=accD[:, :], op=ALU.mult))
                        DVE.wait(acc_tok)

            LAG = 3
            for n in range(N + LAG):
                if n < N:
                    stage1(n)
                if n >= LAG:
                    stage2(n - LAG)
            t_y = acc_tok

            for m in range(4):
                msl = slice(m * 256, (m + 1) * 256)
                SP.wait(g_free)
                tg = d_g.sig(nc.sync.dma_start(out=Gt[:, :, :], in_=GTv[:, :, c0 + m * 256:c0 + (m + 1) * 256]))
                PE.wait(t_y)
                for nt in range(8):
                    pm = psA[5] if nt % 2 == 0 else psA[2]
                    pi = 5 if nt % 2 == 0 else 2
                    PE.wait(ps_free[pi])
                    for kt in range(4):
                        nc.tensor.matmul(pm[:, 0:256], lhsT=wpa[:, kt, nt * 128:(nt + 1) * 128], rhs=yaT[:, kt, msl], start=(kt == 0), stop=(kt == 3))
                    for kt in range(2):
                        mm = nc.tensor.matmul(pm[:, 256:512], lhsT=wpb[:, kt, nt * 128:(nt + 1) * 128], rhs=ybT[:, kt, msl], start=(kt == 0), stop=(kt == 1))
                    tm = PE.sig(mm)
                    y_free = tm
                    DVE.wait(tm, tg, t12_free[nt % 2], mg_free if nt == 0 else None)
                    tq = t12[:, nt % 2, :]
                    nc.vector.tensor_tensor(out=tq[:, 0:256], in0=pm[:, 0:256], in1=Gt[:, nt, :], op=ALU.mult)
                    td = DVE.sig(nc.vector.tensor_tensor(out=tq[:, 256:512], in0=pm[:, 256:512], in1=Gt[:, 8 + nt, :], op=ALU.mult))
                    ps_free[pi] = td
                    DVE.wait(td)
                    td = DVE.sig(nc.vector.tensor_tensor(out=mg[:, nt, :], in0=tq[:, 0:256], in1=tq[:, 256:512], op=ALU.add))
                    t12_free[nt % 2] = td
                g_free = td
                t_mg = td
                for tt in range(2):
                    gt = c * 8 + m * 2 + tt
                    SP.wait(xio_free)
                    tx = d_xi.sig(nc.sync.dma_start(out=xio[:], in_=x_v[gt]))
                    PE.wait(t_mg, ps_free[6], ps_free[7])
                    for nh in range(2):
                        for kt in range(8):
                            mm = nc.tensor.matmul(psA[6 + nh][:, :], lhsT=mg[:, kt, tt * 128:(tt + 1) * 128], rhs=wo[:, kt, nh * 512:(nh + 1) * 512],
                                                  start=(kt == 0), stop=(kt == 7))
                    tm = PE.sig(mm)
                    mg_free = tm
                    DVE.wait(tm, tx, t_g1, t12_free[0], t12_free[1])
                    for nh in range(2):
                        hs_ = slice(nh * 512, (nh + 1) * 512)
                        DVE.wait(DVE.sig(nc.vector.tensor_tensor(out=t12[:, nh, :], in0=psA[6 + nh][:, :], in1=G1bc[:, hs_], op=ALU.mult)))
                        td = DVE.sig(nc.vector.tensor_tensor(out=xio[:, hs_], in0=xio[:, hs_], in1=t12[:, nh, :], op=ALU.add))
                    ps_free[6] = td
                    ps_free[7] = td
                    t12_free[0] = td
                    t12_free[1] = td
                    SP.wait(td)
                    xio_free = d_xo.sig(nc.sync.dma_start(out=X1_v[gt], in_=xio[:]))
        barrier()

    if debug == "att":
        es.close()
        return nc

    pst.close()
    ps_free = [None] * 8
    U32 = mybir.dt.uint32
    I32 = mybir.dt.int32

    def bc_mid(a, n):
        return bass.AP(tensor=a.tensor, offset=a.offset, ap=[list(a.ap[0]), [0, n], list(a.ap[1])])

    def row_bcast2(dst, colfn, diag, pss):
        last = None
        for kt in range(8):
            DVE.wait(last)
            td = DVE.sig(nc.vector.tensor_scalar(out=diag[:], in0=ident[:], scalar1=colfn(kt), scalar2=None, op0=ALU.mult))
            PE.wait(td, t_ones)
            last = PE.sig(nc.tensor.matmul(pss[kt // 4][:, (kt % 4) * 128:(kt % 4 + 1) * 128], lhsT=ones_f[:], rhs=diag[:],
                                           start=True, stop=True))
        DVE.wait(last)
        nc.vector.tensor_copy(out=dst[:, 0:512], in_=pss[0][:, :])
        return DVE.sig(nc.vector.tensor_copy(out=dst[:, 512:1024], in_=pss[1][:, :]))

    with contextlib.ExitStack() as st:
        psF = [st.enter_context(nc.psum_tensor("psF%d" % i, [128, 512], F32)) for i in range(6)]
        psB = [st.enter_context(nc.psum_tensor("psB%d" % i, [128, 1024], BF16)) for i in range(2)]
        cst = sb("cst", [128, NCST], F32, st)
        iota_e = cst[:, 0:64]
        tokid = cst[:, 64:128]
        pcol = cst[:, 128:129]
        iota_b = cst[:, 129:129 + NBLK]
        thr16 = cst[:, 129 + NBLK:129 + NBLK + 16]
        U_b = sb("U_b", [128, 128], BF16, st)
        ones128 = sb("ones128", [128, 128], BF16, st)
        id_b = sb("id_b", [128, 128], BF16, st)
        wr = sb("wr", [128, 8, NE], BF16, st)
        rb_bc = sb("rb_bc", [128, NE], F32, st)
        FNbc = sb("FNbc", [128, D], F32, st)
        G2all = sb("G2all", [128, 4, D], F32, st)
        diag = sb("diag2", [128, 128], F32, st)
        RK = sb("RK", [128, 64, 64], F32, st)
        E6f = sb("E6f", [128, 64, 8], F32, st)
        W6 = sb("W6", [128, 64, 8], F32, st)
        D6f = sb("D6f", [128, 64, 8], F32, st)
        D6i = sb("D6i", [128, 64, 8], I32, st)
        IDXW = sb("IDXW", [128, NBLK], I32, st)
        carry = sb("carry", [128, 64], F32, st)
        xin = [sb("mxin%d" % i, [128, D], F32, st) for i in range(2)]
        xs = [sb("mxs%d" % i, [128, D], F32, st) for i in range(2)]
        junk = sb("mjunk", [128, D], F32, st)
        ssq = sb("mssq", [128, 64], F32, st)
        d_m = dsem("mconst")
        nc.sync.dma_start(out=cst[:], in_=cst_d).then_inc(d_m.sem, 16)
        nc.sync.dma_start(out=rb_bc[:], in_=router_bias[0:1, :].partition_broadcast(128)).then_inc(d_m.sem, 16)
        nc.sync.dma_start(out=FNbc[:], in_=final_norm[0:1, :].partition_broadcast(128)).then_inc(d_m.sem, 16)
        nc.gpsimd.dma_start(out=U_b[:], in_=umat_d).then_inc(d_m.sem, 16)
        nc.gpsimd.dma_start(out=id_b[:], in_=ident_d).then_inc(d_m.sem, 16)
        d_m.cnt += 80
        t_mc = d_m.sig(nc.gpsimd.dma_start(out=wr[:], in_=w_router.rearrange("(kt p) n -> p kt n", p=128)))
        t_o128 = DVE.sig(nc.vector.memset(ones128[:], 1.0))
        t_c0 = DVE.sig(nc.vector.memset(carry[:], 0.0))
        for s_ in range(4):
            PE.wait(ps_free[4], ps_free[5])
            tg_ = row_bcast2(G2all[:, s_, :], lambda kt: modT[:, 40 + kt, s_:s_ + 1], diag, [psF[4], psF[5]])
            ps_free[4] = tg_
            ps_free[5] = tg_
        t_g2 = tg_
        d_x = [dsem("mx%d" % i) for i in range(2)]
        d_h2 = [dsem("mh%d" % i) for i in range(2)]
        xin_free = [None, None]
        xs_free = [None, None]

        with contextlib.ExitStack() as st1:
            A2bc = sb("A2bc", [128, D], F32, st1)
            B2bc = sb("B2bc", [128, D], F32, st1)
            tmpf = sb("tmpf", [128, D], F32, st1)
            h2tm = [sb("h2tm%d" % i, [128, D], BF16, st1) for i in range(2)]
            h2T = [sb("h2T%d" % i, [128, 8, 128], BF16, st1) for i in range(2)]
            rt = sb("rt", [128, 8, 64], F32, st1)
            r8 = sb("r8", [128, 8, 8], F32, st1)
            mk = sb("mk", [128, 64], BF16, st1)
            i8 = sb("i8", [128, 8], U32, st1)
            h2tm_free = [None, None]
            h2T_free = [None, None]
            t_ab = None
            t_h = None
            mk_free = None
            ch = lambda ins: DVE.wait(DVE.sig(ins))
            for gt in range(64):
                seg = gt // 16
                b = gt % 2
                if gt % 16 == 0:
                    PE.wait(ps_free[4], ps_free[5])
                    DVE.wait(t_h)
                    ta_ = row_bcast2(A2bc, lambda kt: A2[:, seg, kt:kt + 1], diag, [psF[4], psF[5]])
                    PE.wait(ta_)
                    t_ab = row_bcast2(B2bc, lambda kt: modT[:, 24 + kt, seg:seg + 1], diag, [psF[4], psF[5]])
                    ps_free[4] = t_ab
                    ps_free[5] = t_ab
                SP.wait(xin_free[b])
                tl = d_x[b].sig(nc.sync.dma_start(out=xin[b][:], in_=X1_v[gt]))
                ACT.wait(tl)
                col = gt
                ta = ACT.sig(nc.scalar.activation(out=junk[:], in_=xin[b][:], func=AF.Square, accum_out=ssq[:, col:col + 1]))
                ACT.wait(ta)
                ta = ACT.sig(nc.scalar.activation(out=ssq[:, col:col + 1], in_=ssq[:, col:col + 1], func=AF.Sqrt, scale=1.0 / D, bias=EPS))
                DVE.wait(ta, xs_free[b])
                ch(nc.vector.reciprocal(out=ssq[:, col:col + 1], in_=ssq[:, col:col + 1]))
                td = DVE.sig(nc.vector.tensor_scalar(out=xs[b][:], in0=xin[b][:], scalar1=ssq[:, col:col + 1], scalar2=None, op0=ALU.mult))
                xin_free[b] = td
                DVE.wait(td, t_ab, h2tm_free[b])
                ch(nc.vector.tensor_tensor(out=tmpf[:], in0=xs[b][:], in1=A2bc[:], op=ALU.mult))
                th = DVE.sig(nc.vector.tensor_tensor(out=h2tm[b][:], in0=tmpf[:], in1=B2bc[:], op=ALU.add))
                SP.wait(th)
                h2tm_free[b] = d_h2[b].sig(nc.sync.dma_start(out=H2[gt * 128:(gt + 1) * 128, :], in_=h2tm[b][:]))
                PE.wait(td, ps_free[2 * b], ps_free[2 * b + 1])
                for kt in range(8):
                    tp = nc.tensor.transpose(psF[2 * b + kt // 4][:, (kt % 4) * 128:(kt % 4 + 1) * 128], xs[b][:, kt * 128:(kt + 1) * 128], ident[:])
                tp = PE.sig(tp)
                xs_free[b] = [tp, th]
                ACT.wait(tp, h2T_free[b])
                for kt in range(8):
                    ta = nc.scalar.activation(out=h2T[b][:, kt, :], in_=psF[2 * b + kt // 4][:, (kt % 4) * 128:(kt % 4 + 1) * 128],
                                              func=AF.Identity, scale=A2[:, seg, kt:kt + 1], bias=modT[:, 24 + kt, seg:seg + 1])
                ta = ACT.sig(ta)
                ps_free[2 * b] = ta
                ps_free[2 * b + 1] = ta
                PE.wait(ta, t_mc, ps_free[4])
                for kt in range(8):
                    mm = nc.tensor.matmul(psF[4][:, 0:64], lhsT=h2T[b][:, kt, :], rhs=wr[:, kt, :], start=(kt == 0), stop=(kt == 7))
                tm = PE.sig(mm)
                h2T_free[b] = tm
                ACT.wait(tm)
                sc = rt[:, 0, :]
                sel = rt[:, 1, :]
                eq = rt[:, 2, :]
                sel2 = rt[:, 3, :]
                selm = rt[:, 4, :]
                em = rt[:, 5, :]
                wdn = rt[:, 6, :]
                dd = rt[:, 7, :]
                v3 = lambda a: a.rearrange("p (g e) -> p g e", g=8)
                DVE.wait(t_h)
                ta2 = ACT.sig(nc.scalar.activation(out=sc, in_=psF[4][:, 0:64], func=AF.Sigmoid))
                ps_free[4] = ta2
                DVE.wait(ta2, t_mc)
                ch(nc.vector.tensor_tensor(out=sel, in0=sc, in1=rb_bc[:, :], op=ALU.add))
                ch(nc.vector.tensor_reduce(out=r8[:, 0, :], in_=v3(sel), axis=AX.X, op=ALU.max))
                ch(nc.vector.tensor_tensor(out=v3(eq), in0=v3(sel), in1=bc_last(r8[:, 0, :], 8), op=ALU.is_equal))
                ch(nc.vector.scalar_tensor_tensor(out=sel2, in0=eq, scalar=-1e30, in1=sel, op0=ALU.mult, op1=ALU.add))
                ch(nc.vector.tensor_reduce(out=r8[:, 1, :], in_=v3(sel2), axis=AX.X, op=ALU.max))
                ch(nc.vector.tensor_tensor(out=r8[:, 2, :], in0=r8[:, 0, :], in1=r8[:, 1, :], op=ALU.add))
                ch(nc.vector.max(out=r8[:, 3, :], in_=r8[:, 2, :]))
                ch(nc.vector.tensor_scalar(out=r8[:, 4, :], in0=r8[:, 2, :], scalar1=r8[:, 3, 3:4], scalar2=None, op0=ALU.is_ge))
                ch(nc.vector.tensor_scalar(out=r8[:, 5, :], in0=r8[:, 4, :], scalar1=1e30, scalar2=-1e30, op0=ALU.mult, op1=ALU.add))
                ch(nc.vector.tensor_tensor(out=v3(selm), in0=v3(sel), in1=bc_last(r8[:, 4, :], 8), op=ALU.mult))
                ch(nc.vector.tensor_tensor(out=v3(selm), in0=v3(selm), in1=bc_last(r8[:, 5, :], 8), op=ALU.add))
                ch(nc.vector.max(out=r8[:, 6, :], in_=selm))
                ch(nc.vector.tensor_scalar(out=em, in0=selm, scalar1=r8[:, 6, 5:6], scalar2=None, op0=ALU.is_ge))
                DVE.wait(mk_free)
                tmk = DVE.sig(nc.vector.tensor_copy(out=mk[:], in_=em))
                ch(nc.vector.tensor_tensor(out=em, in0=em, in1=sc, op=ALU.mult))
                ch(nc.vector.tensor_reduce(out=r8[:, 7, 0:1], in_=em, axis=AX.X, op=ALU.add))
                ch(nc.vector.reciprocal(out=r8[:, 7, 0:1], in_=r8[:, 7, 0:1]))
                ch(nc.vector.tensor_scalar(out=wdn, in0=em, scalar1=r8[:, 7, 0:1], scalar2=2.5, op0=ALU.mult, op1=ALU.mult))
                ch(nc.vector.max(out=W6[:, gt, :], in_=wdn))
                ch(nc.vector.max_index(i8[:], W6[:, gt, :], wdn))
                ch(nc.vector.tensor_copy(out=E6f[:, gt, :], in_=i8[:]))
                PE.wait(tmk, ps_free[5], t_o128, t_mc)
                nc.tensor.matmul(psF[5][:, 0:64], lhsT=U_b[:, :], rhs=mk[:, :], start=True, stop=True)
                tm2 = PE.sig(nc.tensor.matmul(psF[5][:, 64:128], lhsT=ones128[:, :], rhs=mk[:, :], start=True, stop=True))
                mk_free = tm2
                DVE.wait(tm2, t_c0)
                ch(nc.vector.tensor_tensor(out=RK[:, gt, :], in0=psF[5][:, 0:64], in1=carry[:, :], op=ALU.add))
                t_h = DVE.sig(nc.vector.tensor_tensor(out=carry[:, :], in0=psF[5][:, 64:128], in1=carry[:, :], op=ALU.add))
                ps_free[5] = t_h
                DVE.wait(t_h)
            barrier()
        if debug == "m1":
            return nc

        with contextlib.ExitStack() as st1:
            big = sb("big", [128, NBR * 64], F32, st1)
            nbk = sb("nbk", [128, 64], F32, st1)
            bend = sb("bend", [128, 64], F32, st1)
            pstart = sb("pstart", [128, 64], F32, st1)
            ones64 = sb("ones64", [128, 64], F32, st1)
            BEf = sb("BEf", [128, NBLK], F32, st1)
            PAY = sb("PAY", [128, 64, 6, 2], F32, st1)
            fill = sb("fill", [128, NBR * MB // 128, 2], F32, st1)
            SH = sb("SH", [128, 64, 2], F32, st1)
            zrow = sb("zrow", [1, D], BF16, st1)
            j64 = sb("j64", [128, 64], F32, st1)
            d_i = dsem("minit")
            ch = lambda ins: DVE.wait(DVE.sig(ins))
            ch(nc.vector.memset(fill[:, :, 0:1], float(NT)))
            ch(nc.vector.memset(fill[:, :, 1:2], 0.0))
            ch(nc.vector.memset(zrow[:], 0.0))
            ch(nc.vector.memset(SH[:, :, 1:2], 1.0))
            ch(nc.vector.tensor_copy(out=SH[:, :, 0], in_=tokid))
            ch(nc.vector.memset(ones64[:], 1.0))
            SP.wait((DVE, DVE.cnt))
            nc.sync.dma_start(out=ROWTW[0:NBR * MB, :].rearrange("(p j) c -> p j c", p=128), in_=fill[:]).then_inc(d_i.sem, 16)
            nc.sync.dma_start(out=ROWTW[NBR * MB:RROWS, :].rearrange("(j p) c -> p j c", p=128), in_=SH[:]).then_inc(d_i.sem, 16)
            d_i.cnt += 32
            t_init = d_i.sig(nc.sync.dma_start(out=H2[NT:NT + 1, :], in_=zrow[:]))
            big3 = big[:, 0:64 * 16].rearrange("p (e j) -> p e j", j=16)
            ch(nc.vector.tensor_tensor(out=big3, in0=bc_last(carry[:, :], 16), in1=bc_mid(thr16, 64), op=ALU.is_gt))
            ch(nc.vector.tensor_reduce(out=nbk[:], in_=big3, axis=AX.X, op=ALU.add))
            ch(nc.vector.tensor_tensor_scan(out=bend[:], data0=ones64[:], data1=nbk[:], initial=0.0, op0=ALU.mult, op1=ALU.add))
            ch(nc.vector.tensor_tensor(out=pstart[:], in0=bend[:], in1=nbk[:], op=ALU.subtract))
            ch(nc.vector.tensor_scalar(out=pstart[:], in0=pstart[:], scalar1=float(MB), scalar2=None, op0=ALU.mult))
            bigb = big[:, :].rearrange("p (i e) -> p i e", e=64)
            ch(nc.vector.tensor_tensor(out=bigb, in0=bc_last(iota_b[:, 0:NBR], 64), in1=bc_mid(bend[:, :], NBR), op=ALU.is_ge))
            ch(nc.vector.tensor_reduce(out=BEf[:, 0:NBR], in_=bigb, axis=AX.X, op=ALU.add))
            ch(nc.vector.tensor_scalar(out=big[:, 0:NBR], in0=BEf[:, 0:NBR], scalar1=64.0, scalar2=None, op0=ALU.is_ge))
            ch(nc.vector.tensor_tensor(out=BEf[:, 0:NBR], in0=BEf[:, 0:NBR], in1=big[:, 0:NBR], op=ALU.add))
            ch(nc.vector.memset(BEf[:, NBR:NBLK], 64.0))
            ch(nc.vector.tensor_scalar(out=BEf[:, :], in0=BEf[:, :], scalar1=128.0, scalar2=pcol, op0=ALU.mult, op1=ALU.add))
            ch(nc.vector.tensor_copy(out=IDXW[:, :], in_=BEf[:, :]))
            d_sc = dsem("scat")
            POOL.wait(t_init)
            for gt in range(64):
                ch(nc.vector.tensor_tensor(out=RK[:, gt, :], in0=RK[:, gt, :], in1=pstart[:, :], op=ALU.add))
                for k in range(6):
                    stt_ = nc.vector.scalar_tensor_tensor(out=big[:, k * 64:(k + 1) * 64], in0=iota_e, scalar=E6f[:, gt, k:k + 1], in1=RK[:, gt, :],
                                                          op0=ALU.is_equal, op1=ALU.mult, accum_out=D6f[:, gt, k:k + 1])
                DVE.wait(DVE.sig(stt_))
                for k in range(6):
                    nc.vector.tensor_copy(out=PAY[:, gt, k, 0:1], in_=tokid[:, gt:gt + 1])
                ch(nc.vector.tensor_copy(out=PAY[:, gt, :, 1], in_=W6[:, gt, 0:6]))
                tdi = DVE.sig(nc.vector.tensor_copy(out=D6i[:, gt, 0:6], in_=D6f[:, gt, 0:6]))
                DVE.wait(tdi)
                POOL.wait(tdi)
                for k in range(6):
                    t_sc = d_sc.sig(nc.gpsimd.indirect_dma_start(
                        out=ROWTW[:, :], out_offset=bass.IndirectOffsetOnAxis(ap=D6i[:, gt, k:k + 1], axis=0),
                        in_=PAY[:, gt, k, :], in_offset=None))
            barrier()
        if debug == "m1b":
            return nc

        with contextlib.ExitStack() as st1:
            NS = 3
            wg = [sb("wg%d" % i, [128, 2048], BF16, st1) for i in range(NS)]
            wu = [sb("wu%d" % i, [128, 2048], BF16, st1) for i in range(NS)]
            wd = [sb("wd%d" % i, [128, 2048], BF16, st1) for i in range(NS)]
            xg = [sb("xg%d" % i, [128, 4, D], BF16, st1) for i in range(NS)]
            rtw = [sb("rtw%d" % i, [128, 4, 2], F32, st1) for i in range(NS)]
            tki = [sb("tki%d" % i, [128, 4], I32, st1) for i in range(NS)]
            xgT = [sb("xgT%d" % i, [128, 8, MB], BF16, st1) for i in range(2)]
            hm = [sb("hm%d" % i, [128, 2, MB], BF16, st1) for i in range(2)]
            sg = [sb("sg%d" % i, [128, MB], F32, st1) for i in range(2)]
            osb = [sb("osb%d" % i, [128, 4, D], BF16, st1) for i in range(2)]
            d_rt = [dsem("rtw%d" % i) for i in range(NS)]
            d_we = [dsem("we%d" % i) for i in range(NS)]
            d_xg = [dsem("xg%d" % i) for i in range(NS)]
            d_o = [dsem("osb%d" % i) for i in range(2)]
            we_free = [None] * NS
            xg_free = [None] * NS
            rtw_free = [None] * NS
            tki_free = [None] * NS
            t_rtw = [None] * NS
            t_we = [None] * NS
            t_xg = [None] * NS
            xgT_free = [None, None]
            hm_free = [None, None]
            sg_free = [None, None]
            osb_free = [None, None]
            t_xgT = [None, None]
            psB_free = [None, None]
            ev = 0
            bnd_reg = nc.gpsimd.to_reg(65 * 128 - 1)

            def loads(i):
                s3 = i % NS
                SP.wait(rtw_free[s3])
                t_rtw[s3] = d_rt[s3].sig(nc.sync.dma_start(out=rtw[s3][:], in_=ROWTW[i * MB:(i + 1) * MB, :].rearrange("(j p) c -> p j c", p=128)))
                DVE.wait(t_rtw[s3], tki_free[s3])
                tk = DVE.sig(nc.vector.tensor_copy(out=tki[s3][:], in_=rtw[s3][:, :, 0]))
                POOL.wait(xg_free[s3], tk)
                for j in range(4):
                    tx = d_xg[s3].sig(nc.gpsimd.indirect_dma_start(out=xg[s3][:, j, :], out_offset=None, in_=H2[:, :],
                                                                   in_offset=bass.IndirectOffsetOnAxis(ap=tki[s3][:, j:j + 1], axis=0)))
                tki_free[s3] = tx
                t_xg[s3] = tx
                POOL.wait(we_free[s3])
                for m_, wt_ in enumerate((wg, wu, wd)):
                    t_we[s3] = d_we[s3].sig(nc.gpsimd.indirect_dma_start(
                        out=wt_[s3][:, :], out_offset=None, in_=WB[m_][:, :],
                        in_offset=bass.IndirectOffsetOnAxis(ap=IDXW[:, i:i + 1], axis=0), bounds_check=bnd_reg, oob_is_err=False))

            def transposes(i):
                nonlocal ev
                s2 = i % 2
                s3 = i % NS
                PE.wait(t_xg[s3], xgT_free[s2])
                last = []
                for kt in range(8):
                    pb_ = kt % 2
                    PE.wait(psB_free[pb_])
                    for j in range(4):
                        tp = nc.tensor.transpose(psB[pb_][:, j * 128:(j + 1) * 128], xg[s3][:, j, kt * 128:(kt + 1) * 128], id_b[:])
                    tp = PE.sig(tp)
                    if ev % 2 == 0:
                        ACT.wait(tp)
                        te = ACT.sig(nc.scalar.copy(out=xgT[s2][:, kt, :], in_=psB[pb_][:, 0:MB]))
                    else:
                        DVE.wait(tp)
                        te = DVE.sig(nc.vector.tensor_copy(out=xgT[s2][:, kt, :], in_=psB[pb_][:, 0:MB]))
                    ev += 1
                    psB_free[pb_] = te
                    last = (last + [te])[-2:]
                xg_free[s3] = tp
                t_xgT[s2] = last

            for q_ in range(NS):
                for wt_ in (wg, wu, wd):
                    POOL.sig(nc.gpsimd.memset(wt_[q_][:], 0.0))
            we_free = [(POOL, POOL.cnt)] * NS
            for i in range(min(NS, NBLK)):
                loads(i)
            PE.wait(t_mc)
            transposes(0)
            for i in range(NBLK):
                s2 = i % 2
                s3 = i % NS
                PE.wait(t_xgT[s2], t_we[s3])
                for ft in range(2):
                    PE.wait(ps_free[ft * 2], ps_free[ft * 2 + 1])
                    for kt in range(8):
                        nc.tensor.matmul(psF[ft * 2][:, :], lhsT=wg[s3][:, kt * 256 + ft * 128:kt * 256 + (ft + 1) * 128], rhs=xgT[s2][:, kt, :],
                                         start=(kt == 0), stop=(kt == 7))
                    for kt in range(8):
                        mm = nc.tensor.matmul(psF[ft * 2 + 1][:, :], lhsT=wu[s3][:, kt * 256 + ft * 128:kt * 256 + (ft + 1) * 128], rhs=xgT[s2][:, kt, :],
                                              start=(kt == 0), stop=(kt == 7))
                    tm = PE.sig(mm)
                    ACT.wait(tm, sg_free[ft])
                    ta3 = ACT.sig(nc.scalar.activation(out=sg[ft][:], in_=psF[ft * 2][:, :], func=AF.Silu))
                    DVE.wait(ta3, hm_free[s2] if ft == 0 else None)
                    td = DVE.sig(nc.vector.tensor_tensor(out=hm[s2][:, ft, :], in0=psF[ft * 2 + 1][:, :], in1=sg[ft][:], op=ALU.mult))
                    sg_free[ft] = td
                    ps_free[ft * 2] = td
                    ps_free[ft * 2 + 1] = td
                xgT_free[s2] = tm
                t_hm = td
                if i + 1 < NBLK:
                    transposes(i + 1)
                PE.wait(t_hm)
                ACT.wait(osb_free[s2])
                DVE.wait(osb_free[s2])
                evs = []
                for j in range(4):
                    for nh in range(2):
                        pi = 4 + (j * 2 + nh) % 2
                        PE.wait(ps_free[pi])
                        for ft in range(2):
                            mm = nc.tensor.matmul(psF[pi][:, :], lhsT=hm[s2][:, ft, j * 128:(j + 1) * 128],
                                                  rhs=wd[s3][:, ft * 1024 + nh * 512:ft * 1024 + (nh + 1) * 512], start=(ft == 0), stop=(ft == 1))
                        tm = PE.sig(mm)
                        o_ = osb[s2][:, j, nh * 512:(nh + 1) * 512]
                        if nh == 0:
                            ACT.wait(tm, t_rtw[s3])
                            te = ACT.sig(nc.scalar.activation(out=o_, in_=psF[pi][:, :], func=AF.Copy, scale=rtw[s3][:, j, 1:2]))
                        else:
                            DVE.wait(tm, t_rtw[s3])
                            te = DVE.sig(nc.vector.tensor_scalar(out=o_, in0=psF[pi][:, :], scalar1=rtw[s3][:, j, 1:2], scalar2=None, op0=ALU.mult))
                        ps_free[pi] = te
                        evs.append(te)
                hm_free[s2] = tm
                we_free[s3] = tm
                rtw_free[s3] = evs[-2:]
                SP.wait(evs[-1], evs[-2])
                osb_free[s2] = d_o[s2].sig(nc.sync.dma_start(out=OUTR[i * MB:(i + 1) * MB, :].rearrange("(j p) n -> p j n", p=128), in_=osb[s2][:]))
                if i + NS < NBLK:
                    loads(i + NS)
            barrier()
        if debug == "m2":
            return nc

        with contextlib.ExitStack() as st1:
            gbuf = [sb("gbuf%d" % i, [128, 7, D], BF16, st1) for i in range(2)]
            d_g = [dsem("gb%d" % i) for i in range(2)]
            d_y = [dsem("my%d" % i) for i in range(2)]
            gb_free = [None, None]
            ch = lambda ins: DVE.wait(DVE.sig(ins))
            for gt in range(64):
                seg = gt // 16
                b = gt % 2
                POOL.wait(gb_free[b])
                for k in range(6):
                    nc.gpsimd.indirect_dma_start(out=gbuf[b][:, k, :], out_offset=None, in_=OUTR[:, :],
                                                 in_offset=bass.IndirectOffsetOnAxis(ap=D6i[:, gt, k:k + 1], axis=0)).then_inc(d_g[b].sem, 16)
                d_g[b].cnt += 96
                SP.wait(gb_free[b], xin_free[b])
                nc.sync.dma_start(out=gbuf[b][:, 6, :], in_=OUTR[NBR * MB + gt * 128:NBR * MB + (gt + 1) * 128, :]).then_inc(d_g[b].sem, 16)
                d_g[b].cnt += 16
                tl = d_g[b].sig(nc.sync.dma_start(out=xin[b][:], in_=X1_v[gt]))
                PE.wait(tl, ps_free[2 * b], ps_free[2 * b + 1])
                for nh in range(2):
                    for k in range(7):
                        mm = nc.tensor.matmul(psF[2 * b + nh][:, :], lhsT=id_b[:, :], rhs=gbuf[b][:, k, nh * 512:(nh + 1) * 512],
                                              start=(k == 0), stop=(k == 6))
                tm = PE.sig(mm)
                gb_free[b] = tm
                DVE.wait(tm, xs_free[b], t_g2)
                a_ = xs[b]
                for nh in range(2):
                    hs_ = slice(nh * 512, (nh + 1) * 512)
                    ch(nc.vector.tensor_tensor(out=a_[:, hs_], in0=psF[2 * b + nh][:, :], in1=G2all[:, seg, hs_], op=ALU.mult))
                ps_free[2 * b] = (DVE, DVE.cnt)
                ps_free[2 * b + 1] = (DVE, DVE.cnt)
                td = DVE.sig(nc.vector.tensor_tensor(out=a_[:], in0=a_[:], in1=xin[b][:], op=ALU.add))
                xin_free[b] = td
                ACT.wait(td)
                col = gt
                ta = ACT.sig(nc.scalar.activation(out=junk[:], in_=a_[:], func=AF.Square, accum_out=ssq[:, col:col + 1]))
                ACT.wait(ta)
                ta = ACT.sig(nc.scalar.activation(out=ssq[:, col:col + 1], in_=ssq[:, col:col + 1], func=AF.Sqrt, scale=1.0 / D, bias=EPS))
                DVE.wait(ta)
                ch(nc.vector.reciprocal(out=ssq[:, col:col + 1], in_=ssq[:, col:col + 1]))
                td = DVE.sig(nc.vector.scalar_tensor_tensor(out=a_[:], in0=a_[:], scalar=ssq[:, col:col + 1], in1=FNbc[:, :], op0=ALU.mult, op1=ALU.mult))
                SP.wait(td)
                xs_free[b] = d_y[b].sig(nc.sync.dma_start(out=y_v[gt], in_=a_[:]))
            barrier()
    es.close()
    return nc


def _rel_bucket(rel):
    half = 16
    max_exact = 8
    n = np.abs(rel)
    large = max_exact + (np.log(np.maximum(n, 1) / max_exact) / math.log(1024 / max_exact) * (half - max_exact)).astype(np.int32)
    large = np.minimum(large, half - 1)
    return ((rel > 0).astype(np.int32) * half + np.where(n < max_exact, n, large)).astype(np.int32)


def _onehot_tables():
    oh = np.zeros((32, 4, 512), np.float32)
    for kind, (Hb, d) in enumerate(((128, 1), (64, 1), (64, 4), (64, 16))):
        Rmax = 2 * Hb - 1
        for i in range(4 * Hb - 1):
            rel = Rmax - i
            if abs(rel) <= Hb:
                oh[int(_rel_bucket(np.array([rel * d]))[0]), kind, i] = 1.0
    return oh


def _lay_gu(w, wsh):
    a = np.concatenate([w, wsh[None]], axis=0)
    a = a.reshape(65, 8, 128, 256).transpose(0, 2, 1, 3)
    return np.ascontiguousarray(a.reshape(65 * 128, 2048))


def _lay_d(w, wsh):
    a = np.concatenate([w, wsh[None]], axis=0)
    a = a.reshape(65, 2, 128, 1024).transpose(0, 2, 1, 3)
    return np.ascontiguousarray(a.reshape(65 * 128, 2048))


def _consts():
    c = np.zeros((128, NCST), np.float32)
    p = np.arange(128, dtype=np.float32)[:, None]
    c[:, 0:64] = np.arange(64, dtype=np.float32)[None, :]
    c[:, 64:128] = np.arange(64, dtype=np.float32)[None, :] * 128 + p
    c[:, 128:129] = p
    c[:, 129:129 + NBLK] = np.arange(NBLK, dtype=np.float32)[None, :]
    c[:, 129 + NBLK:] = np.arange(16, dtype=np.float32)[None, :] * MB
    return c


def make_in_maps(inp):
    f = lambda a: np.ascontiguousarray(np.asarray(a, dtype=np.float32))
    xp = f(inp["x_prompt"]); xs_ = f(inp["x_sample"]); cp = f(inp["c_prompt"]); cs = f(inp["c_sample"])
    common = {
        "rel_bias": f(inp["rel_bias"]), "sink": f(inp["sink"]).reshape(1, 8),
        "w_ada": f(inp["w_ada"])[0], "b_adaT": f(f(inp["b_ada"])[0].reshape(48, 128).T),
        "norm1T": f(f(inp["norm1"])[0].reshape(8, 128).T), "norm2T": f(f(inp["norm2"])[0].reshape(8, 128).T),
        "final_norm": f(inp["final_norm"]).reshape(1, D),
        "w_in": f(inp["w_in"])[0], "w_pa": f(inp["w_pa"])[0], "w_pb": f(inp["w_pb"])[0], "w_o": f(inp["w_o"])[0],
        "w_router": f(inp["w_router"])[0], "router_bias": f(inp["router_bias"]).reshape(1, NE),
        "wg2": _lay_gu(f(inp["w_gate"])[0], f(inp["ws_gate"])[0]), "wu2": _lay_gu(f(inp["w_up"])[0], f(inp["ws_up"])[0]),
        "wd2": _lay_d(f(inp["w_down"])[0], f(inp["ws_down"])[0]),
        "cst": _consts(), "umat": np.ascontiguousarray(np.triu(np.ones((128, 128), np.float32), 1)),
        "ident": np.eye(128, dtype=np.float32), "jmat": np.ascontiguousarray(np.eye(128, dtype=np.float32)[::-1]),
        "oh": _onehot_tables(),
    }
    maps = []
    for core in range(8):
        if core < 4:
            xc = xp[core]
            cc = np.repeat(cp[core:core + 1], 4, axis=0)
            cn = 1.0
        else:
            k = core - 4
            xc = xs_[4 * k:4 * k + 4].reshape(NT, D)
            cc = cs[4 * k:4 * k + 4]
            cn = 0.0
        m = dict(common)
        m["x"] = f(xc)
        m["cT"] = f(cc.reshape(4, 8, 128).transpose(2, 1, 0))
        m["conn"] = np.full((128, 1), cn, np.float32)
        maps.append(m)
    return maps


_NC_CACHE = {}


def kernel(**inputs):
    if "nc" not in _NC_CACHE:
        _NC_CACHE["nc"] = build()
    nc = _NC_CACHE["nc"]
    maps = make_in_maps(inputs)
    res = run_bass_kernel_spmd(nc, maps, core_ids=list(range(8)))
    ys = [np.asarray(r["y"], dtype=np.float32) for r in res.results]
    y_prompt = np.stack(ys[0:4], axis=0)
    y_sample = np.concatenate([ys[4 + k].reshape(4, 2048, D) for k in range(4)], axis=0)
    return (y_prompt, y_sample)
```

```python
import contextlib
import math
import numpy as np
import concourse.bass as bass
import concourse.mybir as mybir
from concourse.bass_utils import run_bass_kernel_spmd

F32 = mybir.dt.float32
BF16 = mybir.dt.bfloat16
AF = mybir.ActivationFunctionType
ALU = mybir.AluOpType
AX = mybir.AxisListType

NT = 8192
D = 1024
CH = 1024
NCH = NT // CH
EPS = 1e-6
NE = 64
MB = 512
NBR = NT * 6 // MB + NE
NBS = NT // MB
NBLK = NBR + NBS
RROWS = NBLK * MB
NCST = 64 + 64 + 1 + NBLK + 16
GROUPS = ((128, 1), (512, 4), (2048, 16))


def _flat(toks):
    for t in toks:
        if t is None:
            continue
        if isinstance(t, (list, tuple)) and not (len(t) == 2 and isinstance(t[1], int)):
            yield from _flat(t)
        else:
            yield t


class Eng:
    def __init__(self, nc, eng, name, es):
        self.eng = eng
        self.name = name
        self.sem = es.enter_context(nc.semaphore("s_" + name))
        self.cnt = 0
        self.seen = {}

    def wait(self, *toks):
        for p, v in _flat(toks):
            if self.seen.get(id(p), 0) >= v:
                continue
            self.seen[id(p)] = v
            self.eng.wait_ge(p.sem, v)

    def sig(self, instr):
        self.cnt += 1
        instr.then_inc(self.sem, 1)
        return (self, self.cnt)


class DSem:
    def __init__(self, nc, name, es):
        self.sem = es.enter_context(nc.semaphore("d_" + name))
        self.cnt = 0

    def sig(self, instr):
        self.cnt += 16
        instr.then_inc(self.sem, 16)
        return (self, self.cnt)


def build(debug=None):
    nc = bass.Bass("TRN2", target_bir_lowering=False)
    es = contextlib.ExitStack()

    def din(name, shape, dt=F32):
        return nc.dram_tensor(name, list(shape), dt, kind="ExternalInput").ap()

    x = din("x", [NT, D])
    cT = din("cT", [128, 8, 4])
    conn = din("conn", [128, 1])
    rel_bias = din("rel_bias", [32, 20])
    sink = din("sink", [1, 8])
    w_ada = din("w_ada", [D, 6 * D])
    b_adaT = din("b_adaT", [128, 48])
    norm1T = din("norm1T", [128, 8])
    norm2T = din("norm2T", [128, 8])
    final_norm = din("final_norm", [1, D])
    w_in = din("w_in", [D, 5120])
    w_pa = din("w_pa", [512, D])
    w_pb = din("w_pb", [256, D])
    w_o = din("w_o", [D, D])
    w_router = din("w_router", [D, NE])
    router_bias = din("router_bias", [1, NE])
    wg2 = din("wg2", [65 * 128, 2048])
    wu2 = din("wu2", [65 * 128, 2048])
    wd2 = din("wd2", [65 * 128, 2048])
    cst_d = din("cst", [128, NCST])
    umat_d = din("umat", [128, 128])
    ident_d = din("ident", [128, 128])
    jmat_d = din("jmat", [128, 128])
    oh_d = din("oh", [32, 4, 512])
    y = nc.dram_tensor("y", [NT, D], F32, kind="ExternalOutput").ap()

    skind = "ExternalOutput" if debug else "Internal"
    PT = nc.dram_tensor("PT", [2176, NT], BF16, kind=skind).ap()
    GT = nc.dram_tensor("GT", [2048, NT], BF16, kind=skind).ap()
    VV = nc.dram_tensor("VV", [NT, 896], BF16, kind=skind).ap()
    X1 = nc.dram_tensor("X1", [NT, D], F32, kind=skind).ap()
    TBL = nc.dram_tensor("TBL", [20, 512], F32, kind="Internal").ap()
    H2 = nc.dram_tensor("H2", [NT + 1, D], BF16, kind="Internal").ap()
    ROWTW = nc.dram_tensor("ROWTW", [RROWS, 2], F32, kind="Internal").ap()
    OUTR = nc.dram_tensor("OUTR", [RROWS, D], BF16, kind="Internal").ap()
    WB = [nc.dram_tensor("WB%d" % i, [65 * 128, 2048], BF16, kind="Internal").ap() for i in range(3)]

    PE = Eng(nc, nc.tensor, "pe", es)
    ACT = Eng(nc, nc.scalar, "act", es)
    DVE = Eng(nc, nc.vector, "dve", es)
    POOL = Eng(nc, nc.gpsimd, "pool", es)
    SP = Eng(nc, nc.sync, "sp", es)
    ENGS = [PE, ACT, DVE, POOL, SP]

    def sb(name, shape, dt, st=None):
        return (st or es).enter_context(nc.sbuf_tensor("sb_" + name, list(shape), dt))

    pst = contextlib.ExitStack()

    def ps(name, st=None):
        return (st or pst).enter_context(nc.psum_tensor(name, [128, 512], F32))

    all_dsems = []

    def dsem(name):
        d = DSem(nc, name, es)
        all_dsems.append(d)
        return d

    def barrier():
        DVE.sig(nc.vector.memset(bar_t[:, 0:1], 0.0))
        ACT.wait(t_ones)
        ACT.sig(nc.scalar.activation(out=bar_t[:, 1:2], in_=ones_f[:, 0:1], func=AF.Copy))
        POOL.sig(nc.gpsimd.memset(bar_t[:, 2:3], 0.0))
        toks = [(e, e.cnt) for e in ENGS if e.cnt > 0] + [(d, d.cnt) for d in all_dsems if d.cnt > 0]
        for e in ENGS:
            e.wait(*[t for t in toks if t[0] is not e])

    bar_t = sb("bar_t", [128, 4], F32)
    ident = sb("ident", [128, 128], F32)
    jmat = sb("jmat", [128, 128], F32)
    ones_f = sb("ones_f", [128, 128], F32)
    ones_b = sb("ones_b", [128, 64], BF16)
    ones_bd = sb("ones_bd", [128, 128], BF16)
    connc = sb("connc", [128, 1], F32)
    modT = sb("modT", [128, 48, 4], F32)
    A1 = sb("A1", [128, 4, 8], F32)
    A2 = sb("A2", [128, 4, 8], F32)
    n1T = sb("n1T", [128, 8], F32)
    n2T = sb("n2T", [128, 8], F32)
    badaT = sb("badaT", [128, 48], F32)
    scT = sb("scT", [128, 8, 4], F32)
    es_bc = sb("es_bc", [128, 8], F32)
    EBA = sb("EBA", [128, 8, 2, 384], BF16)
    EBB = sb("EBB", [128, 6, 2, 192], BF16)
    psA = [ps("psA%d" % i) for i in range(8)]

    d_c = dsem("const")
    toks = []
    for dst, src in ((ident, ident_d), (jmat, jmat_d), (connc, conn), (n1T, norm1T), (n2T, norm2T),
                     (badaT, b_adaT), (scT, cT)):
        toks.append(d_c.sig(nc.sync.dma_start(out=dst[:], in_=src)))
    toks.append(d_c.sig(nc.sync.dma_start(out=es_bc[:], in_=sink[0:1, :].partition_broadcast(128))))
    t_const = toks[-1]
    t_ones = DVE.sig(nc.vector.memset(ones_f[:], 1.0))
    t_onesb = DVE.sig(nc.vector.memset(ones_b[:], 1.0))
    DVE.wait(DVE.sig(nc.vector.memset(ones_bd[:], 0.0)))
    DVE.wait(DVE.sig(nc.vector.memset(ones_bd[0:64, 0:64], 1.0)))
    t_onesb = DVE.sig(nc.vector.memset(ones_bd[64:128, 64:128], 1.0))
    ACT.wait(t_const)
    t_sc = ACT.sig(nc.scalar.activation(out=scT[:], in_=scT[:], func=AF.Silu))
    t_es = ACT.sig(nc.scalar.activation(out=es_bc[:], in_=es_bc[:], func=AF.Exp))

    with contextlib.ExitStack() as st:
        wst = [sb("wada%d" % i, [128, 8, 512], F32, st) for i in range(2)]
        d_w = [dsem("wada%d" % i) for i in range(2)]
        w_ada_v = w_ada.rearrange("(kt p) n -> p kt n", p=128)
        rel_t = [None, None]
        PE.wait(t_sc)
        ev_t = [None] * 8
        for blk in range(12):
            s = blk % 2
            SP.wait(rel_t[s])
            tl = d_w[s].sig(nc.sync.dma_start(out=wst[s][:], in_=w_ada_v[:, :, blk * 512:(blk + 1) * 512]))
            PE.wait(tl, ev_t[blk % 8])
            pt = psA[blk % 8]
            for ft in range(4):
                for kt in range(8):
                    mm = nc.tensor.matmul(pt[:, ft * 4:ft * 4 + 4], lhsT=wst[s][:, kt, ft * 128:(ft + 1) * 128],
                                          rhs=scT[:, kt, :], start=(kt == 0), stop=(kt == 7))
            tm = PE.sig(mm)
            rel_t[s] = tm
            DVE.wait(tm, t_const)
            for ft in range(4):
                i_ = nc.vector.tensor_scalar(out=modT[:, blk * 4 + ft, :], in0=pt[:, ft * 4:ft * 4 + 4],
                                             scalar1=badaT[:, blk * 4 + ft:blk * 4 + ft + 1], scalar2=None,
                                             op0=ALU.add)
            ev_t[blk % 8] = DVE.sig(i_)
        t_mod = ev_t[11 % 8]
        DVE.wait(t_mod)
        for s in range(4):
            for (Ax, nT, part) in ((A1, n1T, 1), (A2, n2T, 4)):
                DVE.wait(DVE.sig(nc.vector.tensor_scalar(out=Ax[:, s, :], in0=modT[:, part * 8:(part + 1) * 8, s], scalar1=1.0,
                                        scalar2=None, op0=ALU.add)))
                t_A = DVE.sig(nc.vector.tensor_tensor(out=Ax[:, s, :], in0=Ax[:, s, :], in1=nT[:, :], op=ALU.mult))
                DVE.wait(t_A)
        barrier()

    def row_bcast(dst, part, seg, diag, pss):
        last = None
        for kt in range(8):
            DVE.wait(last)
            td = DVE.sig(nc.vector.tensor_scalar(out=diag[:], in0=ident[:], scalar1=modT[:, part * 8 + kt, seg:seg + 1],
                                                 scalar2=None, op0=ALU.mult))
            PE.wait(td, t_ones)
            last = PE.sig(nc.tensor.matmul(pss[kt // 4][:, (kt % 4) * 128:(kt % 4 + 1) * 128], lhsT=ones_f[:], rhs=diag[:],
                                           start=True, stop=True))
        DVE.wait(last)
        nc.vector.tensor_copy(out=dst[:, 0:512], in_=pss[0][:, :])
        return DVE.sig(nc.vector.tensor_copy(out=dst[:, 512:1024], in_=pss[1][:, :]))

    with contextlib.ExitStack() as st:
        rb = sb("rb", [32, 20], F32, st)
        oh = sb("oh", [32, 4, 512], F32, st)
        tb = sb("tb", [32, 512], F32, st)
        tkr = sb("tkr", [128, 3, 128], F32, st)
        d_t = dsem("tbl")
        t1 = d_t.sig(nc.sync.dma_start(out=rb[:], in_=rel_bias))
        t2 = d_t.sig(nc.sync.dma_start(out=oh[:], in_=oh_d))
        ACT.wait(t2)
        t_rb = ACT.sig(nc.scalar.activation(out=rb[:], in_=rb[:], func=AF.Exp))
        PE.wait(t_rb)
        hs = [(0, 8), (8, 4), (12, 4), (16, 4)]
        for kind, (h0, nh) in enumerate(hs):
            tm = PE.sig(nc.tensor.matmul(psA[kind][0:nh, :], lhsT=rb[:, h0:h0 + nh], rhs=oh[:, kind, :], start=True, stop=True))
            DVE.wait(tm)
            tcp = DVE.sig(nc.vector.tensor_copy(out=tb[0:nh, :], in_=psA[kind][0:nh, :]))
            SP.wait(tcp)
            tst = d_t.sig(nc.sync.dma_start(out=TBL[h0:h0 + nh, :], in_=tb[0:nh, :]))
            DVE.wait(tst)
        SP.wait(tst)
        tprev = None
        for h in range(20):
            Hb = 128 if h < 8 else 64
            Rmax = 2 * Hb - 1
            SP.wait(tprev)
            for di in range(3):
                src = bass.AP(tensor=TBL.tensor, offset=h * 512 + Rmax - Hb * (di - 1) - Hb + 1, ap=[[1, Hb], [1, Hb]])
                tl = d_t.sig(nc.sync.dma_start(out=tkr[0:Hb, di, 0:Hb], in_=src))
            PE.wait(tl)
            pt = psA[h % 8]
            if h < 8:
                tm = PE.sig(nc.tensor.matmul(pt[:, 0:384], lhsT=jmat[:, :], rhs=tkr[:, :, :].rearrange("p a b -> p (a b)"),
                                             start=True, stop=True))
                dst0 = EBA[:, h, 0, :]
                dst1 = EBA[:, h, 1, :]
                src_ps = pt[:, 0:384]
                cc = connc[:, 0:1]
            else:
                hh = h - 8
                pp = (hh % 2) * 64
                tm = PE.sig(nc.tensor.matmul(pt[pp:pp + 64, 0:192].rearrange("p (a b) -> p a b", a=3),
                                             lhsT=jmat[0:64, 64:128] if pp == 0 else jmat[0:64, 64:128],
                                             rhs=tkr[0:64, :, 0:64], start=True, stop=True))
                dst0 = EBB[pp:pp + 64, hh // 2, 0, :]
                dst1 = EBB[pp:pp + 64, hh // 2, 1, :]
                src_ps = pt[pp:pp + 64, 0:192]
                cc = connc[pp:pp + 64, 0:1]
            tprev = tm
            DVE.wait(tm)
            nc.vector.tensor_copy(out=dst0, in_=src_ps)
            t_eb = DVE.sig(nc.vector.tensor_scalar(out=dst1, in0=src_ps, scalar1=cc, scalar2=None, op0=ALU.mult))
        barrier()

    pt_cols = [i * 128 for i in range(4)] + [512] + [768 + i * 128 for i in range(6)] + [1536 + i * 128 for i in range(6)]
    x_v = x.rearrange("(t p) n -> t p n", p=128)
    X1_v = X1.rearrange("(t p) n -> t p n", p=128)
    y_v = y.rearrange("(t p) n -> t p n", p=128)

    def norm_transpose(st_name, src_v, tile_idx, xin, xs, ssq, rstd, junk, tok_xin_free, d_x, psT, psT_free, hT, hcol, Ax, seg,
                       Bpart, hT_free):
        raise NotImplementedError

    with contextlib.ExitStack() as st:
        wi = sb("wi", [128, 8, 5120], BF16, st)
        xin = [sb("xin%d" % i, [128, D], F32, st) for i in range(2)]
        xs = [sb("xs%d" % i, [128, D], F32, st) for i in range(2)]
        junk = sb("junk", [128, D], F32, st)
        ssq = sb("ssq", [128, 16], F32, st)
        hT = sb("hT", [128, 8, CH], BF16, st)
        stQ = [sb("stQ%d" % i, [128, 17, 512], BF16, st) for i in range(1)]
        stG = [sb("stG%d" % i, [128, 16, 512], BF16, st) for i in range(1)]
        stV = sb("stV", [128, 8, 896], BF16, st)
        d_wi = dsem("wi")
        d_x = [dsem("x%d" % i) for i in range(2)]
        d_st = dsem("st")
        w_in_v = w_in.rearrange("(kt p) n -> p kt n", p=128)
        t_wi = None
        for blk in range(10):
            t_wi = d_wi.sig(nc.gpsimd.dma_start(out=wi[:, :, blk * 512:(blk + 1) * 512], in_=w_in_v[:, :, blk * 512:(blk + 1) * 512]))
        cvb = [sb("cvb%d" % i, [128, 3, 2048], BF16, st) for i in range(2)]
        d_cl = [dsem("cvl%d" % i) for i in range(2)]
        d_cs = [dsem("cvs%d" % i) for i in range(2)]
        cv_state = {"ld": [None, None], "st": [None, None], "n": 0}
        wsrc = (wg2, wu2, wd2)

        def cv_load(e):
            sl = e % 2
            POOL.wait(cv_state["st"][sl])
            for m_ in range(3):
                t_ = d_cl[sl].sig(nc.gpsimd.dma_start(out=cvb[sl][:, m_, :], in_=wsrc[m_][e * 128:(e + 1) * 128, :]))
            cv_state["ld"][sl] = t_

        def cv_store(e):
            sl = e % 2
            POOL.wait(cv_state["ld"][sl])
            for m_ in range(3):
                t_ = d_cs[sl].sig(nc.gpsimd.dma_start(out=WB[m_][e * 128:(e + 1) * 128, :], in_=cvb[sl][:, m_, :]))
            cv_state["st"][sl] = t_

        def convert_some(k):
            for _ in range(k):
                e = cv_state["n"]
                if e >= 65:
                    return
                if e == 0:
                    cv_load(0)
                if e + 1 < 65:
                    cv_load(e + 1)
                cv_store(e)
                cv_state["n"] += 1

        xin_free = [None, None]
        xs_free = [None, None]
        psT_free = [None] * 4
        pp_free = [None] * 2
        pv_free = None
        hT_free = None
        stq_free = None
        stg_free = None
        stv_free = None
        cnt_tile = 0
        ev_flip = 0
        for c in range(NCH):
            seg = c // 2
            t_h = None
            convert_some(9)
            for tt in range(8):
                gt = c * 8 + tt
                b = gt % 2
                SP.wait(xin_free[b])
                tl = d_x[b].sig(nc.sync.dma_start(out=xin[b][:], in_=x_v[gt]))
                ACT.wait(tl)
                col = gt % 16
                ta = ACT.sig(nc.scalar.activation(out=junk[:], in_=xin[b][:], func=AF.Square, accum_out=ssq[:, col:col + 1]))
                ACT.wait(ta)
                ta = ACT.sig(nc.scalar.activation(out=ssq[:, col:col + 1], in_=ssq[:, col:col + 1], func=AF.Sqrt, scale=1.0 / D, bias=EPS))
                DVE.wait(ta, xs_free[b])
                td = DVE.sig(nc.vector.reciprocal(out=ssq[:, col:col + 1], in_=ssq[:, col:col + 1]))
                DVE.wait(td)
                td = DVE.sig(nc.vector.tensor_scalar(out=xs[b][:], in0=xin[b][:], scalar1=ssq[:, col:col + 1], scalar2=None, op0=ALU.mult))
                xin_free[b] = td
                PE.wait(td, psT_free[2 * b], psT_free[2 * b + 1])
                for kt in range(8):
                    tp = nc.tensor.transpose(psA[2 * b + kt // 4][:, (kt % 4) * 128:(kt % 4 + 1) * 128], xs[b][:, kt * 128:(kt + 1) * 128], ident[:])
                tp = PE.sig(tp)
                xs_free[b] = tp
                ACT.wait(tp, hT_free if tt == 0 else None)
                for kt in range(8):
                    ta = nc.scalar.activation(out=hT[:, kt, tt * 128:(tt + 1) * 128], in_=psA[2 * b + kt // 4][:, (kt % 4) * 128:(kt % 4 + 1) * 128],
                                              func=AF.Identity, scale=A1[:, seg, kt:kt + 1], bias=modT[:, kt, seg:seg + 1])
                ta = ACT.sig(ta)
                psT_free[2 * b] = ta
                psT_free[2 * b + 1] = ta
                t_h = ta
            PE.wait(t_h, t_wi)
            last_pe_read_h = None
            for half in range(2):
                tsl = slice(half * 512, (half + 1) * 512)
                gsl = slice(c * CH + half * 512, c * CH + (half + 1) * 512)
                evs = []
                for ti in range(33):
                    col0 = pt_cols[ti] if ti < 17 else 3072 + (ti - 17) * 128
                    pb_ = ti % 2
                    PE.wait(pp_free[pb_])
                    for kt in range(8):
                        mm = nc.tensor.matmul(psA[4 + pb_][:, :], lhsT=wi[:, kt, col0:col0 + 128], rhs=hT[:, kt, tsl], start=(kt == 0), stop=(kt == 7))
                    tm = PE.sig(mm)
                    last_pe_read_h = tm
                    if ti < 17:
                        if ti == 0:
                            DVE.wait(stq_free)
                        DVE.wait(tm)
                        te = DVE.sig(nc.vector.tensor_copy(out=stQ[0][:, ti, :], in_=psA[4 + pb_][:, :]))
                    else:
                        if ti == 17:
                            ACT.wait(stg_free)
                        ACT.wait(tm)
                        te = ACT.sig(nc.scalar.activation(out=stG[0][:, ti - 17, :], in_=psA[4 + pb_][:, :], func=AF.Sigmoid))
                    pp_free[pb_] = te
                    evs.append(te)
                SP.wait(evs[16])
                stq_free = d_st.sig(nc.sync.dma_start(out=PT.rearrange("(t p) n -> p t n", p=128)[:, :, gsl], in_=stQ[0][:, :, :]))
                SP.wait(evs[32])
                stg_free = d_st.sig(nc.sync.dma_start(out=GT.rearrange("(t p) n -> p t n", p=128)[:, :, gsl], in_=stG[0][:, :, :]))
            DVE.wait(stv_free)
            for i in range(8):
                PE.wait(pv_free)
                jobs = [(640, 128, psA[6][:, 0:128], lambda hk: hk[:, i * 128:(i + 1) * 128]),
                        (2304, 256, psA[6][:, 128:384], lambda hk: hk[:, i * 128:(i + 1) * 128]),
                        (2560, 256, psA[7][:, 0:256],
                         lambda hk: hk.rearrange("p (u r) -> p r u", r=4)[:, i // 2, (i % 2) * 128:(i % 2 + 1) * 128]),
                        (2816, 256, psA[7][0:64, 256:512], lambda hk: hk.rearrange("p (u r) -> p r u", r=16)[:, 2 * i, :]),
                        (2816, 256, psA[7][64:128, 256:512], lambda hk: hk.rearrange("p (u r) -> p r u", r=16)[:, 2 * i + 1, :])]
                for (c0, wd, o_, lf) in jobs:
                    for kt in range(8):
                        mm = nc.tensor.matmul(o_, lhsT=lf(hT[:, kt, :]), rhs=wi[:, kt, c0:c0 + wd], start=(kt == 0), stop=(kt == 7))
                tm = PE.sig(mm)
                last_pe_read_h = tm
                DVE.wait(tm)
                nc.vector.tensor_copy(out=stV[:, i, 0:384], in_=psA[6][:, 0:384])
                te = DVE.sig(nc.vector.tensor_copy(out=stV[:, i, 384:896], in_=psA[7][:, :]))
                pv_free = te
            SP.wait(te)
            stv_free = d_st.sig(nc.sync.dma_start(out=VV[c * CH:(c + 1) * CH, :].rearrange("(i p) f -> p i f", p=128), in_=stV[:, :, :]))
            hT_free = last_pe_read_h
        barrier()

    if debug == "proj":
        es.close()
        return nc

    ps_free = [None] * 8

    def bc_last(a, n):
        return bass.AP(tensor=a.tensor, offset=a.offset, ap=[list(a.ap[0]), list(a.ap[1]), [0, n]])

    with contextlib.ExitStack() as st:
        wpa = sb("wpa", [128, 4, D], BF16, st)
        wpb = sb("wpb", [128, 2, D], BF16, st)
        wo = sb("wo", [128, 8, D], BF16, st)
        d_w2 = dsem("w2")
        nc.gpsimd.dma_start(out=wpa[:], in_=w_pa.rearrange("(kt p) n -> p kt n", p=128)).then_inc(d_w2.sem, 16)
        nc.gpsimd.dma_start(out=wpb[:], in_=w_pb.rearrange("(kt p) n -> p kt n", p=128)).then_inc(d_w2.sem, 16)
        d_w2.cnt = 32
        t_w2 = d_w2.sig(nc.gpsimd.dma_start(out=wo[:], in_=w_o.rearrange("(kt p) n -> p kt n", p=128)))
        QA = sb("QA", [64, 8, CH], BF16, st)
        QB = sb("QB", [128, 6, CH], BF16, st)
        KAw = sb("KAw", [64, 2, 1280], BF16, st)
        KBw = [sb("KBw%d" % g, [128, 2, CH + 128 * d], BF16, st) for g, (w_, d) in enumerate(GROUPS)]
        VAw = sb("VAw", [128, 10, 128], BF16, st)
        VBw = [sb("VBw%d" % g, [128, n_, 256], BF16, st) for g, n_ in enumerate((18, 24, 48))]
        yaT = sb("yaT", [128, 4, CH], BF16, st)
        ybT = sb("ybT", [128, 2, CH], BF16, st)
        accN = sb("accN", [128, CH], F32, st)
        accD = sb("accD", [128, CH], F32, st)
        P0 = [sb("P0_%d" % i, [128, 384], BF16, st) for i in range(2)]
        Pt = [sb("Pt_%d" % i, [128, 384], BF16, st) for i in range(4)]
        rd = sb("rd", [128, 128], F32, st)
        Gt = sb("Gt", [128, 16, 256], BF16, st)
        mg = sb("mg", [128, 8, 256], BF16, st)
        t12 = sb("t12", [128, 2, 512], F32, st)
        G1bc = sb("G1bc", [128, D], F32, st)
        diag = sb("diag", [128, 128], F32, st)
        xio = sb("xio", [128, D], F32, st)
        d_ld = dsem("attld")
        d_g = dsem("gld")
        d_xi = dsem("xi")
        d_xo = dsem("xo")
        PTq = PT[0:512, :].rearrange("(h p) n -> p h n", p=64)
        PTka = PT[512:640, :].rearrange("(h p) n -> p h n", p=64)
        PTqb = PT[640:1408, :].rearrange("(t p) n -> p t n", p=128)
        GTv = GT.rearrange("(t p) n -> p t n", p=128)
        att_free = None
        y_free = None
        g_free = None
        mg_free = None
        xio_free = None
        t_g1 = None
        P0_free = [None, None]
        Pt_free = [None] * 4
        rd_free = None
        t12_free = [None, None]
        for c in range(NCH):
            seg = c // 2
            if c % 2 == 0:
                PE.wait(ps_free[6], ps_free[7])
                DVE.wait(xio_free)
                t_g1 = row_bcast(G1bc, 2, seg, diag, [psA[6], psA[7]])
                ps_free[6] = t_g1
                ps_free[7] = t_g1
            SP.wait(att_free)
            lt = []
            c0 = c * CH
            lt.append(d_ld.sig(nc.sync.dma_start(out=QA[:, :, :], in_=PTq[:, :, c0:c0 + CH])))
            lt.append(d_ld.sig(nc.sync.dma_start(out=QB[:, :, :], in_=PTqb[:, :, c0:c0 + CH])))
            lo, hi = max(0, c0 - 128), min(NT, c0 + CH + 128)
            lt.append(d_ld.sig(nc.sync.dma_start(out=KAw[:, :, lo - (c0 - 128):hi - (c0 - 128)], in_=PTka[:, :, lo:hi])))
            for g, (w_, d) in enumerate(GROUPS):
                H = 64 * d
                lo, hi = max(0, c0 - H), min(NT, c0 + CH + H)
                src = PT[1408 + 256 * g:1408 + 256 * (g + 1), :].rearrange("(t p) n -> p t n", p=128)
                lt.append(d_ld.sig(nc.sync.dma_start(out=KBw[g][:, :, lo - (c0 - H):hi - (c0 - H)], in_=src[:, :, lo:hi])))
            lo, hi = max(0, c0 - 128), min(NT, c0 + CH + 128)
            b0, b1 = (lo - (c0 - 128)) // 128, (hi - (c0 - 128)) // 128
            lt.append(d_ld.sig(nc.sync.dma_start(out=VAw[:, b0:b1, :], in_=VV[lo:hi, 0:128].rearrange("(b p) f -> p b f", p=128))))
            for hp in (0, 64):
                lo, hi = max(0, c0 - 64), min(NT, c0 + CH + 64)
                b0, b1 = (lo - (c0 - 64)) // 64, (hi - (c0 - 64)) // 64
                lt.append(d_ld.sig(nc.sync.dma_start(out=VBw[0][hp:hp + 64, b0:b1, :], in_=VV[lo:hi, 128:384].rearrange("(b p) f -> p b f", p=64))))
                lt.append(d_ld.sig(nc.sync.dma_start(out=VBw[1][hp:hp + 64, 0:16, :], in_=VV[c0:c0 + CH, 384:640].rearrange("(b p) f -> p b f", p=64))))
                if c > 0:
                    lt.append(d_ld.sig(nc.sync.dma_start(out=VBw[1][hp:hp + 64, 16:20, :],
                                                         in_=VV[c0 - CH:c0, 384:640].rearrange("(r b p) f -> p r b f", r=4, b=4, p=64)[:, :, 3, :])))
                if c < NCH - 1:
                    lt.append(d_ld.sig(nc.sync.dma_start(out=VBw[1][hp:hp + 64, 20:24, :],
                                                         in_=VV[c0 + CH:c0 + 2 * CH, 384:640].rearrange("(r b p) f -> p r b f", r=4, b=4, p=64)[:, :, 0, :])))
                for dc in (-1, 0, 1):
                    if 0 <= c + dc < NCH:
                        lt.append(d_ld.sig(nc.sync.dma_start(out=VBw[2][hp:hp + 64, 16 * (dc + 1):16 * (dc + 2), :],
                                                             in_=VV[c0 + dc * CH:c0 + (dc + 1) * CH, 640:896].rearrange("(r p) f -> p r f", p=64))))
            t_ld = lt[-1]
            PE.wait(t_ld, t_w2)

            items = []
            for h in range(8):
                kv = h // 4
                for i in range(8):
                    gb = 8 * c + i
                    dl = [dd for dd in (-1, 0, 1) if 0 <= gb + dd < 64]
                    items.append(dict(kind="A", h=h, kv=kv, i=i, dl=dl, cross=[(gb + dd) // 16 != gb // 16 for dd in dl], Hb=128))
            for p in range(2):
                for g, (w_, d) in enumerate(GROUPS):
                    nb = 16 // d
                    for r in range(d):
                        for b in range(nb):
                            ub = nb * c + b
                            dl = [dd for dd in (-1, 0, 1) if 0 <= ub + dd < 8 * nb]
                            items.append(dict(kind="B", p=p, g=g, d=d, nb=nb, r=r, b=b, dl=dl,
                                              cross=[(ub + dd) // (2 * nb) != ub // (2 * nb) for dd in dl], Hb=64,
                                              first=(r == 0 and b == 0), last_of_pair=(g == 2 and r == d - 1 and b == nb - 1)))
            N = len(items)
            acc_tok = None

            def vidx(it, dd):
                g, nb, b, r = it["g"], it["nb"], it["b"], it["r"]
                ub = nb * c + b + dd
                dc = ub // nb - c
                bp = ub % nb
                if g == 0:
                    return ub - 16 * c + 1
                if g == 1:
                    return 4 * r + bp if dc == 0 else (16 + r if dc < 0 else 20 + r)
                return 16 * (dc + 1) + r

            def stage1(n):
                it = items[n]
                Hb = it["Hb"]
                S = psA[n % 3]
                PE.wait(ps_free[n % 3])
                for dd in it["dl"]:
                    cs = slice((dd + 1) * Hb, (dd + 2) * Hb)
                    if it["kind"] == "A":
                        i = it["i"]
                        mm = nc.tensor.matmul(S[:, cs], lhsT=KAw[0:64, it["kv"], (i + 1 + dd) * 128:(i + 2 + dd) * 128],
                                              rhs=QA[0:64, it["h"], i * 128:(i + 1) * 128], start=True, stop=True)
                    else:
                        d, g, b, r, p = it["d"], it["g"], it["b"], it["r"], it["p"]
                        qoff = d * 64 * b + r
                        koff = d * 64 * (b + dd) + r + 64 * d
                        for hp in (0, 64):
                            mm = nc.tensor.matmul(S[hp:hp + 64, cs], lhsT=KBw[g][hp:hp + 64, p, koff:koff + 63 * d + 1:d],
                                                  rhs=QB[hp:hp + 64, 2 * g + p, qoff:qoff + 63 * d + 1:d], start=True, stop=True)
                tm = PE.sig(mm)
                lo, hi = (it["dl"][0] + 1) * Hb, (it["dl"][-1] + 2) * Hb
                ACT.wait(tm, P0_free[n % 2])
                ta = ACT.sig(nc.scalar.activation(out=P0[n % 2][:, lo:hi], in_=S[:, lo:hi], func=AF.Exp, scale=0.125))
                ps_free[n % 3] = ta
                POOL.wait(ta, Pt_free[n % 4])
                if it["kind"] == "A":
                    eb = lambda v, a, b_: EBA[:, it["h"], v, a:b_]
                else:
                    eb = lambda v, a, b_: EBB[:, 2 * it["g"] + it["p"], v, a:b_]
                if not any(it["cross"]):
                    td = nc.gpsimd.tensor_tensor(out=Pt[n % 4][:, lo:hi], in0=P0[n % 2][:, lo:hi], in1=eb(0, lo, hi), op=ALU.mult)
                else:
                    for dd, cr in zip(it["dl"], it["cross"]):
                        a, b_ = (dd + 1) * Hb, (dd + 2) * Hb
                        td = nc.gpsimd.tensor_tensor(out=Pt[n % 4][:, a:b_], in0=P0[n % 2][:, a:b_], in1=eb(1 if cr else 0, a, b_), op=ALU.mult)
                td = POOL.sig(td)
                P0_free[n % 2] = td
                it["tP"] = td

            def stage2(n):
                nonlocal att_free, rd_free, acc_tok, y_free
                it = items[n]
                Hb = it["Hb"]
                O = psA[3 + n % 2]
                PE.wait(it["tP"], ps_free[3 + n % 2])
                nd = len(it["dl"])
                if it["kind"] == "A":
                    pb_ = (it["h"] % 2) * 64
                    i = it["i"]
                    for k_, dd in enumerate(it["dl"]):
                        nc.tensor.matmul(O[pb_:pb_ + 64, 0:128], lhsT=VAw[:, i + 1 + dd, it["kv"] * 64:(it["kv"] + 1) * 64],
                                         rhs=Pt[n % 4][:, (dd + 1) * 128:(dd + 2) * 128], start=(k_ == 0), stop=(k_ == nd - 1))
                    for k_, dd in enumerate(it["dl"]):
                        mm = nc.tensor.matmul(O[pb_:pb_ + 64, 128:256], lhsT=ones_b[:, 0:64],
                                              rhs=Pt[n % 4][:, (dd + 1) * 128:(dd + 2) * 128], start=(k_ == 0), stop=(k_ == nd - 1))
                    tm = PE.sig(mm)
                    att_free = tm
                    Pt_free[n % 4] = tm
                    DVE.wait(tm, t_es, rd_free, y_free)
                    h = it["h"]
                    DVE.wait(DVE.sig(nc.vector.tensor_scalar(out=rd[pb_:pb_ + 64, :], in0=O[pb_:pb_ + 64, 128:256],
                                                             scalar1=es_bc[pb_:pb_ + 64, h:h + 1], scalar2=None, op0=ALU.add)))
                    DVE.wait(DVE.sig(nc.vector.reciprocal(out=rd[pb_:pb_ + 64, :], in_=rd[pb_:pb_ + 64, :])))
                    td = DVE.sig(nc.vector.tensor_tensor(out=yaT[pb_:pb_ + 64, h // 2, i * 128:(i + 1) * 128], in0=O[pb_:pb_ + 64, 0:128],
                                                         in1=rd[pb_:pb_ + 64, :], op=ALU.mult))
                    rd_free = td
                    ps_free[3 + n % 2] = td
                else:
                    d, g, b, r, p = it["d"], it["g"], it["b"], it["r"], it["p"]
                    for hp in (0, 64):
                        for k_, dd in enumerate(it["dl"]):
                            nc.tensor.matmul(O[hp:hp + 64, 0:64], lhsT=VBw[g][hp:hp + 64, vidx(it, dd), (2 * p + hp // 64) * 64:(2 * p + hp // 64 + 1) * 64],
                                             rhs=Pt[n % 4][hp:hp + 64, (dd + 1) * 64:(dd + 2) * 64], start=(k_ == 0), stop=(k_ == nd - 1))
                    for k_, dd in enumerate(it["dl"]):
                        mm = nc.tensor.matmul(O[:, 64:128], lhsT=ones_bd[:, :],
                                              rhs=Pt[n % 4][:, (dd + 1) * 64:(dd + 2) * 64], start=(k_ == 0), stop=(k_ == nd - 1))
                    tm = PE.sig(mm)
                    att_free = tm
                    Pt_free[n % 4] = tm
                    qoff = d * 64 * b + r
                    sl = slice(qoff, qoff + 63 * d + 1, d)
                    DVE.wait(tm)
                    if it["first"]:
                        DVE.wait(acc_tok, y_free)
                    if g == 0:
                        nc.vector.tensor_copy(out=accN[:, sl], in_=O[:, 0:64])
                        td = DVE.sig(nc.vector.tensor_copy(out=accD[:, sl], in_=O[:, 64:128]))
                    else:
                        nc.vector.tensor_tensor(out=accN[:, sl], in0=accN[:, sl], in1=O[:, 0:64], op=ALU.add)
                        td = DVE.sig(nc.vector.tensor_tensor(out=accD[:, sl], in0=accD[:, sl], in1=O[:, 64:128], op=ALU.add))
                    acc_tok = td
                    ps_free[3 + n % 2] = td
                    if it["last_of_pair"]:
                        DVE.wait(td)
                        DVE.wait(DVE.sig(nc.vector.reciprocal(out=accD[:, :], in_=accD[:, :])))
                        acc_tok = DVE.sig(nc.vector.tensor_tensor(out=ybT[:, p, :], in0=accN[:, :], in1=accD[:, :], op=ALU.mult))
                        DVE.wait(acc_tok)

            LAG = 3
            for n in range(N + LAG):
                if n < N:
                    stage1(n)
                if n >= LAG:
                    stage2(n - LAG)
            t_y = acc_tok

            for m in range(4):
                msl = slice(m * 256, (m + 1) * 256)
                SP.wait(g_free)
                tg = d_g.sig(nc.sync.dma_start(out=Gt[:, :, :], in_=GTv[:, :, c0 + m * 256:c0 + (m + 1) * 256]))
                PE.wait(t_y)
                for nt in range(8):
                    pm = psA[5] if nt % 2 == 0 else psA[2]
                    pi = 5 if nt % 2 == 0 else 2
                    PE.wait(ps_free[pi])
                    for kt in range(4):
                        nc.tensor.matmul(pm[:, 0:256], lhsT=wpa[:, kt, nt * 128:(nt + 1) * 128], rhs=yaT[:, kt, msl], start=(kt == 0), stop=(kt == 3))
                    for kt in range(2):
                        mm = nc.tensor.matmul(pm[:, 256:512], lhsT=wpb[:, kt, nt * 128:(nt + 1) * 128], rhs=ybT[:, kt, msl], start=(kt == 0), stop=(kt == 1))
                    tm = PE.sig(mm)
                    y_free = tm
                    DVE.wait(tm, tg, t12_free[nt % 2], mg_free if nt == 0 else None)
                    tq = t12[:, nt % 2, :]
                    nc.vector.tensor_tensor(out=tq[:, 0:256], in0=pm[:, 0:256], in1=Gt[:, nt, :], op=ALU.mult)
                    td = DVE.sig(nc.vector.tensor_tensor(out=tq[:, 256:512], in0=pm[:, 256:512], in1=Gt[:, 8 + nt, :], op=ALU.mult))
                    ps_free[pi] = td
                    DVE.wait(td)
                    td = DVE.sig(nc.vector.tensor_tensor(out=mg[:, nt, :], in0=tq[:, 0:256], in1=tq[:, 256:512], op=ALU.add))
                    t12_free[nt % 2] = td
                g_free = td
                t_mg = td
                for tt in range(2):
                    gt = c * 8 + m * 2 + tt
                    SP.wait(xio_free)
                    tx = d_xi.sig(nc.sync.dma_start(out=xio[:], in_=x_v[gt]))
                    PE.wait(t_mg, ps_free[6], ps_free[7])
                    for nh in range(2):
                        for kt in range(8):
                            mm = nc.tensor.matmul(psA[6 + nh][:, :], lhsT=mg[:, kt, tt * 128:(tt + 1) * 128], rhs=wo[:, kt, nh * 512:(nh + 1) * 512],
                                                  start=(kt == 0), stop=(kt == 7))
                    tm = PE.sig(mm)
                    mg_free = tm
                    DVE.wait(tm, tx, t_g1, t12_free[0], t12_free[1])
                    for nh in range(2):
                        hs_ = slice(nh * 512, (nh + 1) * 512)
                        DVE.wait(DVE.sig(nc.vector.tensor_tensor(out=t12[:, nh, :], in0=psA[6 + nh][:, :], in1=G1bc[:, hs_], op=ALU.mult)))
                        td = DVE.sig(nc.vector.tensor_tensor(out=xio[:, hs_], in0=xio[:, hs_], in1=t12[:, nh, :], op=ALU.add))
                    ps_free[6] = td
                    ps_free[7] = td
                    t12_free[0] = td
                    t12_free[1] = td
                    SP.wait(td)
                    xio_free = d_xo.sig(nc.sync.dma_start(out=X1_v[gt], in_=xio[:]))
        barrier()

    if debug == "att":
        es.close()
        return nc

    pst.close()
    ps_free = [None] * 8
    U32 = mybir.dt.uint32
    I32 = mybir.dt.int32

    def bc_mid(a, n):
        return bass.AP(tensor=a.tensor, offset=a.offset, ap=[list(a.ap[0]), [0, n], list(a.ap[1])])

    def row_bcast2(dst, colfn, diag, pss):
        last = None
        for kt in range(8):
            DVE.wait(last)
            td = DVE.sig(nc.vector.tensor_scalar(out=diag[:], in0=ident[:], scalar1=colfn(kt), scalar2=None, op0=ALU.mult))
            PE.wait(td, t_ones)
            last = PE.sig(nc.tensor.matmul(pss[kt // 4][:, (kt % 4) * 128:(kt % 4 + 1) * 128], lhsT=ones_f[:], rhs=diag[:],
                                           start=True, stop=True))
        DVE.wait(last)
        nc.vector.tensor_copy(out=dst[:, 0:512], in_=pss[0][:, :])
        return DVE.sig(nc.vector.tensor_copy(out=dst[:, 512:1024], in_=pss[1][:, :]))

    with contextlib.ExitStack() as st:
        psF = [st.enter_context(nc.psum_tensor("psF%d" % i, [128, 512], F32)) for i in range(6)]
        psB = [st.enter_context(nc.psum_tensor("psB%d" % i, [128, 1024], BF16)) for i in range(2)]
        cst = sb("cst", [128, NCST], F32, st)
        iota_e = cst[:, 0:64]
        tokid = cst[:, 64:128]
        pcol = cst[:, 128:129]
        iota_b = cst[:, 129:129 + NBLK]
        thr16 = cst[:, 129 + NBLK:129 + NBLK + 16]
        U_b = sb("U_b", [128, 128], BF16, st)
        ones128 = sb("ones128", [128, 128], BF16, st)
        id_b = sb("id_b", [128, 128], BF16, st)
        wr = sb("wr", [128, 8, NE], BF16, st)
        rb_bc = sb("rb_bc", [128, NE], F32, st)
        FNbc = sb("FNbc", [128, D], F32, st)
        G2all = sb("G2all", [128, 4, D], F32, st)
        diag = sb("diag2", [128, 128], F32, st)
        RK = sb("RK", [128, 64, 64], F32, st)
        E6f = sb("E6f", [128, 64, 8], F32, st)
        W6 = sb("W6", [128, 64, 8], F32, st)
        D6f = sb("D6f", [128, 64, 8], F32, st)
        D6i = sb("D6i", [128, 64, 8], I32, st)
        IDXW = sb("IDXW", [128, NBLK], I32, st)
        carry = sb("carry", [128, 64], F32, st)
        xin = [sb("mxin%d" % i, [128, D], F32, st) for i in range(2)]
        xs = [sb("mxs%d" % i, [128, D], F32, st) for i in range(2)]
        junk = sb("mjunk", [128, D], F32, st)
        ssq = sb("mssq", [128, 64], F32, st)
        d_m = dsem("mconst")
        nc.sync.dma_start(out=cst[:], in_=cst_d).then_inc(d_m.sem, 16)
        nc.sync.dma_start(out=rb_bc[:], in_=router_bias[0:1, :].partition_broadcast(128)).then_inc(d_m.sem, 16)
        nc.sync.dma_start(out=FNbc[:], in_=final_norm[0:1, :].partition_broadcast(128)).then_inc(d_m.sem, 16)
        nc.gpsimd.dma_start(out=U_b[:], in_=umat_d).then_inc(d_m.sem, 16)
        nc.gpsimd.dma_start(out=id_b[:], in_=ident_d).then_inc(d_m.sem, 16)
        d_m.cnt += 80
        t_mc = d_m.sig(nc.gpsimd.dma_start(out=wr[:], in_=w_router.rearrange("(kt p) n -> p kt n", p=128)))
        t_o128 = DVE.sig(nc.vector.memset(ones128[:], 1.0))
        t_c0 = DVE.sig(nc.vector.memset(carry[:], 0.0))
        for s_ in range(4):
            PE.wait(ps_free[4], ps_free[5])
            tg_ = row_bcast2(G2all[:, s_, :], lambda kt: modT[:, 40 + kt, s_:s_ + 1], diag, [psF[4], psF[5]])
            ps_free[4] = tg_
            ps_free[5] = tg_
        t_g2 = tg_
        d_x = [dsem("mx%d" % i) for i in range(2)]
        d_h2 = [dsem("mh%d" % i) for i in range(2)]
        xin_free = [None, None]
        xs_free = [None, None]

        with contextlib.ExitStack() as st1:
            A2bc = sb("A2bc", [128, D], F32, st1)
            B2bc = sb("B2bc", [128, D], F32, st1)
            tmpf = sb("tmpf", [128, D], F32, st1)
            h2tm = [sb("h2tm%d" % i, [128, D], BF16, st1) for i in range(2)]
            h2T = [sb("h2T%d" % i, [128, 8, 128], BF16, st1) for i in range(2)]
            rt = sb("rt", [128, 8, 64], F32, st1)
            r8 = sb("r8", [128, 8, 8], F32, st1)
            mk = sb("mk", [128, 64], BF16, st1)
            i8 = sb("i8", [128, 8], U32, st1)
            h2tm_free = [None, None]
            h2T_free = [None, None]
            t_ab = None
            t_h = None
            mk_free = None
            ch = lambda ins: DVE.wait(DVE.sig(ins))
            for gt in range(64):
                seg = gt // 16
                b = gt % 2
                if gt % 16 == 0:
                    PE.wait(ps_free[4], ps_free[5])
                    DVE.wait(t_h)
                    ta_ = row_bcast2(A2bc, lambda kt: A2[:, seg, kt:kt + 1], diag, [psF[4], psF[5]])
                    PE.wait(ta_)
                    t_ab = row_bcast2(B2bc, lambda kt: modT[:, 24 + kt, seg:seg + 1], diag, [psF[4], psF[5]])
                    ps_free[4] = t_ab
                    ps_free[5] = t_ab
                SP.wait(xin_free[b])
                tl = d_x[b].sig(nc.sync.dma_start(out=xin[b][:], in_=X1_v[gt]))
                ACT.wait(tl)
                col = gt
                ta = ACT.sig(nc.scalar.activation(out=junk[:], in_=xin[b][:], func=AF.Square, accum_out=ssq[:, col:col + 1]))
                ACT.wait(ta)
                ta = ACT.sig(nc.scalar.activation(out=ssq[:, col:col + 1], in_=ssq[:, col:col + 1], func=AF.Sqrt, scale=1.0 / D, bias=EPS))
                DVE.wait(ta, xs_free[b])
                ch(nc.vector.reciprocal(out=ssq[:, col:col + 1], in_=ssq[:, col:col + 1]))
                td = DVE.sig(nc.vector.tensor_scalar(out=xs[b][:], in0=xin[b][:], scalar1=ssq[:, col:col + 1], scalar2=None, op0=ALU.mult))
                xin_free[b] = td
                POOL.wait(td, t_ab, h2tm_free[b])
                tq_ = POOL.sig(nc.gpsimd.tensor_tensor(out=tmpf[:], in0=xs[b][:], in1=A2bc[:], op=ALU.mult))
                POOL.wait(tq_)
                th = POOL.sig(nc.gpsimd.tensor_tensor(out=h2tm[b][:], in0=tmpf[:], in1=B2bc[:], op=ALU.add))
                SP.wait(th)
                h2tm_free[b] = d_h2[b].sig(nc.sync.dma_start(out=H2[gt * 128:(gt + 1) * 128, :], in_=h2tm[b][:]))
                PE.wait(td, ps_free[2 * b], ps_free[2 * b + 1])
                for kt in range(8):
                    tp = nc.tensor.transpose(psF[2 * b + kt // 4][:, (kt % 4) * 128:(kt % 4 + 1) * 128], xs[b][:, kt * 128:(kt + 1) * 128], ident[:])
                tp = PE.sig(tp)
                xs_free[b] = [tp, th]
                ACT.wait(tp, h2T_free[b])
                for kt in range(8):
                    ta = nc.scalar.activation(out=h2T[b][:, kt, :], in_=psF[2 * b + kt // 4][:, (kt % 4) * 128:(kt % 4 + 1) * 128],
                                              func=AF.Identity, scale=A2[:, seg, kt:kt + 1], bias=modT[:, 24 + kt, seg:seg + 1])
                ta = ACT.sig(ta)
                ps_free[2 * b] = ta
                ps_free[2 * b + 1] = ta
                PE.wait(ta, t_mc, ps_free[4])
                for kt in range(8):
                    mm = nc.tensor.matmul(psF[4][:, 0:64], lhsT=h2T[b][:, kt, :], rhs=wr[:, kt, :], start=(kt == 0), stop=(kt == 7))
                tm = PE.sig(mm)
                h2T_free[b] = tm
                ACT.wait(tm)
                sc = rt[:, 0, :]
                sel = rt[:, 1, :]
                eq = rt[:, 2, :]
                sel2 = rt[:, 3, :]
                selm = rt[:, 4, :]
                em = rt[:, 5, :]
                wdn = rt[:, 6, :]
                dd = rt[:, 7, :]
                v3 = lambda a: a.rearrange("p (g e) -> p g e", g=8)
                DVE.wait(t_h)
                ta2 = ACT.sig(nc.scalar.activation(out=sc, in_=psF[4][:, 0:64], func=AF.Sigmoid))
                ps_free[4] = ta2
                DVE.wait(ta2, t_mc)
                ch(nc.vector.tensor_tensor(out=sel, in0=sc, in1=rb_bc[:, :], op=ALU.add))
                ch(nc.vector.tensor_reduce(out=r8[:, 0, :], in_=v3(sel), axis=AX.X, op=ALU.max))
                ch(nc.vector.tensor_tensor(out=v3(eq), in0=v3(sel), in1=bc_last(r8[:, 0, :], 8), op=ALU.is_equal))
                ch(nc.vector.scalar_tensor_tensor(out=sel2, in0=eq, scalar=-1e30, in1=sel, op0=ALU.mult, op1=ALU.add))
                ch(nc.vector.tensor_reduce(out=r8[:, 1, :], in_=v3(sel2), axis=AX.X, op=ALU.max))
                ch(nc.vector.tensor_tensor(out=r8[:, 2, :], in0=r8[:, 0, :], in1=r8[:, 1, :], op=ALU.add))
                ch(nc.vector.max(out=r8[:, 3, :], in_=r8[:, 2, :]))
                ch(nc.vector.tensor_scalar(out=r8[:, 4, :], in0=r8[:, 2, :], scalar1=r8[:, 3, 3:4], scalar2=None, op0=ALU.is_ge))
                ch(nc.vector.tensor_scalar(out=r8[:, 5, :], in0=r8[:, 4, :], scalar1=1e30, scalar2=-1e30, op0=ALU.mult, op1=ALU.add))
                ch(nc.vector.tensor_tensor(out=v3(selm), in0=v3(sel), in1=bc_last(r8[:, 4, :], 8), op=ALU.mult))
                ch(nc.vector.tensor_tensor(out=v3(selm), in0=v3(selm), in1=bc_last(r8[:, 5, :], 8), op=ALU.add))
                ch(nc.vector.max(out=r8[:, 6, :], in_=selm))
                ch(nc.vector.tensor_scalar(out=em, in0=selm, scalar1=r8[:, 6, 5:6], scalar2=None, op0=ALU.is_ge))
                DVE.wait(mk_free)
                tmk = DVE.sig(nc.vector.tensor_copy(out=mk[:], in_=em))
                ch(nc.vector.tensor_tensor(out=em, in0=em, in1=sc, op=ALU.mult))
                ch(nc.vector.tensor_reduce(out=r8[:, 7, 0:1], in_=em, axis=AX.X, op=ALU.add))
                ch(nc.vector.reciprocal(out=r8[:, 7, 0:1], in_=r8[:, 7, 0:1]))
                ch(nc.vector.tensor_scalar(out=wdn, in0=em, scalar1=r8[:, 7, 0:1], scalar2=2.5, op0=ALU.mult, op1=ALU.mult))
                ch(nc.vector.max(out=W6[:, gt, :], in_=wdn))
                ch(nc.vector.max_index(i8[:], W6[:, gt, :], wdn))
                ch(nc.vector.tensor_copy(out=E6f[:, gt, :], in_=i8[:]))
                PE.wait(tmk, ps_free[5], t_o128, t_mc)
                nc.tensor.matmul(psF[5][:, 0:64], lhsT=U_b[:, :], rhs=mk[:, :], start=True, stop=True)
                tm2 = PE.sig(nc.tensor.matmul(psF[5][:, 64:128], lhsT=ones128[:, :], rhs=mk[:, :], start=True, stop=True))
                mk_free = tm2
                DVE.wait(tm2, t_c0)
                ch(nc.vector.tensor_tensor(out=RK[:, gt, :], in0=psF[5][:, 0:64], in1=carry[:, :], op=ALU.add))
                t_h = DVE.sig(nc.vector.tensor_tensor(out=carry[:, :], in0=psF[5][:, 64:128], in1=carry[:, :], op=ALU.add))
                ps_free[5] = t_h
                DVE.wait(t_h)
            barrier()
        if debug == "m1":
            return nc

        with contextlib.ExitStack() as st1:
            big = sb("big", [128, NBR * 64], F32, st1)
            nbk = sb("nbk", [128, 64], F32, st1)
            bend = sb("bend", [128, 64], F32, st1)
            pstart = sb("pstart", [128, 64], F32, st1)
            ones64 = sb("ones64", [128, 64], F32, st1)
            BEf = sb("BEf", [128, NBLK], F32, st1)
            PAY = sb("PAY", [128, 64, 6, 2], F32, st1)
            fill = sb("fill", [128, NBR * MB // 128, 2], F32, st1)
            SH = sb("SH", [128, 64, 2], F32, st1)
            zrow = sb("zrow", [1, D], BF16, st1)
            j64 = sb("j64", [128, 64], F32, st1)
            d_i = dsem("minit")
            ch = lambda ins: DVE.wait(DVE.sig(ins))
            ch(nc.vector.memset(fill[:, :, 0:1], float(NT)))
            ch(nc.vector.memset(fill[:, :, 1:2], 0.0))
            ch(nc.vector.memset(zrow[:], 0.0))
            ch(nc.vector.memset(SH[:, :, 1:2], 1.0))
            ch(nc.vector.tensor_copy(out=SH[:, :, 0], in_=tokid))
            ch(nc.vector.memset(ones64[:], 1.0))
            SP.wait((DVE, DVE.cnt))
            nc.sync.dma_start(out=ROWTW[0:NBR * MB, :].rearrange("(p j) c -> p j c", p=128), in_=fill[:]).then_inc(d_i.sem, 16)
            nc.sync.dma_start(out=ROWTW[NBR * MB:RROWS, :].rearrange("(j p) c -> p j c", p=128), in_=SH[:]).then_inc(d_i.sem, 16)
            d_i.cnt += 32
            t_init = d_i.sig(nc.sync.dma_start(out=H2[NT:NT + 1, :], in_=zrow[:]))
            big3 = big[:, 0:64 * 16].rearrange("p (e j) -> p e j", j=16)
            ch(nc.vector.tensor_tensor(out=big3, in0=bc_last(carry[:, :], 16), in1=bc_mid(thr16, 64), op=ALU.is_gt))
            ch(nc.vector.tensor_reduce(out=nbk[:], in_=big3, axis=AX.X, op=ALU.add))
            ch(nc.vector.tensor_tensor_scan(out=bend[:], data0=ones64[:], data1=nbk[:], initial=0.0, op0=ALU.mult, op1=ALU.add))
            ch(nc.vector.tensor_tensor(out=pstart[:], in0=bend[:], in1=nbk[:], op=ALU.subtract))
            ch(nc.vector.tensor_scalar(out=pstart[:], in0=pstart[:], scalar1=float(MB), scalar2=None, op0=ALU.mult))
            bigb = big[:, :].rearrange("p (i e) -> p i e", e=64)
            ch(nc.vector.tensor_tensor(out=bigb, in0=bc_last(iota_b[:, 0:NBR], 64), in1=bc_mid(bend[:, :], NBR), op=ALU.is_ge))
            ch(nc.vector.tensor_reduce(out=BEf[:, 0:NBR], in_=bigb, axis=AX.X, op=ALU.add))
            ch(nc.vector.tensor_scalar(out=big[:, 0:NBR], in0=BEf[:, 0:NBR], scalar1=64.0, scalar2=None, op0=ALU.is_ge))
            ch(nc.vector.tensor_tensor(out=BEf[:, 0:NBR], in0=BEf[:, 0:NBR], in1=big[:, 0:NBR], op=ALU.add))
            ch(nc.vector.memset(BEf[:, NBR:NBLK], 64.0))
            ch(nc.vector.tensor_scalar(out=BEf[:, :], in0=BEf[:, :], scalar1=128.0, scalar2=pcol, op0=ALU.mult, op1=ALU.add))
            ch(nc.vector.tensor_copy(out=IDXW[:, :], in_=BEf[:, :]))
            d_sc = dsem("scat")
            POOL.wait(t_init)
            for gt in range(64):
                ch(nc.vector.tensor_tensor(out=RK[:, gt, :], in0=RK[:, gt, :], in1=pstart[:, :], op=ALU.add))
                for k in range(6):
                    stt_ = nc.vector.scalar_tensor_tensor(out=big[:, k * 64:(k + 1) * 64], in0=iota_e, scalar=E6f[:, gt, k:k + 1], in1=RK[:, gt, :],
                                                          op0=ALU.is_equal, op1=ALU.mult, accum_out=D6f[:, gt, k:k + 1])
                DVE.wait(DVE.sig(stt_))
                for k in range(6):
                    nc.vector.tensor_copy(out=PAY[:, gt, k, 0:1], in_=tokid[:, gt:gt + 1])
                ch(nc.vector.tensor_copy(out=PAY[:, gt, :, 1], in_=W6[:, gt, 0:6]))
                tdi = DVE.sig(nc.vector.tensor_copy(out=D6i[:, gt, 0:6], in_=D6f[:, gt, 0:6]))
                DVE.wait(tdi)
                POOL.wait(tdi)
                for k in range(6):
                    t_sc = d_sc.sig(nc.gpsimd.indirect_dma_start(
                        out=ROWTW[:, :], out_offset=bass.IndirectOffsetOnAxis(ap=D6i[:, gt, k:k + 1], axis=0),
                        in_=PAY[:, gt, k, :], in_offset=None))
            barrier()
        if debug == "m1b":
            return nc

        with contextlib.ExitStack() as st1:
            NS = 3
            wg = [sb("wg%d" % i, [128, 2048], BF16, st1) for i in range(NS)]
            wu = [sb("wu%d" % i, [128, 2048], BF16, st1) for i in range(NS)]
            wd = [sb("wd%d" % i, [128, 2048], BF16, st1) for i in range(NS)]
            xg = [sb("xg%d" % i, [128, 4, D], BF16, st1) for i in range(NS)]
            rtw = [sb("rtw%d" % i, [128, 4, 2], F32, st1) for i in range(NS)]
            tki = [sb("tki%d" % i, [128, 4], I32, st1) for i in range(NS)]
            xgT = [sb("xgT%d" % i, [128, 8, MB], BF16, st1) for i in range(2)]
            hm = [sb("hm%d" % i, [128, 2, MB], BF16, st1) for i in range(2)]
            sg = [sb("sg%d" % i, [128, MB], F32, st1) for i in range(2)]
            osb = [sb("osb%d" % i, [128, 4, D], BF16, st1) for i in range(2)]
            d_rt = [dsem("rtw%d" % i) for i in range(NS)]
            d_we = [dsem("we%d" % i) for i in range(NS)]
            d_xg = [dsem("xg%d" % i) for i in range(NS)]
            d_o = [dsem("osb%d" % i) for i in range(2)]
            we_free = [None] * NS
            xg_free = [None] * NS
            rtw_free = [None] * NS
            tki_free = [None] * NS
            t_rtw = [None] * NS
            t_we = [None] * NS
            t_xg = [None] * NS
            xgT_free = [None, None]
            hm_free = [None, None]
            sg_free = [None, None]
            osb_free = [None, None]
            t_xgT = [None, None]
            psB_free = [None, None]
            ev = 0
            bnd_reg = nc.gpsimd.to_reg(65 * 128 - 1)

            def loads(i):
                s3 = i % NS
                SP.wait(rtw_free[s3])
                t_rtw[s3] = d_rt[s3].sig(nc.sync.dma_start(out=rtw[s3][:], in_=ROWTW[i * MB:(i + 1) * MB, :].rearrange("(j p) c -> p j c", p=128)))
                DVE.wait(t_rtw[s3], tki_free[s3])
                tk = DVE.sig(nc.vector.tensor_copy(out=tki[s3][:], in_=rtw[s3][:, :, 0]))
                POOL.wait(xg_free[s3], tk)
                for j in range(4):
                    tx = d_xg[s3].sig(nc.gpsimd.indirect_dma_start(out=xg[s3][:, j, :], out_offset=None, in_=H2[:, :],
                                                                   in_offset=bass.IndirectOffsetOnAxis(ap=tki[s3][:, j:j + 1], axis=0)))
                tki_free[s3] = tx
                t_xg[s3] = tx
                POOL.wait(we_free[s3])
                for m_, wt_ in enumerate((wg, wu, wd)):
                    t_we[s3] = d_we[s3].sig(nc.gpsimd.indirect_dma_start(
                        out=wt_[s3][:, :], out_offset=None, in_=WB[m_][:, :],
                        in_offset=bass.IndirectOffsetOnAxis(ap=IDXW[:, i:i + 1], axis=0), bounds_check=bnd_reg, oob_is_err=False))

            def transposes(i):
                nonlocal ev
                s2 = i % 2
                s3 = i % NS
                PE.wait(t_xg[s3], xgT_free[s2])
                last = []
                for kt in range(8):
                    pb_ = kt % 2
                    PE.wait(psB_free[pb_])
                    for j in range(4):
                        tp = nc.tensor.transpose(psB[pb_][:, j * 128:(j + 1) * 128], xg[s3][:, j, kt * 128:(kt + 1) * 128], id_b[:])
                    tp = PE.sig(tp)
                    if ev % 2 == 0:
                        ACT.wait(tp)
                        te = ACT.sig(nc.scalar.copy(out=xgT[s2][:, kt, :], in_=psB[pb_][:, 0:MB]))
                    else:
                        DVE.wait(tp)
                        te = DVE.sig(nc.vector.tensor_copy(out=xgT[s2][:, kt, :], in_=psB[pb_][:, 0:MB]))
                    ev += 1
                    psB_free[pb_] = te
                    last = (last + [te])[-2:]
                xg_free[s3] = tp
                t_xgT[s2] = last

            for q_ in range(NS):
                for wt_ in (wg, wu, wd):
                    POOL.sig(nc.gpsimd.memset(wt_[q_][:], 0.0))
            we_free = [(POOL, POOL.cnt)] * NS
            for i in range(min(NS, NBLK)):
                loads(i)
            PE.wait(t_mc)
            transposes(0)
            for i in range(NBLK):
                s2 = i % 2
                s3 = i % NS
                PE.wait(t_xgT[s2], t_we[s3])
                for ft in range(2):
                    PE.wait(ps_free[ft * 2], ps_free[ft * 2 + 1])
                    for kt in range(8):
                        nc.tensor.matmul(psF[ft * 2][:, :], lhsT=wg[s3][:, kt * 256 + ft * 128:kt * 256 + (ft + 1) * 128], rhs=xgT[s2][:, kt, :],
                                         start=(kt == 0), stop=(kt == 7))
                    for kt in range(8):
                        mm = nc.tensor.matmul(psF[ft * 2 + 1][:, :], lhsT=wu[s3][:, kt * 256 + ft * 128:kt * 256 + (ft + 1) * 128], rhs=xgT[s2][:, kt, :],
                                              start=(kt == 0), stop=(kt == 7))
                    tm = PE.sig(mm)
                    ACT.wait(tm, sg_free[ft])
                    ta3 = ACT.sig(nc.scalar.activation(out=sg[ft][:], in_=psF[ft * 2][:, :], func=AF.Silu))
                    DVE.wait(ta3, hm_free[s2] if ft == 0 else None)
                    td = DVE.sig(nc.vector.tensor_tensor(out=hm[s2][:, ft, :], in0=psF[ft * 2 + 1][:, :], in1=sg[ft][:], op=ALU.mult))
                    sg_free[ft] = td
                    ps_free[ft * 2] = td
                    ps_free[ft * 2 + 1] = td
                xgT_free[s2] = tm
                t_hm = td
                if i + 1 < NBLK:
                    transposes(i + 1)
                PE.wait(t_hm)
                ACT.wait(osb_free[s2])
                DVE.wait(osb_free[s2])
                evs = []
                for j in range(4):
                    for nh in range(2):
                        pi = 4 + (j * 2 + nh) % 2
                        PE.wait(ps_free[pi])
                        for ft in range(2):
                            mm = nc.tensor.matmul(psF[pi][:, :], lhsT=hm[s2][:, ft, j * 128:(j + 1) * 128],
                                                  rhs=wd[s3][:, ft * 1024 + nh * 512:ft * 1024 + (nh + 1) * 512], start=(ft == 0), stop=(ft == 1))
                        tm = PE.sig(mm)
                        o_ = osb[s2][:, j, nh * 512:(nh + 1) * 512]
                        if nh == 0:
                            ACT.wait(tm, t_rtw[s3])
                            te = ACT.sig(nc.scalar.activation(out=o_, in_=psF[pi][:, :], func=AF.Copy, scale=rtw[s3][:, j, 1:2]))
                        else:
                            DVE.wait(tm, t_rtw[s3])
                            te = DVE.sig(nc.vector.tensor_scalar(out=o_, in0=psF[pi][:, :], scalar1=rtw[s3][:, j, 1:2], scalar2=None, op0=ALU.mult))
                        ps_free[pi] = te
                        evs.append(te)
                hm_free[s2] = tm
                we_free[s3] = tm
                rtw_free[s3] = evs[-2:]
                SP.wait(evs[-1], evs[-2])
                osb_free[s2] = d_o[s2].sig(nc.sync.dma_start(out=OUTR[i * MB:(i + 1) * MB, :].rearrange("(j p) n -> p j n", p=128), in_=osb[s2][:]))
                if i + NS < NBLK:
                    loads(i + NS)
            barrier()
        if debug == "m2":
            return nc

        with contextlib.ExitStack() as st1:
            gbuf = [sb("gbuf%d" % i, [128, 7, D], BF16, st1) for i in range(2)]
            d_g = [dsem("gb%d" % i) for i in range(2)]
            d_y = [dsem("my%d" % i) for i in range(2)]
            gb_free = [None, None]
            ch = lambda ins: DVE.wait(DVE.sig(ins))
            for gt in range(64):
                seg = gt // 16
                b = gt % 2
                POOL.wait(gb_free[b])
                for k in range(6):
                    nc.gpsimd.indirect_dma_start(out=gbuf[b][:, k, :], out_offset=None, in_=OUTR[:, :],
                                                 in_offset=bass.IndirectOffsetOnAxis(ap=D6i[:, gt, k:k + 1], axis=0)).then_inc(d_g[b].sem, 16)
                d_g[b].cnt += 96
                SP.wait(gb_free[b], xin_free[b])
                nc.sync.dma_start(out=gbuf[b][:, 6, :], in_=OUTR[NBR * MB + gt * 128:NBR * MB + (gt + 1) * 128, :]).then_inc(d_g[b].sem, 16)
                d_g[b].cnt += 16
                tl = d_g[b].sig(nc.sync.dma_start(out=xin[b][:], in_=X1_v[gt]))
                PE.wait(tl, ps_free[2 * b], ps_free[2 * b + 1])
                for nh in range(2):
                    for k in range(7):
                        mm = nc.tensor.matmul(psF[2 * b + nh][:, :], lhsT=id_b[:, :], rhs=gbuf[b][:, k, nh * 512:(nh + 1) * 512],
                                              start=(k == 0), stop=(k == 6))
                tm = PE.sig(mm)
                gb_free[b] = tm
                DVE.wait(tm, xs_free[b], t_g2)
                a_ = xs[b]
                for nh in range(2):
                    hs_ = slice(nh * 512, (nh + 1) * 512)
                    ch(nc.vector.tensor_tensor(out=a_[:, hs_], in0=psF[2 * b + nh][:, :], in1=G2all[:, seg, hs_], op=ALU.mult))
                ps_free[2 * b] = (DVE, DVE.cnt)
                ps_free[2 * b + 1] = (DVE, DVE.cnt)
                td = DVE.sig(nc.vector.tensor_tensor(out=a_[:], in0=a_[:], in1=xin[b][:], op=ALU.add))
                xin_free[b] = td
                ACT.wait(td)
                col = gt
                ta = ACT.sig(nc.scalar.activation(out=junk[:], in_=a_[:], func=AF.Square, accum_out=ssq[:, col:col + 1]))
                ACT.wait(ta)
                ta = ACT.sig(nc.scalar.activation(out=ssq[:, col:col + 1], in_=ssq[:, col:col + 1], func=AF.Sqrt, scale=1.0 / D, bias=EPS))
                DVE.wait(ta)
                ch(nc.vector.reciprocal(out=ssq[:, col:col + 1], in_=ssq[:, col:col + 1]))
                td = DVE.sig(nc.vector.scalar_tensor_tensor(out=a_[:], in0=a_[:], scalar=ssq[:, col:col + 1], in1=FNbc[:, :], op0=ALU.mult, op1=ALU.mult))
                SP.wait(td)
                xs_free[b] = d_y[b].sig(nc.sync.dma_start(out=y_v[gt], in_=a_[:]))
            barrier()
    es.close()
    return nc


def _rel_bucket(rel):
    half = 16
    max_exact = 8
    n = np.abs(rel)
    large = max_exact + (np.log(np.maximum(n, 1) / max_exact) / math.log(1024 / max_exact) * (half - max_exact)).astype(np.int32)
    large = np.minimum(large, half - 1)
    return ((rel > 0).astype(np.int32) * half + np.where(n < max_exact, n, large)).astype(np.int32)


def _onehot_tables():
    oh = np.zeros((32, 4, 512), np.float32)
    for kind, (Hb, d) in enumerate(((128, 1), (64, 1), (64, 4), (64, 16))):
        Rmax = 2 * Hb - 1
        for i in range(4 * Hb - 1):
            rel = Rmax - i
            if abs(rel) <= Hb:
                oh[int(_rel_bucket(np.array([rel * d]))[0]), kind, i] = 1.0
    return oh


def _lay_gu(w, wsh):
    a = np.concatenate([w, wsh[None]], axis=0)
    a = a.reshape(65, 8, 128, 256).transpose(0, 2, 1, 3)
    return np.ascontiguousarray(a.reshape(65 * 128, 2048))


def _lay_d(w, wsh):
    a = np.concatenate([w, wsh[None]], axis=0)
    a = a.reshape(65, 2, 128, 1024).transpose(0, 2, 1, 3)
    return np.ascontiguousarray(a.reshape(65 * 128, 2048))


def _consts():
    c = np.zeros((128, NCST), np.float32)
    p = np.arange(128, dtype=np.float32)[:, None]
    c[:, 0:64] = np.arange(64, dtype=np.float32)[None, :]
    c[:, 64:128] = np.arange(64, dtype=np.float32)[None, :] * 128 + p
    c[:, 128:129] = p
    c[:, 129:129 + NBLK] = np.arange(NBLK, dtype=np.float32)[None, :]
    c[:, 129 + NBLK:] = np.arange(16, dtype=np.float32)[None, :] * MB
    return c


def make_in_maps(inp):
    f = lambda a: np.ascontiguousarray(np.asarray(a, dtype=np.float32))
    xp = f(inp["x_prompt"]); xs_ = f(inp["x_sample"]); cp = f(inp["c_prompt"]); cs = f(inp["c_sample"])
    common = {
        "rel_bias": f(inp["rel_bias"]), "sink": f(inp["sink"]).reshape(1, 8),
        "w_ada": f(inp["w_ada"])[0], "b_adaT": f(f(inp["b_ada"])[0].reshape(48, 128).T),
        "norm1T": f(f(inp["norm1"])[0].reshape(8, 128).T), "norm2T": f(f(inp["norm2"])[0].reshape(8, 128).T),
        "final_norm": f(inp["final_norm"]).reshape(1, D),
        "w_in": f(inp["w_in"])[0], "w_pa": f(inp["w_pa"])[0], "w_pb": f(inp["w_pb"])[0], "w_o": f(inp["w_o"])[0],
        "w_router": f(inp["w_router"])[0], "router_bias": f(inp["router_bias"]).reshape(1, NE),
        "wg2": _lay_gu(f(inp["w_gate"])[0], f(inp["ws_gate"])[0]), "wu2": _lay_gu(f(inp["w_up"])[0], f(inp["ws_up"])[0]),
        "wd2": _lay_d(f(inp["w_down"])[0], f(inp["ws_down"])[0]),
        "cst": _consts(), "umat": np.ascontiguousarray(np.triu(np.ones((128, 128), np.float32), 1)),
        "ident": np.eye(128, dtype=np.float32), "jmat": np.ascontiguousarray(np.eye(128, dtype=np.float32)[::-1]),
        "oh": _onehot_tables(),
    }
    maps = []
    for core in range(8):
        if core < 4:
            xc = xp[core]
            cc = np.repeat(cp[core:core + 1], 4, axis=0)
            cn = 1.0
        else:
            k = core - 4
            xc = xs_[4 * k:4 * k + 4].reshape(NT, D)
            cc = cs[4 * k:4 * k + 4]
            cn = 0.0
        m = dict(common)
        m["x"] = f(xc)
        m["cT"] = f(cc.reshape(4, 8, 128).transpose(2, 1, 0))
        m["conn"] = np.full((128, 1), cn, np.float32)
        maps.append(m)
    return maps


_NC_CACHE = {}


def kernel(**inputs):
    if "nc" not in _NC_CACHE:
        _NC_CACHE["nc"] = build()
    nc = _NC_CACHE["nc"]
    maps = make_in_maps(inputs)
    res = run_bass_kernel_spmd(nc, maps, core_ids=list(range(8)))
    ys = [np.asarray(r["y"], dtype=np.float32) for r in res.results]
    y_prompt = np.stack(ys[0:4], axis=0)
    y_sample = np.concatenate([ys[4 + k].reshape(4, 2048, D) for k in range(4)], axis=0)
    return (y_prompt, y_sample)
```

```python
import contextlib
import math
import numpy as np
import concourse.bass as bass
import concourse.mybir as mybir
from concourse.bass_utils import run_bass_kernel_spmd

F32 = mybir.dt.float32
BF16 = mybir.dt.bfloat16
AF = mybir.ActivationFunctionType
ALU = mybir.AluOpType
AX = mybir.AxisListType

NT = 8192
D = 1024
CH = 1024
NCH = NT // CH
EPS = 1e-6
NE = 64
MB = 512
NBR = NT * 6 // MB + NE
NBS = NT // MB
NBLK = NBR + NBS
RROWS = NBLK * MB
NCST = 64 + 64 + 1 + NBLK + 16
GROUPS = ((128, 1), (512, 4), (2048, 16))


def _flat(toks):
    for t in toks:
        if t is None:
            continue
        if isinstance(t, (list, tuple)) and not (len(t) == 2 and isinstance(t[1], int)):
            yield from _flat(t)
        else:
            yield t


class Eng:
    def __init__(self, nc, eng, name, es):
        self.eng = eng
        self.name = name
        self.sem = es.enter_context(nc.semaphore("s_" + name))
        self.cnt = 0
        self.seen = {}

    def wait(self, *toks):
        for p, v in _flat(toks):
            if self.seen.get(id(p), 0) >= v:
                continue
            self.seen[id(p)] = v
            self.eng.wait_ge(p.sem, v)

    def sig(self, instr):
        self.cnt += 1
        instr.then_inc(self.sem, 1)
        return (self, self.cnt)


class DSem:
    def __init__(self, nc, name, es):
        self.sem = es.enter_context(nc.semaphore("d_" + name))
        self.cnt = 0

    def sig(self, instr):
        self.cnt += 16
        instr.then_inc(self.sem, 16)
        return (self, self.cnt)


def build(debug=None):
    nc = bass.Bass("TRN2", target_bir_lowering=False)
    es = contextlib.ExitStack()

    def din(name, shape, dt=F32):
        return nc.dram_tensor(name, list(shape), dt, kind="ExternalInput").ap()

    x = din("x", [NT, D])
    cT = din("cT", [128, 8, 4])
    conn = din("conn", [128, 1])
    rel_bias = din("rel_bias", [32, 20])
    sink = din("sink", [1, 8])
    w_ada = din("w_ada", [D, 6 * D])
    b_adaT = din("b_adaT", [128, 48])
    norm1T = din("norm1T", [128, 8])
    norm2T = din("norm2T", [128, 8])
    final_norm = din("final_norm", [1, D])
    w_in = din("w_in", [D, 5120])
    w_pa = din("w_pa", [512, D])
    w_pb = din("w_pb", [256, D])
    w_o = din("w_o", [D, D])
    w_router = din("w_router", [D, NE])
    router_bias = din("router_bias", [1, NE])
    wg2 = din("wg2", [65 * 128, 2048])
    wu2 = din("wu2", [65 * 128, 2048])
    wd2 = din("wd2", [65 * 128, 2048])
    cst_d = din("cst", [128, NCST])
    umat_d = din("umat", [128, 128])
    ident_d = din("ident", [128, 128])
    jmat_d = din("jmat", [128, 128])
    oh_d = din("oh", [32, 4, 512])
    y = nc.dram_tensor("y", [NT, D], F32, kind="ExternalOutput").ap()

    skind = "ExternalOutput" if debug else "Internal"
    PT = nc.dram_tensor("PT", [2176, NT], BF16, kind=skind).ap()
    GT = nc.dram_tensor("GT", [2048, NT], BF16, kind=skind).ap()
    VV = nc.dram_tensor("VV", [NT, 896], BF16, kind=skind).ap()
    X1 = nc.dram_tensor("X1", [NT, D], F32, kind=skind).ap()
    TBL = nc.dram_tensor("TBL", [20, 512], F32, kind="Internal").ap()
    H2 = nc.dram_tensor("H2", [NT + 1, D], BF16, kind="Internal").ap()
    ROWTW = nc.dram_tensor("ROWTW", [RROWS, 2], F32, kind="Internal").ap()
    OUTR = nc.dram_tensor("OUTR", [RROWS, D], BF16, kind="Internal").ap()
    WB = [nc.dram_tensor("WB%d" % i, [65 * 128, 2048], BF16, kind="Internal").ap() for i in range(3)]

    PE = Eng(nc, nc.tensor, "pe", es)
    ACT = Eng(nc, nc.scalar, "act", es)
    DVE = Eng(nc, nc.vector, "dve", es)
    POOL = Eng(nc, nc.gpsimd, "pool", es)
    SP = Eng(nc, nc.sync, "sp", es)
    ENGS = [PE, ACT, DVE, POOL, SP]

    def sb(name, shape, dt, st=None):
        return (st or es).enter_context(nc.sbuf_tensor("sb_" + name, list(shape), dt))

    pst = contextlib.ExitStack()

    def ps(name, st=None):
        return (st or pst).enter_context(nc.psum_tensor(name, [128, 512], F32))

    all_dsems = []

    def dsem(name):
        d = DSem(nc, name, es)
        all_dsems.append(d)
        return d

    def barrier():
        DVE.sig(nc.vector.memset(bar_t[:, 0:1], 0.0))
        ACT.wait(t_ones)
        ACT.sig(nc.scalar.activation(out=bar_t[:, 1:2], in_=ones_f[:, 0:1], func=AF.Copy))
        POOL.sig(nc.gpsimd.memset(bar_t[:, 2:3], 0.0))
        toks = [(e, e.cnt) for e in ENGS if e.cnt > 0] + [(d, d.cnt) for d in all_dsems if d.cnt > 0]
        for e in ENGS:
            e.wait(*[t for t in toks if t[0] is not e])

    bar_t = sb("bar_t", [128, 4], F32)
    ident = sb("ident", [128, 128], F32)
    jmat = sb("jmat", [128, 128], F32)
    ones_f = sb("ones_f", [128, 128], F32)
    ones_b = sb("ones_b", [128, 64], BF16)
    ones_bd = sb("ones_bd", [128, 128], BF16)
    connc = sb("connc", [128, 1], F32)
    modT = sb("modT", [128, 48, 4], F32)
    A1 = sb("A1", [128, 4, 8], F32)
    A2 = sb("A2", [128, 4, 8], F32)
    n1T = sb("n1T", [128, 8], F32)
    n2T = sb("n2T", [128, 8], F32)
    badaT = sb("badaT", [128, 48], F32)
    scT = sb("scT", [128, 8, 4], F32)
    es_bc = sb("es_bc", [128, 8], F32)
    EBA = sb("EBA", [128, 8, 2, 384], BF16)
    EBB = sb("EBB", [128, 6, 2, 192], BF16)
    psA = [ps("psA%d" % i) for i in range(8)]

    d_c = dsem("const")
    toks = []
    for dst, src in ((ident, ident_d), (jmat, jmat_d), (connc, conn), (n1T, norm1T), (n2T, norm2T),
                     (badaT, b_adaT), (scT, cT)):
        toks.append(d_c.sig(nc.sync.dma_start(out=dst[:], in_=src)))
    toks.append(d_c.sig(nc.sync.dma_start(out=es_bc[:], in_=sink[0:1, :].partition_broadcast(128))))
    t_const = toks[-1]
    t_ones = DVE.sig(nc.vector.memset(ones_f[:], 1.0))
    t_onesb = DVE.sig(nc.vector.memset(ones_b[:], 1.0))
    DVE.wait(DVE.sig(nc.vector.memset(ones_bd[:], 0.0)))
    DVE.wait(DVE.sig(nc.vector.memset(ones_bd[0:64, 0:64], 1.0)))
    t_onesb = DVE.sig(nc.vector.memset(ones_bd[64:128, 64:128], 1.0))
    ACT.wait(t_const)
    t_sc = ACT.sig(nc.scalar.activation(out=scT[:], in_=scT[:], func=AF.Silu))
    t_es = ACT.sig(nc.scalar.activation(out=es_bc[:], in_=es_bc[:], func=AF.Exp))

    with contextlib.ExitStack() as st:
        wst = [sb("wada%d" % i, [128, 8, 512], F32, st) for i in range(2)]
        d_w = [dsem("wada%d" % i) for i in range(2)]
        w_ada_v = w_ada.rearrange("(kt p) n -> p kt n", p=128)
        rel_t = [None, None]
        PE.wait(t_sc)
        ev_t = [None] * 8
        for blk in range(12):
            s = blk % 2
            SP.wait(rel_t[s])
            tl = d_w[s].sig(nc.sync.dma_start(out=wst[s][:], in_=w_ada_v[:, :, blk * 512:(blk + 1) * 512]))
            PE.wait(tl, ev_t[blk % 8])
            pt = psA[blk % 8]
            for ft in range(4):
                for kt in range(8):
                    mm = nc.tensor.matmul(pt[:, ft * 4:ft * 4 + 4], lhsT=wst[s][:, kt, ft * 128:(ft + 1) * 128],
                                          rhs=scT[:, kt, :], start=(kt == 0), stop=(kt == 7))
            tm = PE.sig(mm)
            rel_t[s] = tm
            DVE.wait(tm, t_const)
            for ft in range(4):
                i_ = nc.vector.tensor_scalar(out=modT[:, blk * 4 + ft, :], in0=pt[:, ft * 4:ft * 4 + 4],
                                             scalar1=badaT[:, blk * 4 + ft:blk * 4 + ft + 1], scalar2=None,
                                             op0=ALU.add)
            ev_t[blk % 8] = DVE.sig(i_)
        t_mod = ev_t[11 % 8]
        DVE.wait(t_mod)
        for s in range(4):
            for (Ax, nT, part) in ((A1, n1T, 1), (A2, n2T, 4)):
                DVE.wait(DVE.sig(nc.vector.tensor_scalar(out=Ax[:, s, :], in0=modT[:, part * 8:(part + 1) * 8, s], scalar1=1.0,
                                        scalar2=None, op0=ALU.add)))
                t_A = DVE.sig(nc.vector.tensor_tensor(out=Ax[:, s, :], in0=Ax[:, s, :], in1=nT[:, :], op=ALU.mult))
                DVE.wait(t_A)
        barrier()

    def row_bcast(dst, part, seg, diag, pss):
        last = None
        for kt in range(8):
            DVE.wait(last)
            td = DVE.sig(nc.vector.tensor_scalar(out=diag[:], in0=ident[:], scalar1=modT[:, part * 8 + kt, seg:seg + 1],
                                                 scalar2=None, op0=ALU.mult))
            PE.wait(td, t_ones)
            last = PE.sig(nc.tensor.matmul(pss[kt // 4][:, (kt % 4) * 128:(kt % 4 + 1) * 128], lhsT=ones_f[:], rhs=diag[:],
                                           start=True, stop=True))
        DVE.wait(last)
        nc.vector.tensor_copy(out=dst[:, 0:512], in_=pss[0][:, :])
        return DVE.sig(nc.vector.tensor_copy(out=dst[:, 512:1024], in_=pss[1][:, :]))

    with contextlib.ExitStack() as st:
        rb = sb("rb", [32, 20], F32, st)
        oh = sb("oh", [32, 4, 512], F32, st)
        tb = sb("tb", [32, 512], F32, st)
        tkr = sb("tkr", [128, 3, 128], F32, st)
        d_t = dsem("tbl")
        t1 = d_t.sig(nc.sync.dma_start(out=rb[:], in_=rel_bias))
        t2 = d_t.sig(nc.sync.dma_start(out=oh[:], in_=oh_d))
        ACT.wait(t2)
        t_rb = ACT.sig(nc.scalar.activation(out=rb[:], in_=rb[:], func=AF.Exp))
        PE.wait(t_rb)
        hs = [(0, 8), (8, 4), (12, 4), (16, 4)]
        for kind, (h0, nh) in enumerate(hs):
            tm = PE.sig(nc.tensor.matmul(psA[kind][0:nh, :], lhsT=rb[:, h0:h0 + nh], rhs=oh[:, kind, :], start=True, stop=True))
            DVE.wait(tm)
            tcp = DVE.sig(nc.vector.tensor_copy(out=tb[0:nh, :], in_=psA[kind][0:nh, :]))
            SP.wait(tcp)
            tst = d_t.sig(nc.sync.dma_start(out=TBL[h0:h0 + nh, :], in_=tb[0:nh, :]))
            DVE.wait(tst)
        SP.wait(tst)
        tprev = None
        for h in range(20):
            Hb = 128 if h < 8 else 64
            Rmax = 2 * Hb - 1
            SP.wait(tprev)
            for di in range(3):
                src = bass.AP(tensor=TBL.tensor, offset=h * 512 + Rmax - Hb * (di - 1) - Hb + 1, ap=[[1, Hb], [1, Hb]])
                tl = d_t.sig(nc.sync.dma_start(out=tkr[0:Hb, di, 0:Hb], in_=src))
            PE.wait(tl)
            pt = psA[h % 8]
            if h < 8:
                tm = PE.sig(nc.tensor.matmul(pt[:, 0:384], lhsT=jmat[:, :], rhs=tkr[:, :, :].rearrange("p a b -> p (a b)"),
                                             start=True, stop=True))
                dst0 = EBA[:, h, 0, :]
                dst1 = EBA[:, h, 1, :]
                src_ps = pt[:, 0:384]
                cc = connc[:, 0:1]
            else:
                hh = h - 8
                pp = (hh % 2) * 64
                tm = PE.sig(nc.tensor.matmul(pt[pp:pp + 64, 0:192].rearrange("p (a b) -> p a b", a=3),
                                             lhsT=jmat[0:64, 64:128] if pp == 0 else jmat[0:64, 64:128],
                                             rhs=tkr[0:64, :, 0:64], start=True, stop=True))
                dst0 = EBB[pp:pp + 64, hh // 2, 0, :]
                dst1 = EBB[pp:pp + 64, hh // 2, 1, :]
                src_ps = pt[pp:pp + 64, 0:192]
                cc = connc[pp:pp + 64, 0:1]
            tprev = tm
            DVE.wait(tm)
            nc.vector.tensor_copy(out=dst0, in_=src_ps)
            t_eb = DVE.sig(nc.vector.tensor_scalar(out=dst1, in0=src_ps, scalar1=cc, scalar2=None, op0=ALU.mult))
        barrier()

    pt_cols = [i * 128 for i in range(4)] + [512] + [768 + i * 128 for i in range(6)] + [1536 + i * 128 for i in range(6)]
    x_v = x.rearrange("(t p) n -> t p n", p=128)
    X1_v = X1.rearrange("(t p) n -> t p n", p=128)
    y_v = y.rearrange("(t p) n -> t p n", p=128)

    def norm_transpose(st_name, src_v, tile_idx, xin, xs, ssq, rstd, junk, tok_xin_free, d_x, psT, psT_free, hT, hcol, Ax, seg,
                       Bpart, hT_free):
        raise NotImplementedError

    with contextlib.ExitStack() as st:
        wi = sb("wi", [128, 8, 5120], BF16, st)
        xin = [sb("xin%d" % i, [128, D], F32, st) for i in range(2)]
        xs = [sb("xs%d" % i, [128, D], F32, st) for i in range(2)]
        junk = sb("junk", [128, D], F32, st)
        ssq = sb("ssq", [128, 16], F32, st)
        hT = sb("hT", [128, 8, CH], BF16, st)
        stQ = [sb("stQ%d" % i, [128, 17, 512], BF16, st) for i in range(1)]
        stG = [sb("stG%d" % i, [128, 16, 512], BF16, st) for i in range(1)]
        stV = sb("stV", [128, 8, 896], BF16, st)
        d_wi = dsem("wi")
        d_x = [dsem("x%d" % i) for i in range(2)]
        d_st = dsem("st")
        w_in_v = w_in.rearrange("(kt p) n -> p kt n", p=128)
        t_wi = None
        for blk in range(10):
            t_wi = d_wi.sig(nc.gpsimd.dma_start(out=wi[:, :, blk * 512:(blk + 1) * 512], in_=w_in_v[:, :, blk * 512:(blk + 1) * 512]))
        cvb = [sb("cvb%d" % i, [128, 3, 2048], BF16, st) for i in range(2)]
        d_cl = [dsem("cvl%d" % i) for i in range(2)]
        d_cs = [dsem("cvs%d" % i) for i in range(2)]
        cv_state = {"ld": [None, None], "st": [None, None], "n": 0}
        wsrc = (wg2, wu2, wd2)

        def cv_load(e):
            sl = e % 2
            POOL.wait(cv_state["st"][sl])
            for m_ in range(3):
                t_ = d_cl[sl].sig(nc.gpsimd.dma_start(out=cvb[sl][:, m_, :], in_=wsrc[m_][e * 128:(e + 1) * 128, :]))
            cv_state["ld"][sl] = t_

        def cv_store(e):
            sl = e % 2
            POOL.wait(cv_state["ld"][sl])
            for m_ in range(3):
                t_ = d_cs[sl].sig(nc.gpsimd.dma_start(out=WB[m_][e * 128:(e + 1) * 128, :], in_=cvb[sl][:, m_, :]))
            cv_state["st"][sl] = t_

        def convert_some(k):
            for _ in range(k):
                e = cv_state["n"]
                if e >= 65:
                    return
                if e == 0:
                    cv_load(0)
                if e + 1 < 65:
                    cv_load(e + 1)
                cv_store(e)
                cv_state["n"] += 1

        xin_free = [None, None]
        xs_free = [None, None]
        psT_free = [None] * 4
        pp_free = [None] * 2
        pv_free = None
        hT_free = None
        stq_free = None
        stg_free = None
        stv_free = None
        cnt_tile = 0
        ev_flip = 0
        for c in range(NCH):
            seg = c // 2
            t_h = None
            convert_some(9)
            for tt in range(8):
                gt = c * 8 + tt
                b = gt % 2
                SP.wait(xin_free[b])
                tl = d_x[b].sig(nc.sync.dma_start(out=xin[b][:], in_=x_v[gt]))
                ACT.wait(tl)
                col = gt % 16
                ta = ACT.sig(nc.scalar.activation(out=junk[:], in_=xin[b][:], func=AF.Square, accum_out=ssq[:, col:col + 1]))
                ACT.wait(ta)
                ta = ACT.sig(nc.scalar.activation(out=ssq[:, col:col + 1], in_=ssq[:, col:col + 1], func=AF.Sqrt, scale=1.0 / D, bias=EPS))
                DVE.wait(ta, xs_free[b])
                td = DVE.sig(nc.vector.reciprocal(out=ssq[:, col:col + 1], in_=ssq[:, col:col + 1]))
                DVE.wait(td)
                td = DVE.sig(nc.vector.tensor_scalar(out=xs[b][:], in0=xin[b][:], scalar1=ssq[:, col:col + 1], scalar2=None, op0=ALU.mult))
                xin_free[b] = td
                PE.wait(td, psT_free[2 * b], psT_free[2 * b + 1])
                for kt in range(8):
                    tp = nc.tensor.transpose(psA[2 * b + kt // 4][:, (kt % 4) * 128:(kt % 4 + 1) * 128], xs[b][:, kt * 128:(kt + 1) * 128], ident[:])
                tp = PE.sig(tp)
                xs_free[b] = tp
                ACT.wait(tp, hT_free if tt == 0 else None)
                for kt in range(8):
                    ta = nc.scalar.activation(out=hT[:, kt, tt * 128:(tt + 1) * 128], in_=psA[2 * b + kt // 4][:, (kt % 4) * 128:(kt % 4 + 1) * 128],
                                              func=AF.Identity, scale=A1[:, seg, kt:kt + 1], bias=modT[:, kt, seg:seg + 1])
                ta = ACT.sig(ta)
                psT_free[2 * b] = ta
                psT_free[2 * b + 1] = ta
                t_h = ta
            PE.wait(t_h, t_wi)
            last_pe_read_h = None
            for half in range(2):
                tsl = slice(half * 512, (half + 1) * 512)
                gsl = slice(c * CH + half * 512, c * CH + (half + 1) * 512)
                evs = []
                for ti in range(33):
                    col0 = pt_cols[ti] if ti < 17 else 3072 + (ti - 17) * 128
                    pb_ = ti % 2
                    PE.wait(pp_free[pb_])
                    for kt in range(8):
                        mm = nc.tensor.matmul(psA[4 + pb_][:, :], lhsT=wi[:, kt, col0:col0 + 128], rhs=hT[:, kt, tsl], start=(kt == 0), stop=(kt == 7))
                    tm = PE.sig(mm)
                    last_pe_read_h = tm
                    if ti < 17:
                        if ti == 0:
                            DVE.wait(stq_free)
                        DVE.wait(tm)
                        te = DVE.sig(nc.vector.tensor_copy(out=stQ[0][:, ti, :], in_=psA[4 + pb_][:, :]))
                    else:
                        if ti == 17:
                            ACT.wait(stg_free)
                        ACT.wait(tm)
                        te = ACT.sig(nc.scalar.activation(out=stG[0][:, ti - 17, :], in_=psA[4 + pb_][:, :], func=AF.Sigmoid))
                    pp_free[pb_] = te
                    evs.append(te)
                SP.wait(evs[16])
                stq_free = d_st.sig(nc.sync.dma_start(out=PT.rearrange("(t p) n -> p t n", p=128)[:, :, gsl], in_=stQ[0][:, :, :]))
                SP.wait(evs[32])
                stg_free = d_st.sig(nc.sync.dma_start(out=GT.rearrange("(t p) n -> p t n", p=128)[:, :, gsl], in_=stG[0][:, :, :]))
            DVE.wait(stv_free)
            for i in range(8):
                PE.wait(pv_free)
                jobs = [(640, 128, psA[6][:, 0:128], lambda hk: hk[:, i * 128:(i + 1) * 128]),
                        (2304, 256, psA[6][:, 128:384], lambda hk: hk[:, i * 128:(i + 1) * 128]),
                        (2560, 256, psA[7][:, 0:256],
                         lambda hk: hk.rearrange("p (u r) -> p r u", r=4)[:, i // 2, (i % 2) * 128:(i % 2 + 1) * 128]),
                        (2816, 256, psA[7][0:64, 256:512], lambda hk: hk.rearrange("p (u r) -> p r u", r=16)[:, 2 * i, :]),
                        (2816, 256, psA[7][64:128, 256:512], lambda hk: hk.rearrange("p (u r) -> p r u", r=16)[:, 2 * i + 1, :])]
                for (c0, wd, o_, lf) in jobs:
                    for kt in range(8):
                        mm = nc.tensor.matmul(o_, lhsT=lf(hT[:, kt, :]), rhs=wi[:, kt, c0:c0 + wd], start=(kt == 0), stop=(kt == 7))
                tm = PE.sig(mm)
                last_pe_read_h = tm
                DVE.wait(tm)
                nc.vector.tensor_copy(out=stV[:, i, 0:384], in_=psA[6][:, 0:384])
                te = DVE.sig(nc.vector.tensor_copy(out=stV[:, i, 384:896], in_=psA[7][:, :]))
                pv_free = te
            SP.wait(te)
            stv_free = d_st.sig(nc.sync.dma_start(out=VV[c * CH:(c + 1) * CH, :].rearrange("(i p) f -> p i f", p=128), in_=stV[:, :, :]))
            hT_free = last_pe_read_h
        barrier()

    if debug == "proj":
        es.close()
        return nc

    ps_free = [None] * 8

    def bc_last(a, n):
        return bass.AP(tensor=a.tensor, offset=a.offset, ap=[list(a.ap[0]), list(a.ap[1]), [0, n]])

    with contextlib.ExitStack() as st:
        wpa = sb("wpa", [128, 4, D], BF16, st)
        wpb = sb("wpb", [128, 2, D], BF16, st)
        wo = sb("wo", [128, 8, D], BF16, st)
        d_w2 = dsem("w2")
        nc.gpsimd.dma_start(out=wpa[:], in_=w_pa.rearrange("(kt p) n -> p kt n", p=128)).then_inc(d_w2.sem, 16)
        nc.gpsimd.dma_start(out=wpb[:], in_=w_pb.rearrange("(kt p) n -> p kt n", p=128)).then_inc(d_w2.sem, 16)
        d_w2.cnt = 32
        t_w2 = d_w2.sig(nc.gpsimd.dma_start(out=wo[:], in_=w_o.rearrange("(kt p) n -> p kt n", p=128)))
        QA = sb("QA", [64, 8, CH], BF16, st)
        QB = sb("QB", [128, 6, CH], BF16, st)
        KAw = sb("KAw", [64, 2, 1280], BF16, st)
        KBw = [sb("KBw%d" % g, [128, 2, CH + 128 * d], BF16, st) for g, (w_, d) in enumerate(GROUPS)]
        VAw = sb("VAw", [128, 10, 128], BF16, st)
        VBw = [sb("VBw%d" % g, [128, n_, 256], BF16, st) for g, n_ in enumerate((18, 24, 48))]
        yaT = sb("yaT", [128, 4, CH], BF16, st)
        ybT = sb("ybT", [128, 2, CH], BF16, st)
        accN = sb("accN", [128, CH], F32, st)
        accD = sb("accD", [128, CH], F32, st)
        P0 = [sb("P0_%d" % i, [128, 384], BF16, st) for i in range(2)]
        Pt = [sb("Pt_%d" % i, [128, 384], BF16, st) for i in range(4)]
        rd = sb("rd", [128, 128], F32, st)
        Gt = sb("Gt", [128, 16, 256], BF16, st)
        mg = sb("mg", [128, 8, 256], BF16, st)
        t12 = sb("t12", [128, 2, 512], F32, st)
        G1bc = sb("G1bc", [128, D], F32, st)
        diag = sb("diag", [128, 128], F32, st)
        xio = sb("xio", [128, D], F32, st)
        d_ld = dsem("attld")
        d_g = dsem("gld")
        d_xi = dsem("xi")
        d_xo = dsem("xo")
        PTq = PT[0:512, :].rearrange("(h p) n -> p h n", p=64)
        PTka = PT[512:640, :].rearrange("(h p) n -> p h n", p=64)
        PTqb = PT[640:1408, :].rearrange("(t p) n -> p t n", p=128)
        GTv = GT.rearrange("(t p) n -> p t n", p=128)
        att_free = None
        y_free = None
        g_free = None
        mg_free = None
        xio_free = None
        t_g1 = None
        P0_free = [None, None]
        Pt_free = [None] * 4
        rd_free = None
        t12_free = [None, None]
        for c in range(NCH):
            seg = c // 2
            if c % 2 == 0:
                PE.wait(ps_free[6], ps_free[7])
                DVE.wait(xio_free)
                t_g1 = row_bcast(G1bc, 2, seg, diag, [psA[6], psA[7]])
                ps_free[6] = t_g1
                ps_free[7] = t_g1
            SP.wait(att_free)
            lt = []
            c0 = c * CH
            lt.append(d_ld.sig(nc.sync.dma_start(out=QA[:, :, :], in_=PTq[:, :, c0:c0 + CH])))
            lt.append(d_ld.sig(nc.sync.dma_start(out=QB[:, :, :], in_=PTqb[:, :, c0:c0 + CH])))
            lo, hi = max(0, c0 - 128), min(NT, c0 + CH + 128)
            lt.append(d_ld.sig(nc.sync.dma_start(out=KAw[:, :, lo - (c0 - 128):hi - (c0 - 128)], in_=PTka[:, :, lo:hi])))
            for g, (w_, d) in enumerate(GROUPS):
                H = 64 * d
                lo, hi = max(0, c0 - H), min(NT, c0 + CH + H)
                src = PT[1408 + 256 * g:1408 + 256 * (g + 1), :].rearrange("(t p) n -> p t n", p=128)
                lt.append(d_ld.sig(nc.sync.dma_start(out=KBw[g][:, :, lo - (c0 - H):hi - (c0 - H)], in_=src[:, :, lo:hi])))
            lo, hi = max(0, c0 - 128), min(NT, c0 + CH + 128)
            b0, b1 = (lo - (c0 - 128)) // 128, (hi - (c0 - 128)) // 128
            lt.append(d_ld.sig(nc.sync.dma_start(out=VAw[:, b0:b1, :], in_=VV[lo:hi, 0:128].rearrange("(b p) f -> p b f", p=128))))
            for hp in (0, 64):
                lo, hi = max(0, c0 - 64), min(NT, c0 + CH + 64)
                b0, b1 = (lo - (c0 - 64)) // 64, (hi - (c0 - 64)) // 64
                lt.append(d_ld.sig(nc.sync.dma_start(out=VBw[0][hp:hp + 64, b0:b1, :], in_=VV[lo:hi, 128:384].rearrange("(b p) f -> p b f", p=64))))
                lt.append(d_ld.sig(nc.sync.dma_start(out=VBw[1][hp:hp + 64, 0:16, :], in_=VV[c0:c0 + CH, 384:640].rearrange("(b p) f -> p b f", p=64))))
                if c > 0:
                    lt.append(d_ld.sig(nc.sync.dma_start(out=VBw[1][hp:hp + 64, 16:20, :],
                                                         in_=VV[c0 - CH:c0, 384:640].rearrange("(r b p) f -> p r b f", r=4, b=4, p=64)[:, :, 3, :])))
                if c < NCH - 1:
                    lt.append(d_ld.sig(nc.sync.dma_start(out=VBw[1][hp:hp + 64, 20:24, :],
                                                         in_=VV[c0 + CH:c0 + 2 * CH, 384:640].rearrange("(r b p) f -> p r b f", r=4, b=4, p=64)[:, :, 0, :])))
                for dc in (-1, 0, 1):
                    if 0 <= c + dc < NCH:
                        lt.append(d_ld.sig(nc.sync.dma_start(out=VBw[2][hp:hp + 64, 16 * (dc + 1):16 * (dc + 2), :],
                                                             in_=VV[c0 + dc * CH:c0 + (dc + 1) * CH, 640:896].rearrange("(r p) f -> p r f", p=64))))
            t_ld = lt[-1]
            PE.wait(t_ld, t_w2)

            items = []
            for h in range(8):
                kv = h // 4
                for i in range(8):
                    gb = 8 * c + i
                    dl = [dd for dd in (-1, 0, 1) if 0 <= gb + dd < 64]
                    items.append(dict(kind="A", h=h, kv=kv, i=i, dl=dl, cross=[(gb + dd) // 16 != gb // 16 for dd in dl], Hb=128))
            for p in range(2):
                for g, (w_, d) in enumerate(GROUPS):
                    nb = 16 // d
                    for r in range(d):
                        for b in range(nb):
                            ub = nb * c + b
                            dl = [dd for dd in (-1, 0, 1) if 0 <= ub + dd < 8 * nb]
                            items.append(dict(kind="B", p=p, g=g, d=d, nb=nb, r=r, b=b, dl=dl,
                                              cross=[(ub + dd) // (2 * nb) != ub // (2 * nb) for dd in dl], Hb=64,
                                              first=(r == 0 and b == 0), last_of_pair=(g == 2 and r == d - 1 and b == nb - 1)))
            N = len(items)
            acc_tok = None

            def vidx(it, dd):
                g, nb, b, r = it["g"], it["nb"], it["b"], it["r"]
                ub = nb * c + b + dd
                dc = ub // nb - c
                bp = ub % nb
                if g == 0:
                    return ub - 16 * c + 1
                if g == 1:
                    return 4 * r + bp if dc == 0 else (16 + r if dc < 0 else 20 + r)
                return 16 * (dc + 1) + r

            def stage1(n):
                it = items[n]
                Hb = it["Hb"]
                S = psA[n % 3]
                PE.wait(ps_free[n % 3])
                for dd in it["dl"]:
                    cs = slice((dd + 1) * Hb, (dd + 2) * Hb)
                    if it["kind"] == "A":
                        i = it["i"]
                        mm = nc.tensor.matmul(S[:, cs], lhsT=KAw[0:64, it["kv"], (i + 1 + dd) * 128:(i + 2 + dd) * 128],
                                              rhs=QA[0:64, it["h"], i * 128:(i + 1) * 128], start=True, stop=True)
                    else:
                        d, g, b, r, p = it["d"], it["g"], it["b"], it["r"], it["p"]
                        qoff = d * 64 * b + r
                        koff = d * 64 * (b + dd) + r + 64 * d
                        for hp in (0, 64):
                            mm = nc.tensor.matmul(S[hp:hp + 64, cs], lhsT=KBw[g][hp:hp + 64, p, koff:koff + 63 * d + 1:d],
                                                  rhs=QB[hp:hp + 64, 2 * g + p, qoff:qoff + 63 * d + 1:d], start=True, stop=True)
                tm = PE.sig(mm)
                lo, hi = (it["dl"][0] + 1) * Hb, (it["dl"][-1] + 2) * Hb
                ACT.wait(tm, P0_free[n % 2])
                ta = ACT.sig(nc.scalar.activation(out=P0[n % 2][:, lo:hi], in_=S[:, lo:hi], func=AF.Exp, scale=0.125))
                ps_free[n % 3] = ta
                POOL.wait(ta, Pt_free[n % 4])
                if it["kind"] == "A":
                    eb = lambda v, a, b_: EBA[:, it["h"], v, a:b_]
                else:
                    eb = lambda v, a, b_: EBB[:, 2 * it["g"] + it["p"], v, a:b_]
                if not any(it["cross"]):
                    td = nc.gpsimd.tensor_tensor(out=Pt[n % 4][:, lo:hi], in0=P0[n % 2][:, lo:hi], in1=eb(0, lo, hi), op=ALU.mult)
                else:
                    for dd, cr in zip(it["dl"], it["cross"]):
                        a, b_ = (dd + 1) * Hb, (dd + 2) * Hb
                        td = nc.gpsimd.tensor_tensor(out=Pt[n % 4][:, a:b_], in0=P0[n % 2][:, a:b_], in1=eb(1 if cr else 0, a, b_), op=ALU.mult)
                td = POOL.sig(td)
                P0_free[n % 2] = td
                it["tP"] = td

            def stage2(n):
                nonlocal att_free, rd_free, acc_tok, y_free
                it = items[n]
                Hb = it["Hb"]
                O = psA[3 + n % 2]
                PE.wait(it["tP"], ps_free[3 + n % 2])
                nd = len(it["dl"])
                if it["kind"] == "A":
                    pb_ = (it["h"] % 2) * 64
                    i = it["i"]
                    for k_, dd in enumerate(it["dl"]):
                        nc.tensor.matmul(O[pb_:pb_ + 64, 0:128], lhsT=VAw[:, i + 1 + dd, it["kv"] * 64:(it["kv"] + 1) * 64],
                                         rhs=Pt[n % 4][:, (dd + 1) * 128:(dd + 2) * 128], start=(k_ == 0), stop=(k_ == nd - 1))
                    for k_, dd in enumerate(it["dl"]):
                        mm = nc.tensor.matmul(O[pb_:pb_ + 64, 128:256], lhsT=ones_b[:, 0:64],
                                              rhs=Pt[n % 4][:, (dd + 1) * 128:(dd + 2) * 128], start=(k_ == 0), stop=(k_ == nd - 1))
                    tm = PE.sig(mm)
                    att_free = tm
                    Pt_free[n % 4] = tm
                    DVE.wait(tm, t_es, rd_free, y_free)
                    h = it["h"]
                    DVE.wait(DVE.sig(nc.vector.tensor_scalar(out=rd[pb_:pb_ + 64, :], in0=O[pb_:pb_ + 64, 128:256],
                                                             scalar1=es_bc[pb_:pb_ + 64, h:h + 1], scalar2=None, op0=ALU.add)))
                    DVE.wait(DVE.sig(nc.vector.reciprocal(out=rd[pb_:pb_ + 64, :], in_=rd[pb_:pb_ + 64, :])))
                    td = DVE.sig(nc.vector.tensor_tensor(out=yaT[pb_:pb_ + 64, h // 2, i * 128:(i + 1) * 128], in0=O[pb_:pb_ + 64, 0:128],
                                                         in1=rd[pb_:pb_ + 64, :], op=ALU.mult))
                    rd_free = td
                    ps_free[3 + n % 2] = td
                else:
                    d, g, b, r, p = it["d"], it["g"], it["b"], it["r"], it["p"]
                    for hp in (0, 64):
                        for k_, dd in enumerate(it["dl"]):
                            nc.tensor.matmul(O[hp:hp + 64, 0:64], lhsT=VBw[g][hp:hp + 64, vidx(it, dd), (2 * p + hp // 64) * 64:(2 * p + hp // 64 + 1) * 64],
                                             rhs=Pt[n % 4][hp:hp + 64, (dd + 1) * 64:(dd + 2) * 64], start=(k_ == 0), stop=(k_ == nd - 1))
                    for k_, dd in enumerate(it["dl"]):
                        mm = nc.tensor.matmul(O[:, 64:128], lhsT=ones_bd[:, :],
                                              rhs=Pt[n % 4][:, (dd + 1) * 64:(dd + 2) * 64], start=(k_ == 0), stop=(k_ == nd - 1))
                    tm = PE.sig(mm)
                    att_free = tm
                    Pt_free[n % 4] = tm
                    qoff = d * 64 * b + r
                    sl = slice(qoff, qoff + 63 * d + 1, d)
                    DVE.wait(tm)
                    if it["first"]:
                        DVE.wait(acc_tok, y_free)
                    if g == 0:
                        nc.vector.tensor_copy(out=accN[:, sl], in_=O[:, 0:64])
                        td = DVE.sig(nc.vector.tensor_copy(out=accD[:, sl], in_=O[:, 64:128]))
                    else:
                        nc.vector.tensor_tensor(out=accN[:, sl], in0=accN[:, sl], in1=O[:, 0:64], op=ALU.add)
                        td = DVE.sig(nc.vector.tensor_tensor(out=accD[:, sl], in0=accD[:, sl], in1=O[:, 64:128], op=ALU.add))
                    acc_tok = td
                    ps_free[3 + n % 2] = td
                    if it["last_of_pair"]:
                        DVE.wait(td)
                        DVE.wait(DVE.sig(nc.vector.reciprocal(out=accD[:, :], in_=accD[:, :])))
                        acc_tok = DVE.sig(nc.vector.tensor_tensor(out=ybT[:, p, :], in0=accN[:, :], in1=accD[:, :], op=ALU.mult))
                        DVE.wait(acc_tok)

            LAG = 3
            for n in range(N + LAG):
                if n < N:
                    stage1(n)
                if n >= LAG:
                    stage2(n - LAG)
            t_y = acc_tok

            for m in range(4):
                msl = slice(m * 256, (m + 1) * 256)
                SP.wait(g_free)
                tg = d_g.sig(nc.sync.dma_start(out=Gt[:, :, :], in_=GTv[:, :, c0 + m * 256:c0 + (m + 1) * 256]))
                PE.wait(t_y)
                for nt in range(8):
                    pm = psA[5] if nt % 2 == 0 else psA[2]
                    pi = 5 if nt % 2 == 0 else 2
                    PE.wait(ps_free[pi])
                    for kt in range(4):
                        nc.tensor.matmul(pm[:, 0:256], lhsT=wpa[:, kt, nt * 128:(nt + 1) * 128], rhs=yaT[:, kt, msl], start=(kt == 0), stop=(kt == 3))
                    for kt in range(2):
                        mm = nc.tensor.matmul(pm[:, 256:512], lhsT=wpb[:, kt, nt * 128:(nt + 1) * 128], rhs=ybT[:, kt, msl], start=(kt == 0), stop=(kt == 1))
                    tm = PE.sig(mm)
                    y_free = tm
                    DVE.wait(tm, tg, t12_free[nt % 2], mg_free if nt == 0 else None)
                    tq = t12[:, nt % 2, :]
                    nc.vector.tensor_tensor(out=tq[:, 0:256], in0=pm[:, 0:256], in1=Gt[:, nt, :], op=ALU.mult)
                    td = DVE.sig(nc.vector.tensor_tensor(out=tq[:, 256:512], in0=pm[:, 256:512], in1=Gt[:, 8 + nt, :], op=ALU.mult))
                    ps_free[pi] = td
                    DVE.wait(td)
                    td = DVE.sig(nc.vector.tensor_tensor(out=mg[:, nt, :], in0=tq[:, 0:256], in1=tq[:, 256:512], op=ALU.add))
                    t12_free[nt % 2] = td
                g_free = td
                t_mg = td
                for tt in range(2):
                    gt = c * 8 + m * 2 + tt
                    SP.wait(xio_free)
                    tx = d_xi.sig(nc.sync.dma_start(out=xio[:], in_=x_v[gt]))
                    PE.wait(t_mg, ps_free[6], ps_free[7])
                    for nh in range(2):
                        for kt in range(8):
                            mm = nc.tensor.matmul(psA[6 + nh][:, :], lhsT=mg[:, kt, tt * 128:(tt + 1) * 128], rhs=wo[:, kt, nh * 512:(nh + 1) * 512],
                                                  start=(kt == 0), stop=(kt == 7))
                    tm = PE.sig(mm)
                    mg_free = tm
                    DVE.wait(tm, tx, t_g1, t12_free[0], t12_free[1])
                    for nh in range(2):
                        hs_ = slice(nh * 512, (nh + 1) * 512)
                        DVE.wait(DVE.sig(nc.vector.tensor_tensor(out=t12[:, nh, :], in0=psA[6 + nh][:, :], in1=G1bc[:, hs_], op=ALU.mult)))
                        td = DVE.sig(nc.vector.tensor_tensor(out=xio[:, hs_], in0=xio[:, hs_], in1=t12[:, nh, :], op=ALU.add))
                    ps_free[6] = td
                    ps_free[7] = td
                    t12_free[0] = td
                    t12_free[1] = td
                    SP.wait(td)
                    xio_free = d_xo.sig(nc.sync.dma_start(out=X1_v[gt], in_=xio[:]))
        barrier()

    if debug == "att":
        es.close()
        return nc

    pst.close()
    ps_free = [None] * 8
    U32 = mybir.dt.uint32
    I32 = mybir.dt.int32

    def bc_mid(a, n):
        return bass.AP(tensor=a.tensor, offset=a.offset, ap=[list(a.ap[0]), [0, n], list(a.ap[1])])

    def row_bcast2(dst, colfn, diag, pss):
        last = None
        for kt in range(8):
            DVE.wait(last)
            td = DVE.sig(nc.vector.tensor_scalar(out=diag[:], in0=ident[:], scalar1=colfn(kt), scalar2=None, op0=ALU.mult))
            PE.wait(td, t_ones)
            last = PE.sig(nc.tensor.matmul(pss[kt // 4][:, (kt % 4) * 128:(kt % 4 + 1) * 128], lhsT=ones_f[:], rhs=diag[:],
                                           start=True, stop=True))
        DVE.wait(last)
        nc.vector.tensor_copy(out=dst[:, 0:512], in_=pss[0][:, :])
        return DVE.sig(nc.vector.tensor_copy(out=dst[:, 512:1024], in_=pss[1][:, :]))

    with contextlib.ExitStack() as st:
        psF = [st.enter_context(nc.psum_tensor("psF%d" % i, [128, 512], F32)) for i in range(6)]
        psB = [st.enter_context(nc.psum_tensor("psB%d" % i, [128, 1024], BF16)) for i in range(2)]
        cst = sb("cst", [128, NCST], F32, st)
        iota_e = cst[:, 0:64]
        tokid = cst[:, 64:128]
        pcol = cst[:, 128:129]
        iota_b = cst[:, 129:129 + NBLK]
        thr16 = cst[:, 129 + NBLK:129 + NBLK + 16]
        U_b = sb("U_b", [128, 128], BF16, st)
        ones128 = sb("ones128", [128, 128], BF16, st)
        id_b = sb("id_b", [128, 128], BF16, st)
        wr = sb("wr", [128, 8, NE], BF16, st)
        rb_bc = sb("rb_bc", [128, NE], F32, st)
        FNbc = sb("FNbc", [128, D], F32, st)
        G2all = sb("G2all", [128, 4, D], F32, st)
        diag = sb("diag2", [128, 128], F32, st)
        RK = sb("RK", [128, 64, 64], F32, st)
        E6f = sb("E6f", [128, 64, 8], F32, st)
        W6 = sb("W6", [128, 64, 8], F32, st)
        D6f = sb("D6f", [128, 64, 8], F32, st)
        D6i = sb("D6i", [128, 64, 8], I32, st)
        IDXW = sb("IDXW", [128, NBLK], I32, st)
        carry = sb("carry", [128, 64], F32, st)
        xin = [sb("mxin%d" % i, [128, D], F32, st) for i in range(2)]
        xs = [sb("mxs%d" % i, [128, D], F32, st) for i in range(2)]
        junk = sb("mjunk", [128, D], F32, st)
        ssq = sb("mssq", [128, 64], F32, st)
        d_m = dsem("mconst")
        nc.sync.dma_start(out=cst[:], in_=cst_d).then_inc(d_m.sem, 16)
        nc.sync.dma_start(out=rb_bc[:], in_=router_bias[0:1, :].partition_broadcast(128)).then_inc(d_m.sem, 16)
        nc.sync.dma_start(out=FNbc[:], in_=final_norm[0:1, :].partition_broadcast(128)).then_inc(d_m.sem, 16)
        nc.gpsimd.dma_start(out=U_b[:], in_=umat_d).then_inc(d_m.sem, 16)
        nc.gpsimd.dma_start(out=id_b[:], in_=ident_d).then_inc(d_m.sem, 16)
        d_m.cnt += 80
        t_mc = d_m.sig(nc.gpsimd.dma_start(out=wr[:], in_=w_router.rearrange("(kt p) n -> p kt n", p=128)))
        t_o128 = DVE.sig(nc.vector.memset(ones128[:], 1.0))
        t_c0 = DVE.sig(nc.vector.memset(carry[:], 0.0))
        for s_ in range(4):
            PE.wait(ps_free[4], ps_free[5])
            tg_ = row_bcast2(G2all[:, s_, :], lambda kt: modT[:, 40 + kt, s_:s_ + 1], diag, [psF[4], psF[5]])
            ps_free[4] = tg_
            ps_free[5] = tg_
        t_g2 = tg_
        d_x = [dsem("mx%d" % i) for i in range(2)]
        d_h2 = [dsem("mh%d" % i) for i in range(2)]
        xin_free = [None, None]
        xs_free = [None, None]

        with contextlib.ExitStack() as st1:
            A2bc = sb("A2bc", [128, D], F32, st1)
            B2bc = sb("B2bc", [128, D], F32, st1)
            tmpf = sb("tmpf", [128, D], F32, st1)
            h2tm = [sb("h2tm%d" % i, [128, D], BF16, st1) for i in range(2)]
            h2T = [sb("h2T%d" % i, [128, 8, 128], BF16, st1) for i in range(2)]
            rt = sb("rt", [128, 8, 64], F32, st1)
            r8 = sb("r8", [128, 8, 8], F32, st1)
            mk = sb("mk", [128, 64], BF16, st1)
            i8 = sb("i8", [128, 8], U32, st1)
            xin4 = [xin[0], xin[1], sb("mxin2", [128, D], F32, st1), sb("mxin3", [128, D], F32, st1)]
            xs4 = [xs[0], xs[1], sb("mxs2", [128, D], F32, st1), sb("mxs3", [128, D], F32, st1)]
            h2tm4 = h2tm + [sb("h2tm%d" % i, [128, D], BF16, st1) for i in (2, 3)]
            h2T4 = h2T + [sb("h2T%d" % i, [128, 8, 128], BF16, st1) for i in (2, 3)]
            rt4 = [rt] + [sb("rt_%d" % i, [128, 8, 64], F32, st1) for i in (1, 2, 3)]
            r84 = [r8] + [sb("r8_%d" % i, [128, 8, 8], F32, st1) for i in (1, 2, 3)]
            mk4 = [mk] + [sb("mk_%d" % i, [128, 64], BF16, st1) for i in (1, 2, 3)]
            i84 = [i8] + [sb("i8_%d" % i, [128, 8], U32, st1) for i in (1, 2, 3)]
            d_x4 = d_x + [dsem("mx%d" % i) for i in (2, 3)]
            d_h24 = d_h2 + [dsem("mh%d" % i) for i in (2, 3)]
            xin_free4 = [None] * 4
            xs_free4 = [None] * 4
            h2tm_free4 = [None] * 4
            h2T_free4 = [None] * 4
            mk_free4 = [None] * 4
            chain_end4 = [None] * 4
            st_ = {"t_ab": None, "pool": None, "t_h": None}
            v3 = lambda a: a.rearrange("p (g e) -> p g e", g=8)
            ch = lambda ins: DVE.wait(DVE.sig(ins))

            def front(gt):
                s4 = gt % 4
                b = gt % 2
                seg = gt // 16
                if gt % 16 == 0:
                    PE.wait(ps_free[4], ps_free[5])
                    DVE.wait(st_["pool"])
                    ta_ = row_bcast2(A2bc, lambda kt: A2[:, seg, kt:kt + 1], diag, [psF[4], psF[5]])
                    PE.wait(ta_)
                    st_["t_ab"] = row_bcast2(B2bc, lambda kt: modT[:, 24 + kt, seg:seg + 1], diag, [psF[4], psF[5]])
                    ps_free[4] = st_["t_ab"]
                    ps_free[5] = st_["t_ab"]
                SP.wait(xin_free4[s4])
                tl = d_x4[s4].sig(nc.sync.dma_start(out=xin4[s4][:], in_=X1_v[gt]))
                ACT.wait(tl)
                col = gt
                ta = ACT.sig(nc.scalar.activation(out=junk[:], in_=xin4[s4][:], func=AF.Square, accum_out=ssq[:, col:col + 1]))
                ACT.wait(ta)
                ta = ACT.sig(nc.scalar.activation(out=ssq[:, col:col + 1], in_=ssq[:, col:col + 1], func=AF.Sqrt, scale=1.0 / D, bias=EPS))
                DVE.wait(ta, xs_free4[s4])
                ch(nc.vector.reciprocal(out=ssq[:, col:col + 1], in_=ssq[:, col:col + 1]))
                td = DVE.sig(nc.vector.tensor_scalar(out=xs4[s4][:], in0=xin4[s4][:], scalar1=ssq[:, col:col + 1], scalar2=None, op0=ALU.mult))
                xin_free4[s4] = td
                POOL.wait(td, st_["t_ab"], h2tm_free4[s4])
                tq_ = POOL.sig(nc.gpsimd.tensor_tensor(out=tmpf[:], in0=xs4[s4][:], in1=A2bc[:], op=ALU.mult))
                POOL.wait(tq_)
                th = POOL.sig(nc.gpsimd.tensor_tensor(out=h2tm4[s4][:], in0=tmpf[:], in1=B2bc[:], op=ALU.add))
                st_["pool"] = th
                SP.wait(th)
                h2tm_free4[s4] = d_h24[s4].sig(nc.sync.dma_start(out=H2[gt * 128:(gt + 1) * 128, :], in_=h2tm4[s4][:]))
                PE.wait(td, ps_free[2 * b], ps_free[2 * b + 1])
                for kt in range(8):
                    tp = nc.tensor.transpose(psF[2 * b + kt // 4][:, (kt % 4) * 128:(kt % 4 + 1) * 128], xs4[s4][:, kt * 128:(kt + 1) * 128], ident[:])
                tp = PE.sig(tp)
                xs_free4[s4] = [tp, th]
                ACT.wait(tp, h2T_free4[s4])
                for kt in range(8):
                    ta = nc.scalar.activation(out=h2T4[s4][:, kt, :], in_=psF[2 * b + kt // 4][:, (kt % 4) * 128:(kt % 4 + 1) * 128],
                                              func=AF.Identity, scale=A2[:, seg, kt:kt + 1], bias=modT[:, 24 + kt, seg:seg + 1])
                ta = ACT.sig(ta)
                ps_free[2 * b] = ta
                ps_free[2 * b + 1] = ta
                PE.wait(ta, t_mc, ps_free[4])
                for kt in range(8):
                    mm = nc.tensor.matmul(psF[4][:, 0:64], lhsT=h2T4[s4][:, kt, :], rhs=wr[:, kt, :], start=(kt == 0), stop=(kt == 7))
                tm = PE.sig(mm)
                h2T_free4[s4] = tm
                ACT.wait(tm, chain_end4[s4])
                ta2 = ACT.sig(nc.scalar.activation(out=rt4[s4][:, 0, :], in_=psF[4][:, 0:64], func=AF.Sigmoid))
                ps_free[4] = ta2
                return ta2

            def chain_ops(gt):
                s4 = gt % 4
                rt_, r8_, mk_, i8_ = rt4[s4], r84[s4], mk4[s4], i84[s4]
                sc, sel, eq, sel2, selm, em, wdn = (rt_[:, i, :] for i in range(7))
                V = nc.vector
                return [
                    lambda: V.tensor_tensor(out=sel, in0=sc, in1=rb_bc[:, :], op=ALU.add),
                    lambda: V.tensor_reduce(out=r8_[:, 0, :], in_=v3(sel), axis=AX.X, op=ALU.max),
                    lambda: V.tensor_tensor(out=v3(eq), in0=v3(sel), in1=bc_last(r8_[:, 0, :], 8), op=ALU.is_equal),
                    lambda: V.scalar_tensor_tensor(out=sel2, in0=eq, scalar=-1e30, in1=sel, op0=ALU.mult, op1=ALU.add),
                    lambda: V.tensor_reduce(out=r8_[:, 1, :], in_=v3(sel2), axis=AX.X, op=ALU.max),
                    lambda: V.tensor_tensor(out=r8_[:, 2, :], in0=r8_[:, 0, :], in1=r8_[:, 1, :], op=ALU.add),
                    lambda: V.max(out=r8_[:, 3, :], in_=r8_[:, 2, :]),
                    lambda: V.tensor_scalar(out=r8_[:, 4, :], in0=r8_[:, 2, :], scalar1=r8_[:, 3, 3:4], scalar2=None, op0=ALU.is_ge),
                    lambda: V.tensor_scalar(out=r8_[:, 5, :], in0=r8_[:, 4, :], scalar1=1e30, scalar2=-1e30, op0=ALU.mult, op1=ALU.add),
                    lambda: V.tensor_tensor(out=v3(selm), in0=v3(sel), in1=bc_last(r8_[:, 4, :], 8), op=ALU.mult),
                    lambda: V.tensor_tensor(out=v3(selm), in0=v3(selm), in1=bc_last(r8_[:, 5, :], 8), op=ALU.add),
                    lambda: V.max(out=r8_[:, 6, :], in_=selm),
                    lambda: V.tensor_scalar(out=em, in0=selm, scalar1=r8_[:, 6, 5:6], scalar2=None, op0=ALU.is_ge),
                    lambda: V.tensor_copy(out=mk_[:], in_=em),
                    lambda: V.tensor_tensor(out=em, in0=em, in1=sc, op=ALU.mult),
                    lambda: V.tensor_reduce(out=r8_[:, 7, 0:1], in_=em, axis=AX.X, op=ALU.add),
                    lambda: V.reciprocal(out=r8_[:, 7, 0:1], in_=r8_[:, 7, 0:1]),
                    lambda: V.tensor_scalar(out=wdn, in0=em, scalar1=r8_[:, 7, 0:1], scalar2=2.5, op0=ALU.mult, op1=ALU.mult),
                    lambda: V.max(out=W6[:, gt, :], in_=wdn),
                    lambda: V.max_index(i8_[:], W6[:, gt, :], wdn),
                    lambda: V.tensor_copy(out=E6f[:, gt, :], in_=i8_[:]),
                ]

            def post(gt, t_chain):
                s4 = gt % 4
                PE.wait(t_chain, ps_free[5], t_o128, t_mc)
                nc.tensor.matmul(psF[5][:, 0:64], lhsT=U_b[:, :], rhs=mk4[s4][:, :], start=True, stop=True)
                tm2 = PE.sig(nc.tensor.matmul(psF[5][:, 64:128], lhsT=ones128[:, :], rhs=mk4[s4][:, :], start=True, stop=True))
                mk_free4[s4] = tm2
                DVE.wait(tm2, t_c0, st_["t_h"])
                ch(nc.vector.tensor_tensor(out=RK[:, gt, :], in0=psF[5][:, 0:64], in1=carry[:, :], op=ALU.add))
                st_["t_h"] = DVE.sig(nc.vector.tensor_tensor(out=carry[:, :], in0=psF[5][:, 64:128], in1=carry[:, :], op=ALU.add))
                ps_free[5] = st_["t_h"]
                DVE.wait(st_["t_h"])

            pairs = [(2 * k, 2 * k + 1) for k in range(32)]
            ta2s = {}
            for gt in pairs[0]:
                ta2s[gt] = front(gt)
            for k in range(32):
                if k + 1 < 32:
                    for gt in pairs[k + 1]:
                        ta2s[gt] = front(gt)
                chains = [chain_ops(gt) for gt in pairs[k]]
                toks = [[ta2s[gt], t_mc, mk_free4[gt % 4]] for gt in pairs[k]]
                for j in range(len(chains[0])):
                    for ci in range(2):
                        DVE.wait(toks[ci])
                        toks[ci] = DVE.sig(chains[ci][j]())
                for ci, gt in enumerate(pairs[k]):
                    chain_end4[gt % 4] = toks[ci]
                    post(gt, toks[ci])
            xin_free[0], xin_free[1] = [xin_free4[0], xin_free4[2]], [xin_free4[1], xin_free4[3]]
            xs_free[0], xs_free[1] = [xs_free4[0], xs_free4[2]], [xs_free4[1], xs_free4[3]]
            barrier()
        if debug == "m1":
            return nc

        with contextlib.ExitStack() as st1:
            big = sb("big", [128, NBR * 64], F32, st1)
            nbk = sb("nbk", [128, 64], F32, st1)
            bend = sb("bend", [128, 64], F32, st1)
            pstart = sb("pstart", [128, 64], F32, st1)
            ones64 = sb("ones64", [128, 64], F32, st1)
            BEf = sb("BEf", [128, NBLK], F32, st1)
            PAY = sb("PAY", [128, 64, 6, 2], F32, st1)
            fill = sb("fill", [128, NBR * MB // 128, 2], F32, st1)
            SH = sb("SH", [128, 64, 2], F32, st1)
            zrow = sb("zrow", [1, D], BF16, st1)
            j64 = sb("j64", [128, 64], F32, st1)
            d_i = dsem("minit")
            ch = lambda ins: DVE.wait(DVE.sig(ins))
            ch(nc.vector.memset(fill[:, :, 0:1], float(NT)))
            ch(nc.vector.memset(fill[:, :, 1:2], 0.0))
            ch(nc.vector.memset(zrow[:], 0.0))
            ch(nc.vector.memset(SH[:, :, 1:2], 1.0))
            ch(nc.vector.tensor_copy(out=SH[:, :, 0], in_=tokid))
            ch(nc.vector.memset(ones64[:], 1.0))
            SP.wait((DVE, DVE.cnt))
            nc.sync.dma_start(out=ROWTW[0:NBR * MB, :].rearrange("(p j) c -> p j c", p=128), in_=fill[:]).then_inc(d_i.sem, 16)
            nc.sync.dma_start(out=ROWTW[NBR * MB:RROWS, :].rearrange("(j p) c -> p j c", p=128), in_=SH[:]).then_inc(d_i.sem, 16)
            d_i.cnt += 32
            t_init = d_i.sig(nc.sync.dma_start(out=H2[NT:NT + 1, :], in_=zrow[:]))
            big3 = big[:, 0:64 * 16].rearrange("p (e j) -> p e j", j=16)
            ch(nc.vector.tensor_tensor(out=big3, in0=bc_last(carry[:, :], 16), in1=bc_mid(thr16, 64), op=ALU.is_gt))
            ch(nc.vector.tensor_reduce(out=nbk[:], in_=big3, axis=AX.X, op=ALU.add))
            ch(nc.vector.tensor_tensor_scan(out=bend[:], data0=ones64[:], data1=nbk[:], initial=0.0, op0=ALU.mult, op1=ALU.add))
            ch(nc.vector.tensor_tensor(out=pstart[:], in0=bend[:], in1=nbk[:], op=ALU.subtract))
            ch(nc.vector.tensor_scalar(out=pstart[:], in0=pstart[:], scalar1=float(MB), scalar2=None, op0=ALU.mult))
            bigb = big[:, :].rearrange("p (i e) -> p i e", e=64)
            ch(nc.vector.tensor_tensor(out=bigb, in0=bc_last(iota_b[:, 0:NBR], 64), in1=bc_mid(bend[:, :], NBR), op=ALU.is_ge))
            ch(nc.vector.tensor_reduce(out=BEf[:, 0:NBR], in_=bigb, axis=AX.X, op=ALU.add))
            ch(nc.vector.tensor_scalar(out=big[:, 0:NBR], in0=BEf[:, 0:NBR], scalar1=64.0, scalar2=None, op0=ALU.is_ge))
            ch(nc.vector.tensor_tensor(out=BEf[:, 0:NBR], in0=BEf[:, 0:NBR], in1=big[:, 0:NBR], op=ALU.add))
            ch(nc.vector.memset(BEf[:, NBR:NBLK], 64.0))
            ch(nc.vector.tensor_scalar(out=BEf[:, :], in0=BEf[:, :], scalar1=128.0, scalar2=pcol, op0=ALU.mult, op1=ALU.add))
            ch(nc.vector.tensor_copy(out=IDXW[:, :], in_=BEf[:, :]))
            d_sc = dsem("scat")
            POOL.wait(t_init)
            for gt in range(64):
                ch(nc.vector.tensor_tensor(out=RK[:, gt, :], in0=RK[:, gt, :], in1=pstart[:, :], op=ALU.add))
                for k in range(6):
                    stt_ = nc.vector.scalar_tensor_tensor(out=big[:, k * 64:(k + 1) * 64], in0=iota_e, scalar=E6f[:, gt, k:k + 1], in1=RK[:, gt, :],
                                                          op0=ALU.is_equal, op1=ALU.mult, accum_out=D6f[:, gt, k:k + 1])
                DVE.wait(DVE.sig(stt_))
                for k in range(6):
                    nc.vector.tensor_copy(out=PAY[:, gt, k, 0:1], in_=tokid[:, gt:gt + 1])
                ch(nc.vector.tensor_copy(out=PAY[:, gt, :, 1], in_=W6[:, gt, 0:6]))
                tdi = DVE.sig(nc.vector.tensor_copy(out=D6i[:, gt, 0:6], in_=D6f[:, gt, 0:6]))
                DVE.wait(tdi)
                POOL.wait(tdi)
                for k in range(6):
                    t_sc = d_sc.sig(nc.gpsimd.indirect_dma_start(
                        out=ROWTW[:, :], out_offset=bass.IndirectOffsetOnAxis(ap=D6i[:, gt, k:k + 1], axis=0),
                        in_=PAY[:, gt, k, :], in_offset=None))
            barrier()
        if debug == "m1b":
            return nc

        with contextlib.ExitStack() as st1:
            NS = 3
            wg = [sb("wg%d" % i, [128, 2048], BF16, st1) for i in range(NS)]
            wu = [sb("wu%d" % i, [128, 2048], BF16, st1) for i in range(NS)]
            wd = [sb("wd%d" % i, [128, 2048], BF16, st1) for i in range(NS)]
            xg = [sb("xg%d" % i, [128, 4, D], BF16, st1) for i in range(NS)]
            rtw = [sb("rtw%d" % i, [128, 4, 2], F32, st1) for i in range(NS)]
            tki = [sb("tki%d" % i, [128, 4], I32, st1) for i in range(NS)]
            xgT = [sb("xgT%d" % i, [128, 8, MB], BF16, st1) for i in range(2)]
            hm = [sb("hm%d" % i, [128, 2, MB], BF16, st1) for i in range(2)]
            sg = [sb("sg%d" % i, [128, MB], F32, st1) for i in range(2)]
            osb = [sb("osb%d" % i, [128, 4, D], BF16, st1) for i in range(2)]
            d_rt = [dsem("rtw%d" % i) for i in range(NS)]
            d_we = [dsem("we%d" % i) for i in range(NS)]
            d_xg = [dsem("xg%d" % i) for i in range(NS)]
            d_o = [dsem("osb%d" % i) for i in range(2)]
            we_free = [None] * NS
            xg_free = [None] * NS
            rtw_free = [None] * NS
            tki_free = [None] * NS
            t_rtw = [None] * NS
            t_we = [None] * NS
            t_xg = [None] * NS
            xgT_free = [None, None]
            hm_free = [None, None]
            sg_free = [None, None]
            osb_free = [None, None]
            t_xgT = [None, None]
            psB_free = [None, None]
            ev = 0
            bnd_reg = nc.gpsimd.to_reg(65 * 128 - 1)

            def loads(i):
                s3 = i % NS
                SP.wait(rtw_free[s3])
                t_rtw[s3] = d_rt[s3].sig(nc.sync.dma_start(out=rtw[s3][:], in_=ROWTW[i * MB:(i + 1) * MB, :].rearrange("(j p) c -> p j c", p=128)))
                DVE.wait(t_rtw[s3], tki_free[s3])
                tk = DVE.sig(nc.vector.tensor_copy(out=tki[s3][:], in_=rtw[s3][:, :, 0]))
                POOL.wait(xg_free[s3], tk)
                for j in range(4):
                    tx = d_xg[s3].sig(nc.gpsimd.indirect_dma_start(out=xg[s3][:, j, :], out_offset=None, in_=H2[:, :],
                                                                   in_offset=bass.IndirectOffsetOnAxis(ap=tki[s3][:, j:j + 1], axis=0)))
                tki_free[s3] = tx
                t_xg[s3] = tx
                POOL.wait(we_free[s3])
                for m_, wt_ in enumerate((wg, wu, wd)):
                    t_we[s3] = d_we[s3].sig(nc.gpsimd.indirect_dma_start(
                        out=wt_[s3][:, :], out_offset=None, in_=WB[m_][:, :],
                        in_offset=bass.IndirectOffsetOnAxis(ap=IDXW[:, i:i + 1], axis=0), bounds_check=bnd_reg, oob_is_err=False))

            def transposes(i):
                nonlocal ev
                s2 = i % 2
                s3 = i % NS
                PE.wait(t_xg[s3], xgT_free[s2])
                last = []
                for kt in range(8):
                    pb_ = kt % 2
                    PE.wait(psB_free[pb_])
                    for j in range(4):
                        tp = nc.tensor.transpose(psB[pb_][:, j * 128:(j + 1) * 128], xg[s3][:, j, kt * 128:(kt + 1) * 128], id_b[:])
                    tp = PE.sig(tp)
                    if ev % 2 == 0:
                        ACT.wait(tp)
                        te = ACT.sig(nc.scalar.copy(out=xgT[s2][:, kt, :], in_=psB[pb_][:, 0:MB]))
                    else:
                        DVE.wait(tp)
                        te = DVE.sig(nc.vector.tensor_copy(out=xgT[s2][:, kt, :], in_=psB[pb_][:, 0:MB]))
                    ev += 1
                    psB_free[pb_] = te
                    last = (last + [te])[-2:]
                xg_free[s3] = tp
                t_xgT[s2] = last

            for q_ in range(NS):
                for wt_ in (wg, wu, wd):
                    POOL.sig(nc.gpsimd.memset(wt_[q_][:], 0.0))
            we_free = [(POOL, POOL.cnt)] * NS
            for i in range(min(NS, NBLK)):
                loads(i)
            PE.wait(t_mc)
            transposes(0)
            for i in range(NBLK):
                s2 = i % 2
                s3 = i % NS
                PE.wait(t_xgT[s2], t_we[s3])
                for ft in range(2):
                    PE.wait(ps_free[ft * 2], ps_free[ft * 2 + 1])
                    for kt in range(8):
                        nc.tensor.matmul(psF[ft * 2][:, :], lhsT=wg[s3][:, kt * 256 + ft * 128:kt * 256 + (ft + 1) * 128], rhs=xgT[s2][:, kt, :],
                                         start=(kt == 0), stop=(kt == 7))
                    for kt in range(8):
                        mm = nc.tensor.matmul(psF[ft * 2 + 1][:, :], lhsT=wu[s3][:, kt * 256 + ft * 128:kt * 256 + (ft + 1) * 128], rhs=xgT[s2][:, kt, :],
                                              start=(kt == 0), stop=(kt == 7))
                    tm = PE.sig(mm)
                    ACT.wait(tm, sg_free[ft])
                    ta3 = ACT.sig(nc.scalar.activation(out=sg[ft][:], in_=psF[ft * 2][:, :], func=AF.Silu))
                    DVE.wait(ta3, hm_free[s2] if ft == 0 else None)
                    td = DVE.sig(nc.vector.tensor_tensor(out=hm[s2][:, ft, :], in0=psF[ft * 2 + 1][:, :], in1=sg[ft][:], op=ALU.mult))
                    sg_free[ft] = td
                    ps_free[ft * 2] = td
                    ps_free[ft * 2 + 1] = td
                xgT_free[s2] = tm
                t_hm = td
                if i + 1 < NBLK:
                    transposes(i + 1)
                PE.wait(t_hm)
                ACT.wait(osb_free[s2])
                DVE.wait(osb_free[s2])
                evs = []
                for j in range(4):
                    for nh in range(2):
                        pi = 4 + (j * 2 + nh) % 2
                        PE.wait(ps_free[pi])
                        for ft in range(2):
                            mm = nc.tensor.matmul(psF[pi][:, :], lhsT=hm[s2][:, ft, j * 128:(j + 1) * 128],
                                                  rhs=wd[s3][:, ft * 1024 + nh * 512:ft * 1024 + (nh + 1) * 512], start=(ft == 0), stop=(ft == 1))
                        tm = PE.sig(mm)
                        o_ = osb[s2][:, j, nh * 512:(nh + 1) * 512]
                        if nh == 0:
                            ACT.wait(tm, t_rtw[s3])
                            te = ACT.sig(nc.scalar.activation(out=o_, in_=psF[pi][:, :], func=AF.Copy, scale=rtw[s3][:, j, 1:2]))
                        else:
                            DVE.wait(tm, t_rtw[s3])
                            te = DVE.sig(nc.vector.tensor_scalar(out=o_, in0=psF[pi][:, :], scalar1=rtw[s3][:, j, 1:2], scalar2=None, op0=ALU.mult))
                        ps_free[pi] = te
                        evs.append(te)
                hm_free[s2] = tm
                we_free[s3] = tm
                rtw_free[s3] = evs[-2:]
                SP.wait(evs[-1], evs[-2])
                osb_free[s2] = d_o[s2].sig(nc.sync.dma_start(out=OUTR[i * MB:(i + 1) * MB, :].rearrange("(j p) n -> p j n", p=128), in_=osb[s2][:]))
                if i + NS < NBLK:
                    loads(i + NS)
            barrier()
        if debug == "m2":
            return nc

        with contextlib.ExitStack() as st1:
            gbuf = [sb("gbuf%d" % i, [128, 7, D], BF16, st1) for i in range(2)]
            d_g = [dsem("gb%d" % i) for i in range(2)]
            d_y = [dsem("my%d" % i) for i in range(2)]
            gb_free = [None, None]
            ch = lambda ins: DVE.wait(DVE.sig(ins))
            for gt in range(64):
                seg = gt // 16
                b = gt % 2
                POOL.wait(gb_free[b])
                for k in range(6):
                    nc.gpsimd.indirect_dma_start(out=gbuf[b][:, k, :], out_offset=None, in_=OUTR[:, :],
                                                 in_offset=bass.IndirectOffsetOnAxis(ap=D6i[:, gt, k:k + 1], axis=0)).then_inc(d_g[b].sem, 16)
                d_g[b].cnt += 96
                SP.wait(gb_free[b], xin_free[b])
                nc.sync.dma_start(out=gbuf[b][:, 6, :], in_=OUTR[NBR * MB + gt * 128:NBR * MB + (gt + 1) * 128, :]).then_inc(d_g[b].sem, 16)
                d_g[b].cnt += 16
                tl = d_g[b].sig(nc.sync.dma_start(out=xin[b][:], in_=X1_v[gt]))
                PE.wait(tl, ps_free[2 * b], ps_free[2 * b + 1])
                for nh in range(2):
                    for k in range(7):
                        mm = nc.tensor.matmul(psF[2 * b + nh][:, :], lhsT=id_b[:, :], rhs=gbuf[b][:, k, nh * 512:(nh + 1) * 512],
                                              start=(k == 0), stop=(k == 6))
                tm = PE.sig(mm)
                gb_free[b] = tm
                DVE.wait(tm, xs_free[b], t_g2)
                a_ = xs[b]
                for nh in range(2):
                    hs_ = slice(nh * 512, (nh + 1) * 512)
                    ch(nc.vector.tensor_tensor(out=a_[:, hs_], in0=psF[2 * b + nh][:, :], in1=G2all[:, seg, hs_], op=ALU.mult))
                ps_free[2 * b] = (DVE, DVE.cnt)
                ps_free[2 * b + 1] = (DVE, DVE.cnt)
                td = DVE.sig(nc.vector.tensor_tensor(out=a_[:], in0=a_[:], in1=xin[b][:], op=ALU.add))
                xin_free[b] = td
                ACT.wait(td)
                col = gt
                ta = ACT.sig(nc.scalar.activation(out=junk[:], in_=a_[:], func=AF.Square, accum_out=ssq[:, col:col + 1]))
                ACT.wait(ta)
                ta = ACT.sig(nc.scalar.activation(out=ssq[:, col:col + 1], in_=ssq[:, col:col + 1], func=AF.Sqrt, scale=1.0 / D, bias=EPS))
                DVE.wait(ta)
                ch(nc.vector.reciprocal(out=ssq[:, col:col + 1], in_=ssq[:, col:col + 1]))
                td = DVE.sig(nc.vector.scalar_tensor_tensor(out=a_[:], in0=a_[:], scalar=ssq[:, col:col + 1], in1=FNbc[:, :], op0=ALU.mult, op1=ALU.mult))
                SP.wait(td)
                xs_free[b] = d_y[b].sig(nc.sync.dma_start(out=y_v[gt], in_=a_[:]))
            barrier()
    es.close()
    return nc


def _rel_bucket(rel):
    half = 16
    max_exact = 8
    n = np.abs(rel)
    large = max_exact + (np.log(np.maximum(n, 1) / max_exact) / math.log(1024 / max_exact) * (half - max_exact)).astype(np.int32)
    large = np.minimum(large, half - 1)
    return ((rel > 0).astype(np.int32) * half + np.where(n < max_exact, n, large)).astype(np.int32)


def _onehot_tables():
    oh = np.zeros((32, 4, 512), np.float32)
    for kind, (Hb, d) in enumerate(((128, 1), (64, 1), (64, 4), (64, 16))):
        Rmax = 2 * Hb - 1
        for i in range(4 * Hb - 1):
            rel = Rmax - i
            if abs(rel) <= Hb:
                oh[int(_rel_bucket(np.array([rel * d]))[0]), kind, i] = 1.0
    return oh


def _lay_gu(w, wsh):
    a = np.concatenate([w, wsh[None]], axis=0)
    a = a.reshape(65, 8, 128, 256).transpose(0, 2, 1, 3)
    return np.ascontiguousarray(a.reshape(65 * 128, 2048))


def _lay_d(w, wsh):
    a = np.concatenate([w, wsh[None]], axis=0)
    a = a.reshape(65, 2, 128, 1024).transpose(0, 2, 1, 3)
    return np.ascontiguousarray(a.reshape(65 * 128, 2048))


def _consts():
    c = np.zeros((128, NCST), np.float32)
    p = np.arange(128, dtype=np.float32)[:, None]
    c[:, 0:64] = np.arange(64, dtype=np.float32)[None, :]
    c[:, 64:128] = np.arange(64, dtype=np.float32)[None, :] * 128 + p
    c[:, 128:129] = p
    c[:, 129:129 + NBLK] = np.arange(NBLK, dtype=np.float32)[None, :]
    c[:, 129 + NBLK:] = np.arange(16, dtype=np.float32)[None, :] * MB
    return c


def make_in_maps(inp):
    f = lambda a: np.ascontiguousarray(np.asarray(a, dtype=np.float32))
    xp = f(inp["x_prompt"]); xs_ = f(inp["x_sample"]); cp = f(inp["c_prompt"]); cs = f(inp["c_sample"])
    common = {
        "rel_bias": f(inp["rel_bias"]), "sink": f(inp["sink"]).reshape(1, 8),
        "w_ada": f(inp["w_ada"])[0], "b_adaT": f(f(inp["b_ada"])[0].reshape(48, 128).T),
        "norm1T": f(f(inp["norm1"])[0].reshape(8, 128).T), "norm2T": f(f(inp["norm2"])[0].reshape(8, 128).T),
        "final_norm": f(inp["final_norm"]).reshape(1, D),
        "w_in": f(inp["w_in"])[0], "w_pa": f(inp["w_pa"])[0], "w_pb": f(inp["w_pb"])[0], "w_o": f(inp["w_o"])[0],
        "w_router": f(inp["w_router"])[0], "router_bias": f(inp["router_bias"]).reshape(1, NE),
        "wg2": _lay_gu(f(inp["w_gate"])[0], f(inp["ws_gate"])[0]), "wu2": _lay_gu(f(inp["w_up"])[0], f(inp["ws_up"])[0]),
        "wd2": _lay_d(f(inp["w_down"])[0], f(inp["ws_down"])[0]),
        "cst": _consts(), "umat": np.ascontiguousarray(np.triu(np.ones((128, 128), np.float32), 1)),
        "ident": np.eye(128, dtype=np.float32), "jmat": np.ascontiguousarray(np.eye(128, dtype=np.float32)[::-1]),
        "oh": _onehot_tables(),
    }
    maps = []
    for core in range(8):
        if core < 4:
            xc = xp[core]
            cc = np.repeat(cp[core:core + 1], 4, axis=0)
            cn = 1.0
        else:
            k = core - 4
            xc = xs_[4 * k:4 * k + 4].reshape(NT, D)
            cc = cs[4 * k:4 * k + 4]
            cn = 0.0
        m = dict(common)
        m["x"] = f(xc)
        m["cT"] = f(cc.reshape(4, 8, 128).transpose(2, 1, 0))
        m["conn"] = np.full((128, 1), cn, np.float32)
        maps.append(m)
    return maps


_NC_CACHE = {}


def kernel(**inputs):
    if "nc" not in _NC_CACHE:
        _NC_CACHE["nc"] = build()
    nc = _NC_CACHE["nc"]
    maps = make_in_maps(inputs)
    res = run_bass_kernel_spmd(nc, maps, core_ids=list(range(8)))
    ys = [np.asarray(r["y"], dtype=np.float32) for r in res.results]
    y_prompt = np.stack(ys[0:4], axis=0)
    y_sample = np.concatenate([ys[4 + k].reshape(4, 2048, D) for k in range(4)], axis=0)
    return (y_prompt, y_sample)
```
